# Optimizing a Trainium2 kernel written in Bass

```python
import math
import jax
import jax.numpy as jnp
from jax import lax
import numpy as np

D_MODEL = 1024
BATCH = 8
SEQ = 8192
DEPTH = 2

GRID_W = 64
N_BRANCHES = 4
BRANCH_W = D_MODEL // N_BRANCHES
HEAD_DIM = 64
N_Q_HEADS = BRANCH_W // HEAD_DIM
N_KV_HEADS = 2
Q_GROUP = N_Q_HEADS // N_KV_HEADS
ROPE_HALF = HEAD_DIM // 2
ROPE_THETA = 10000.0
Q_BLOCK = 128
SC_WIDTH = 3
CF_WIDTH = 31
POOL_WINDOWS = (2, 4, 8, 16)
POOL_GROUP = BRANCH_W // len(POOL_WINDOWS)
N_EXPERTS = 32
TOP_K = 4
D_EXPERT = D_MODEL
SWIGLU_LIMIT = 7.0
SWIGLU_ALPHA = 1.702
MOE_BLOCK = 256
DN_ALPHA = (2.0 * DEPTH) ** 0.25
DN_BETA = (8.0 * DEPTH) ** -0.25
LN_EPS = 1e-5
RMS_EPS = 1e-6

Q_COLS = N_Q_HEADS * HEAD_DIM
KV_COLS = N_KV_HEADS * HEAD_DIM
IN_SIZES = (Q_COLS, KV_COLS, KV_COLS, 3 * BRANCH_W, 2 * BRANCH_W, BRANCH_W, N_BRANCHES * D_MODEL)
IN_COLS = sum(IN_SIZES)
IN_SPLITS = [int(s) for s in np.cumsum(IN_SIZES)[:-1]]

kernel_name = "hybrid_gated_branch_encoder"


def layer_norm(x, g, b):
    xf = x.astype(jnp.float32)
    mu = jnp.mean(xf, axis=-1, keepdims=True)
    var = jnp.mean(jnp.square(xf - mu), axis=-1, keepdims=True)
    return ((xf - mu) * lax.rsqrt(var + LN_EPS) * g + b).astype(x.dtype)


def rms_norm_f32(x, g):
    xf = x.astype(jnp.float32)
    return xf * lax.rsqrt(jnp.mean(jnp.square(xf), axis=-1, keepdims=True) + RMS_EPS) * g


def axial_rope_tables(seq):
    rows = seq // GRID_W
    row = jnp.repeat(jnp.arange(rows, dtype=jnp.float32), GRID_W)
    col = jnp.tile(jnp.arange(GRID_W, dtype=jnp.float32), rows)
    inv = ROPE_THETA ** (-jnp.arange(0, ROPE_HALF, 2, dtype=jnp.float32) / ROPE_HALF)
    ang_r = row[:, None] * inv
    ang_c = col[:, None] * inv
    return (jnp.cos(ang_r), jnp.sin(ang_r), jnp.cos(ang_c), jnp.sin(ang_c))


def apply_axial_rope(x, tabs):
    cr, sr, cc, sc = (t[:, None, :] for t in tabs)
    r1, r2, c1, c2 = jnp.split(x, 4, axis=-1)
    return jnp.concatenate([r1 * cr - r2 * sr, r2 * cr + r1 * sr,
                            c1 * cc - c2 * sc, c2 * cc + c1 * sc], axis=-1)


def gqa_attention(zq, zk, zv, q_gain, k_gain, tabs):
    B, S, _ = zq.shape
    dt = zq.dtype
    q = apply_axial_rope(rms_norm_f32(zq.reshape(B, S, N_Q_HEADS, HEAD_DIM), q_gain), tabs) * (HEAD_DIM ** -0.5)
    k = apply_axial_rope(rms_norm_f32(zk.reshape(B, S, N_KV_HEADS, HEAD_DIM), k_gain), tabs).astype(dt)
    v = zv.reshape(B, S, N_KV_HEADS, HEAD_DIM)
    q = q.astype(dt).reshape(B, S // Q_BLOCK, Q_BLOCK, N_KV_HEADS, Q_GROUP, HEAD_DIM)
    q = jnp.moveaxis(q, 1, 0)

    def block(qb):
        s = jnp.einsum('bqkgd,bskd->bkgqs', qb, k, preferred_element_type=jnp.float32)
        p = jax.nn.softmax(s, axis=-1).astype(dt)
        return jnp.einsum('bkgqs,bskd->bqkgd', p, v)

    o = lax.map(block, q)
    return jnp.moveaxis(o, 0, 1).reshape(B, S, N_Q_HEADS * HEAD_DIM)


def depthwise_conv(x, w):
    K, C = w.shape
    return lax.conv_general_dilated(x, w[:, None, :].astype(x.dtype), window_strides=(1,),
                                    padding=[(K // 2, K // 2)],
                                    dimension_numbers=('NWC', 'WIO', 'NWC'),
                                    feature_group_count=C)


def short_gated_conv(z, conv_w):
    b_gate, c_gate, u = jnp.split(z, 3, axis=-1)
    return b_gate * depthwise_conv(c_gate * u, conv_w)


def conformer_conv(z, conv_w, conv_b, ln_g, ln_b):
    a, g = jnp.split(z, 2, axis=-1)
    y = depthwise_conv(a * jax.nn.sigmoid(g), conv_w) + conv_b
    return jax.nn.silu(layer_norm(y, ln_g, ln_b))


def multiscale_pool(u, pool_w, pool_scale):
    B, S, W = u.shape
    uf = u.astype(jnp.float32)
    csum = jnp.pad(jnp.cumsum(uf, axis=1), ((0, 0), (1, 0), (0, 0)))
    t = jnp.arange(S)
    outs = []
    for i, w in enumerate(POOL_WINDOWS):
        lo = jnp.maximum(t - w // 2, 0)
        hi = jnp.minimum(t + (w - 1 - w // 2), S - 1)
        sl = slice(i * POOL_GROUP, (i + 1) * POOL_GROUP)
        c = csum[:, :, sl]
        mean = (c[:, hi + 1] - c[:, lo]) / (hi - lo + 1).astype(jnp.float32)[:, None]
        outs.append(mean - uf[:, :, sl])
    y = jnp.stack(outs, axis=2).astype(u.dtype)
    y = jnp.einsum('bsgc,gcd->bsgd', y, pool_w).reshape(B, S, W)
    return y * pool_scale


def token_mixer(x, w_in, b_in, q_g, k_g, sc_w, cf_w, cf_b, cf_g, cf_beta,
                pool_w, pool_scale, w_branch, w_out, tabs):
    B, S, D = x.shape
    z = x @ w_in + b_in
    zq, zk, zv, zsc, zcf, zpool, zgate = jnp.split(z, IN_SPLITS, axis=-1)
    branches = jnp.stack([
        gqa_attention(zq, zk, zv, q_g, k_g, tabs),
        short_gated_conv(zsc, sc_w),
        conformer_conv(zcf, cf_w, cf_b, cf_g, cf_beta),
        multiscale_pool(zpool, pool_w, pool_scale),
    ], axis=2)
    proj = jnp.einsum('bsgc,gcd->bsgd', branches, w_branch)
    gates = jax.nn.sigmoid(zgate.reshape(B, S, N_BRANCHES, D))
    merged = jnp.sum(gates * proj, axis=2)
    return merged @ w_out


def moe_ffn(x, w_router, b_router, w_gu, b_gu, w_down, b_down):
    B, S, D = x.shape
    xt = x.reshape(-1, D)
    n_tok = xt.shape[0]
    n_assign = n_tok * TOP_K
    logits = jnp.dot(xt, w_router, preferred_element_type=jnp.float32) + b_router.astype(jnp.float32)
    top_logits, top_idx = lax.top_k(logits, TOP_K)
    top_w = jax.nn.softmax(top_logits, axis=-1)
    flat_e = top_idx.reshape(-1)
    flat_tok = jnp.broadcast_to(jnp.arange(n_tok, dtype=jnp.int32)[:, None], (n_tok, TOP_K)).reshape(-1)
    order = jnp.argsort(flat_e)
    se, stok, sw = flat_e[order], flat_tok[order], top_w.reshape(-1)[order]
    counts = jnp.bincount(flat_e, length=N_EXPERTS)
    start = jnp.cumsum(counts) - counts
    padded = (counts + MOE_BLOCK - 1) // MOE_BLOCK * MOE_BLOCK
    pend = jnp.cumsum(padded)
    pstart = pend - padded
    dest = pstart[se] + jnp.arange(n_assign, dtype=jnp.int32) - start[se]
    n_slots = (n_assign + MOE_BLOCK - 1) // MOE_BLOCK * MOE_BLOCK + N_EXPERTS * MOE_BLOCK
    slot_tok = jnp.zeros((n_slots,), jnp.int32).at[dest].set(stok)
    slot_w = jnp.zeros((n_slots,), jnp.float32).at[dest].set(sw)
    n_blocks = n_slots // MOE_BLOCK
    blk_e = jnp.minimum(jnp.searchsorted(pend, jnp.arange(n_blocks) * MOE_BLOCK, side='right'),
                        N_EXPERTS - 1)

    def expert_block(args):
        tok, e = args
        h = jnp.dot(xt[tok], w_gu[e]) + b_gu[e]
        glu, lin = jnp.split(h, 2, axis=-1)
        glu = jnp.minimum(glu, SWIGLU_LIMIT)
        lin = jnp.clip(lin, -SWIGLU_LIMIT, SWIGLU_LIMIT)
        act = glu * jax.nn.sigmoid(SWIGLU_ALPHA * glu) * (lin + 1.0)
        return jnp.dot(act, w_down[e]) + b_down[e]

    y = lax.map(expert_block, (slot_tok.reshape(n_blocks, MOE_BLOCK), blk_e))
    y = y.reshape(n_slots, D).astype(jnp.float32) * slot_w[:, None]
    out = jax.ops.segment_sum(y, slot_tok, num_segments=n_tok)
    return out.astype(x.dtype).reshape(B, S, D)


def setup_inputs(seed: int = 0) -> dict:
    key = jax.random.key(seed)
    ks = jax.random.split(key, 26)
    f32 = jnp.float32

    def nrm(k, shape, scale):
        return jax.random.normal(k, shape, f32) * scale

    D, W, E, F, L = D_MODEL, BRANCH_W, N_EXPERTS, D_EXPERT, DEPTH
    return {
        "x": nrm(ks[0], (BATCH, SEQ, D), 1.0),
        "ln_in_g": 1.0 + nrm(ks[1], (D,), 0.02),
        "ln_in_b": nrm(ks[2], (D,), 0.02),
        "w_in": nrm(ks[3], (L, D, IN_COLS), D ** -0.5),
        "b_in": nrm(ks[4], (L, IN_COLS), 0.02),
        "q_norm_g": 1.0 + nrm(ks[5], (L, HEAD_DIM), 0.02),
        "k_norm_g": 1.0 + nrm(ks[6], (L, HEAD_DIM), 0.02),
        "sc_conv_w": nrm(ks[7], (L, SC_WIDTH, W), SC_WIDTH ** -0.5),
        "cf_conv_w": nrm(ks[8], (L, CF_WIDTH, W), CF_WIDTH ** -0.5),
        "cf_conv_b": nrm(ks[9], (L, W), 0.02),
        "cf_ln_g": 1.0 + nrm(ks[10], (L, W), 0.02),
        "cf_ln_b": nrm(ks[11], (L, W), 0.02),
        "pool_w": nrm(ks[12], (L, len(POOL_WINDOWS), POOL_GROUP, POOL_GROUP), POOL_GROUP ** -0.5),
        "pool_scale": 1.0 + nrm(ks[13], (L, W), 0.02),
        "w_branch": nrm(ks[14], (L, N_BRANCHES, W, D), DN_BETA * W ** -0.5),
        "w_out": nrm(ks[15], (L, D, D), DN_BETA * D ** -0.5),
        "ln1_g": 1.0 + nrm(ks[16], (L, D), 0.02),
        "ln1_b": nrm(ks[17], (L, D), 0.02),
        "w_router": nrm(ks[18], (L, D, E), D ** -0.5),
        "b_router": nrm(ks[19], (L, E), 0.01),
        "w_gate_up": nrm(ks[20], (L, E, D, 2 * F), D ** -0.5),
        "b_gate_up": nrm(ks[21], (L, E, 2 * F), 0.02),
        "w_down": nrm(ks[22], (L, E, F, D), DN_BETA * F ** -0.5),
        "b_down": nrm(ks[23], (L, E, D), 0.02),
        "ln2_g": 1.0 + nrm(ks[24], (L, D), 0.02),
        "ln2_b": nrm(ks[25], (L, D), 0.02),
    }


def reference(x, ln_in_g, ln_in_b, w_in, b_in, q_norm_g, k_norm_g, sc_conv_w, cf_conv_w,
              cf_conv_b, cf_ln_g, cf_ln_b, pool_w, pool_scale, w_branch, w_out, ln1_g, ln1_b,
              w_router, b_router, w_gate_up, b_gate_up, w_down, b_down, ln2_g, ln2_b):
    tabs = axial_rope_tables(x.shape[1])
    h = layer_norm(x, ln_in_g, ln_in_b)
    for l in range(DEPTH):
        mix = token_mixer(h, w_in[l], b_in[l], q_norm_g[l], k_norm_g[l], sc_conv_w[l],
                          cf_conv_w[l], cf_conv_b[l], cf_ln_g[l], cf_ln_b[l], pool_w[l],
                          pool_scale[l], w_branch[l], w_out[l], tabs)
        h = layer_norm(DN_ALPHA * h + mix, ln1_g[l], ln1_b[l])
        ffn = moe_ffn(h, w_router[l], b_router[l], w_gate_up[l], b_gate_up[l], w_down[l], b_down[l])
        h = layer_norm(DN_ALPHA * h + ffn, ln2_g[l], ln2_b[l])
    return h
```

```python
import math
from contextlib import ExitStack
import numpy as np
import concourse.bass as bass
import concourse.mybir as mybir
from concourse.bass_utils import run_bass_kernel_spmd

F32 = mybir.dt.float32
BF16 = mybir.dt.bfloat16
I32 = mybir.dt.int32
AF = mybir.ActivationFunctionType
ALU = mybir.AluOpType
AX = mybir.AxisListType

ENGS = ("sync", "scalar", "vector", "gpsimd", "tensor")
S = 8192
D = 1024
NT = 64
NB = 16
NE = 32
CAP = 1536
NSLOT = NE * CAP
DEPTH = 2
DN_ALPHA = (2.0 * DEPTH) ** 0.25
LN_EPS = 1e-5
HALO = 16
ZW = 512 + 2 * HALO


class Prog:
    def __init__(self, nc):
        self.nc = nc
        self.esem = {e: nc.alloc_semaphore("es_" + e) for e in ENGS}
        self.ebase = {e: 0 for e in ENGS}
        self.dsem = {}
        self.nflush = 0
        self._reset()

    def _reset(self):
        self.ops = {e: [] for e in ENGS}
        self.waited = {e: {} for e in ENGS}
        self.last_w = {}
        self.readers = {}
        self.signal = {e: set() for e in ENGS}

    def _need(self, eng, ev, waits):
        if ev is None:
            return
        key = (ev[0], ev[1])
        if self.waited[eng].get(key, -1) >= ev[2]:
            return
        waits[key] = max(waits.get(key, -1), ev[2])

    def _deps(self, eng, reads, writes, extra=()):
        waits = {}
        for k in reads:
            self._need(eng, self.last_w.get(k), waits)
        for k in writes:
            self._need(eng, self.last_w.get(k), waits)
            for ev in self.readers.get(k, ()):
                self._need(eng, ev, waits)
        for ev in extra:
            self._need(eng, ev, waits)
        out = []
        for key, v in waits.items():
            self.waited[eng][key] = v
            out.append((key[0], key[1], v))
            if key[0] == "e":
                self.signal[key[1]].add(v)
        return out

    def _commit(self, ev, reads, writes):
        for k in reads:
            self.readers.setdefault(k, []).append(ev)
        for k in writes:
            self.last_w[k] = ev
            self.readers[k] = []

    def op(self, eng, fn, reads=(), writes=(), extra=()):
        waits = self._deps(eng, reads, writes, extra)
        idx = len(self.ops[eng])
        self.ops[eng].append(dict(fn=fn, waits=waits, dma=None))
        ev = ("e", eng, idx)
        self._commit(ev, reads, writes)
        return ev

    def dma(self, eng, fn, semkey, reads=(), writes=(), extra=()):
        waits = self._deps(eng, reads, writes, extra)
        sn = "ds_" + semkey
        if sn not in self.dsem:
            self.dsem[sn] = [self.nc.alloc_semaphore(sn), 0]
        st = self.dsem[sn]
        st[1] += 16
        self.ops[eng].append(dict(fn=fn, waits=waits, dma=(st[0], st[1])))
        ev = ("d", sn, st[1])
        self._commit(ev, reads, writes)
        return ev

    def wait(self, eng, evs):
        waits = self._deps(eng, (), (), evs)
        self.ops[eng].append(dict(fn=None, waits=waits, dma=None))

    def flush(self, final_events=()):
        nc = self.nc
        if final_events:
            self.wait("sync", final_events)
        val = {}
        for e in ENGS:
            v = self.ebase[e]
            m = {}
            for idx in sorted(self.signal[e]):
                v += 1
                m[idx] = v
            val[e] = m
            self.ebase[e] = v
        ops, esem, dsem = self.ops, self.esem, self.dsem

        def emit(e, name):
            for idx, o in enumerate(ops[name]):
                for kind, who, v in o["waits"]:
                    if kind == "e":
                        e.wait_ge(esem[who], val[who][v])
                    else:
                        e.wait_ge(dsem[who][0], v)
                if o["fn"] is None:
                    continue
                ins = o["fn"](e)
                if o["dma"] is not None:
                    ins.then_inc(o["dma"][0], 16)
                elif idx in val[name]:
                    ins.then_inc(esem[name], 1)

        with nc.Block() as block:
            @block.sync
            def _(e):
                emit(e, "sync")

            @block.scalar
            def _(e):
                emit(e, "scalar")

            @block.vector
            def _(e):
                emit(e, "vector")

            @block.gpsimd
            def _(e):
                emit(e, "gpsimd")

            @block.tensor
            def _(e):
                emit(e, "tensor")
        self.nflush += 1
        self._reset()


_REG = {}


def bc_reg(e, P):
    key = P.nflush
    if key not in _REG:
        _REG.clear()
        _REG[key] = e.to_reg(NSLOT - 1)
    return _REG[key]


class Ring:
    def __init__(self, items):
        self.items = items
        self.i = -1

    def next(self):
        self.i = (self.i + 1) % len(self.items)
        return self.items[self.i]


class Ctx:
    _uid = [0]

    def __init__(self, nc, P):
        self.nc, self.P = nc, P
        self.stack = ExitStack()
        self.n = 0
        Ctx._uid[0] += 1
        self.uid = Ctx._uid[0]

    def sb(self, shape, dt, name=None):
        self.n += 1
        return self.stack.enter_context(self.nc.sbuf_tensor(name or f"t{self.n}_{self.uid}", list(shape), dt))

    def ps(self, shape, dt, name=None):
        self.n += 1
        return self.stack.enter_context(self.nc.psum_tensor(name or f"p{self.n}_{self.uid}", list(shape), dt))

    def ring(self, n, shape, dt, key, psum=False):
        return Ring([((self.ps if psum else self.sb)(shape, dt), f"{key}{i}") for i in range(n)])

    def close(self):
        self.stack.close()


def load_consts(cx, P, ident_d):
    nc = cx.nc
    idf = cx.sb([128, 128], F32)
    idb = cx.sb([128, 128], BF16)
    P.dma("sync", lambda e: e.dma_start(out=idf[:], in_=ident_d[:, :]), "c0", writes=["idf"])
    P.op("vector", lambda e: e.tensor_copy(out=idb[:], in_=idf[:]), reads=["idf"], writes=["idb"])
    return idf, idb


def ln_core(P, cx, r, rk, gt, bt, of, ofk, ob, obk, sm):
    st, mv, rs, nb, xn, k = sm
    for c in range(2):
        P.op("vector", lambda e, c=c: e.bn_stats(out=st[:, c, :], in_=r[:, c * 512:(c + 1) * 512]),
             reads=[rk], writes=[k + "st%d" % c])
    P.op("vector", lambda e: e.bn_aggr(out=mv[:], in_=st[:].rearrange("p a b -> p (a b)")),
         reads=[k + "st0", k + "st1"], writes=[k + "mv"])
    P.op("vector", lambda e: e.tensor_scalar(out=rs[:], in0=mv[:, 1:2], scalar1=LN_EPS, scalar2=None, op0=ALU.add),
         reads=[k + "mv"], writes=[k + "rs"])
    P.op("scalar", lambda e: e.activation(out=rs[:], in_=rs[:], func=AF.Sqrt), reads=[k + "rs"], writes=[k + "rs"])
    P.op("vector", lambda e: e.reciprocal(out=rs[:], in_=rs[:]), reads=[k + "rs"], writes=[k + "rs"])
    P.op("vector", lambda e: e.tensor_scalar(out=nb[:], in0=mv[:, 0:1], scalar1=rs[:, 0:1], scalar2=-1.0,
                                             op0=ALU.mult, op1=ALU.mult),
         reads=[k + "mv", k + "rs"], writes=[k + "nb"])
    P.op("scalar", lambda e: e.activation(out=xn[:], in_=r[:], func=AF.Identity, scale=rs[:, 0:1], bias=nb[:, 0:1]),
         reads=[rk, k + "rs", k + "nb"], writes=[k + "xn"])
    P.op("vector", lambda e: e.tensor_tensor(out=xn[:], in0=xn[:], in1=gt[:], op=ALU.mult),
         reads=[k + "xn", "lng"], writes=[k + "xn"])
    P.op("gpsimd", lambda e: e.tensor_tensor(out=of[:], in0=xn[:], in1=bt[:], op=ALU.add),
         reads=[k + "xn", "lnb"], writes=[ofk])
    if ob is not None:
        P.op("scalar", lambda e: e.copy(out=ob[:], in_=of[:]), reads=[ofk], writes=[obk])


def ln_scratch(cx, n=2):
    return Ring([(cx.sb([128, 2, 6], F32), cx.sb([128, 2], F32), cx.sb([128, 1], F32), cx.sb([128, 1], F32),
                  cx.sb([128, 1024], F32), f"ln{i}") for i in range(n)])


def load_ln_params(P, cx, g_ap, b_ap):
    gt = cx.sb([128, 1024], F32)
    bt = cx.sb([128, 1024], F32)
    P.dma("sync", lambda e: e.dma_start(out=gt[:], in_=g_ap.partition_broadcast(128)), "c1", writes=["lng"])
    P.dma("sync", lambda e: e.dma_start(out=bt[:], in_=b_ap.partition_broadcast(128)), "c2", writes=["lnb"])
    return gt, bt


def transpose_store_hT(P, hb, hbk, pT, pTk, hTt, hTk, idb, hT_d, t, semkey):
    for c in range(8):
        P.op("tensor", lambda e, c=c: e.transpose(out=pT[:, c, :], in_=hb[:, c * 128:(c + 1) * 128], identity=idb[:]),
             reads=[hbk, "idb"], writes=[pTk + "_%d" % c])
    P.op("vector", lambda e: e.tensor_copy(out=hTt[:], in_=pT[:]), reads=[pTk + "_%d" % c for c in range(8)],
         writes=[hTk])
    return P.dma("sync", lambda e: e.dma_start(
        out=hT_d.rearrange("(c p) s -> p c s", p=128)[:, :, t * 128:(t + 1) * 128], in_=hTt[:]),
        semkey, reads=[hTk])


def phase_ln_in(nc, P, T):
    cx = Ctx(nc, P)
    idf, idb = load_consts(cx, P, T["ident"])
    gt, bt = load_ln_params(P, cx, T["ln_in_g"], T["ln_in_b"])
    xr = cx.ring(2, [128, 1024], F32, "x")
    ofr = cx.ring(2, [128, 1024], F32, "of")
    obr = cx.ring(2, [128, 1024], BF16, "ob")
    pTr = cx.ring(2, [128, 8, 128], BF16, "pT", psum=True)
    hTr = cx.ring(2, [128, 8, 128], BF16, "hT")
    smr = ln_scratch(cx)
    evs = []
    for t in range(NT):
        xt, xk = xr.next()
        P.dma("sync", lambda e, xt=xt, t=t: e.dma_start(out=xt[:], in_=T["x"][t * 128:(t + 1) * 128, :]), xk, writes=[xk])
        of, ofk = ofr.next()
        ob, obk = obr.next()
        ln_core(P, cx, xt, xk, gt, bt, of, ofk, ob, obk, smr.next())
        evs.append(P.dma("sync", lambda e, of=of, t=t: e.dma_start(out=T["h_d"][t * 128:(t + 1) * 128, :], in_=of[:]),
                         ofk, reads=[ofk]))
        pT, pTk = pTr.next()
        hTt, hTk = hTr.next()
        evs.append(transpose_store_hT(P, ob, obk, pT, pTk, hTt, hTk, idb, T["hT_d"], t, hTk))
    P.flush(evs)
    cx.close()


A_STAGE = 9
QPERM = [0, 2, 1, 3]


def phase_A(nc, P, T, l, QTa, QTb, KT, Vaug):
    cx = Ctx(nc, P)
    idf, idb = load_consts(cx, P, T["ident"])
    w_in = T["w_in"][l]
    b_in = T["b_in"][l]
    wv = w_in.rearrange("(c p) n -> p c n", p=128)
    w1 = cx.sb([128, 8, 2048], BF16)
    for s in range(4):
        hs = QPERM[s]
        P.dma("gpsimd", lambda e, s=s, hs=hs: e.dma_start(out=w1[:, :, s * 64:(s + 1) * 64], in_=wv[:, :, hs * 64:(hs + 1) * 64]),
              "w1q", writes=[("w1", s)])
    P.dma("gpsimd", lambda e: e.dma_start(out=w1[:, :, 256:2048], in_=wv[:, :, 256:2048]), "w1r", writes=[("w1", 4)])
    w1k = [("w1", i) for i in range(5)]
    bq = cx.sb([128, 512], F32)
    for s in range(4):
        hs = QPERM[s]
        P.dma("sync", lambda e, s=s, hs=hs: e.dma_start(out=bq[:, s * 64:(s + 1) * 64],
                                                        in_=b_in[hs * 64:(hs + 1) * 64].partition_broadcast(128)),
              "c1", writes=[("bq", s)])
    P.dma("sync", lambda e: e.dma_start(out=bq[:, 256:512], in_=b_in[256:512].partition_broadcast(128)), "c2",
          writes=[("bq", 4)])
    bqk = [("bq", i) for i in range(5)]
    bst = cx.sb([12, 128], F32)
    bc = cx.sb([128, 12], F32)
    P.dma("sync", lambda e: e.dma_start(out=bst[:], in_=b_in[512:2048].rearrange("(c p) -> c p", p=128)), "c3", writes=["bst"])
    pB = cx.ps([128, 12], F32)
    P.op("tensor", lambda e: e.transpose(out=pB[:], in_=bst[:], identity=idf[0:12, 0:12]), reads=["bst", "idf"], writes=["pB"])
    P.op("vector", lambda e: e.tensor_copy(out=bc[:], in_=pB[:]), reads=["pB"], writes=["bc"])
    gq = cx.sb([128, 384], F32)
    for s in range(6):
        src = T["q_norm_g"][l] if s < 4 else T["k_norm_g"][l]
        P.dma("sync", lambda e, s=s, src=src: e.dma_start(out=gq[:, s * 64:(s + 1) * 64], in_=src.partition_broadcast(128)),
              "c4" if s < 4 else "c5", writes=[("gq", s)])
    P.op("vector", lambda e: e.tensor_scalar(out=gq[:, 0:256], in0=gq[:, 0:256], scalar1=0.125, scalar2=None, op0=ALU.mult),
         reads=[("gq", s) for s in range(4)], writes=["gqs"])
    gqk = ["gqs", ("gq", 4), ("gq", 5)]
    P.op("gpsimd", lambda e: e.memset(Vaug[:, :, :, 64:66], 1.0), writes=["Vones"])

    hTr = cx.ring(2, [128, 8, 512], BF16, "hTb")
    csr = cx.ring(2, [128, 2, 384], F32, "cs")
    pqr = cx.ring(2, [128, 512], F32, "pq", psum=True)
    pcr = cx.ring(2, [128, 512], F32, "pc", psum=True)
    pTr = cx.ring(2, [128, 3, 128], BF16, "pT3", psum=True)
    zbr = cx.ring(2, [128, 512], F32, "zb")
    sqr = cx.ring(2, [128, 384], F32, "sq")
    ssr = cx.ring(2, [128, 6], F32, "ss")
    xnr = cx.ring(2, [128, 384], F32, "xn")
    t1r = cx.ring(2, [128, 384], F32, "t1")
    t2r = cx.ring(2, [128, 384], F32, "t2")
    qbr = cx.ring(2, [128, 384], BF16, "qb")
    ztr = cx.ring(3, [128, 512], F32, "zt")
    hT_v = T["hT_d"].rearrange("(c p) s -> p c s", p=128)
    zc_v = T["zc_d"]
    evs = []
    for blk in range(NB if A_STAGE >= 1 else 0):
        hT, hTk = hTr.next()
        P.dma("sync", lambda e, hT=hT, blk=blk: e.dma_start(out=hT[:], in_=hT_v[:, :, blk * 512:(blk + 1) * 512]), hTk, writes=[hTk])
        for tt in range(4 if A_STAGE >= 2 else 0):
            t = blk * 4 + tt
            cs, csk = csr.next()
            P.dma("sync", lambda e, cs=cs, t=t: e.dma_start(out=cs[:, 0, :], in_=T["cos6"][t * 128:(t + 1) * 128, :]), csk + "a", writes=[csk + "a"])
            P.dma("sync", lambda e, cs=cs, t=t: e.dma_start(out=cs[:, 1, :], in_=T["sin6"][t * 128:(t + 1) * 128, :]), csk + "b", writes=[csk + "b"])
            pq, pqk = pqr.next()
            for kc in range(8):
                P.op("tensor", lambda e, pq=pq, hT=hT, kc=kc, tt=tt: e.matmul(pq[:], lhsT=hT[:, kc, tt * 128:(tt + 1) * 128], rhs=w1[:, kc, 0:512],
                                                                       start=(kc == 0), stop=(kc == 7)),
                     reads=[hTk] + w1k, writes=[pqk])
            zb, zbk = zbr.next()
            P.op("vector", lambda e, zb=zb, pq=pq: e.tensor_tensor(out=zb[:], in0=pq[:], in1=bq[:], op=ALU.add),
                 reads=[pqk] + bqk, writes=[zbk])
            if A_STAGE < 2.15:
                continue
            sq, sqk = sqr.next()
            P.op("gpsimd", lambda e, sq=sq, zb=zb: e.tensor_tensor(out=sq[:], in0=zb[:, 0:384], in1=zb[:, 0:384], op=ALU.mult),
                 reads=[zbk], writes=[sqk])
            ss, ssk = ssr.next()
            P.op("vector", lambda e, ss=ss, sq=sq: e.tensor_reduce(out=ss[:], in_=sq[:].rearrange("p (h d) -> p h d", d=64), axis=AX.X, op=ALU.add),
                 reads=[sqk], writes=[ssk])
            P.op("vector", lambda e, ss=ss: e.tensor_scalar(out=ss[:], in0=ss[:], scalar1=1.0 / 64, scalar2=1e-6, op0=ALU.mult, op1=ALU.add),
                 reads=[ssk], writes=[ssk])
            P.op("scalar", lambda e, ss=ss: e.activation(out=ss[:], in_=ss[:], func=AF.Sqrt), reads=[ssk], writes=[ssk])
            P.op("vector", lambda e, ss=ss: e.reciprocal(out=ss[:], in_=ss[:]), reads=[ssk], writes=[ssk])
            if A_STAGE < 2.25:
                continue
            xn, xnk = xnr.next()
            P.op("vector", lambda e, xn=xn, zb=zb, ss=ss: e.tensor_tensor(
                out=xn[:].rearrange("p (h d) -> p h d", d=64), in0=zb[:, 0:384].rearrange("p (h d) -> p h d", d=64),
                in1=ss[:, :].unsqueeze(2).to_broadcast([128, 6, 64]), op=ALU.mult), reads=[zbk, ssk], writes=[xnk])
            if A_STAGE < 2.35:
                continue
            P.op("gpsimd", lambda e, xn=xn: e.tensor_tensor(out=xn[:], in0=xn[:], in1=gq[:], op=ALU.mult), reads=[xnk] + gqk, writes=[xnk])
            if A_STAGE < 2.45:
                continue
            t1, t1k = t1r.next()
            P.op("vector", lambda e, t1=t1, xn=xn, cs=cs: e.tensor_tensor(out=t1[:], in0=xn[:], in1=cs[:, 0, :], op=ALU.mult),
                 reads=[xnk, csk + "a"], writes=[t1k])
            t2, t2k = t2r.next()
            xv = xn[:].rearrange("p (a b c) -> p a b c", b=2, c=16)
            sv = cs[:, 1, :].rearrange("p (a b c) -> p a b c", b=2, c=16)
            tv = t2[:].rearrange("p (a b c) -> p a b c", b=2, c=16)
            P.op("gpsimd", lambda e, xv=xv, sv=sv, tv=tv: e.tensor_tensor(out=tv[:, :, 0, :], in0=xv[:, :, 1, :], in1=sv[:, :, 0, :], op=ALU.mult),
                 reads=[xnk, csk + "b"], writes=[t2k + "x"])
            P.op("gpsimd", lambda e, xv=xv, sv=sv, tv=tv: e.tensor_tensor(out=tv[:, :, 1, :], in0=xv[:, :, 0, :], in1=sv[:, :, 1, :], op=ALU.mult),
                 reads=[xnk, csk + "b"], writes=[t2k + "y"])
            qb, qbk = qbr.next()
            P.op("vector", lambda e, qb=qb, t1=t1, t2=t2: e.tensor_tensor(out=qb[:], in0=t1[:], in1=t2[:], op=ALU.add),
                 reads=[t1k, t2k + "x", t2k + "y"], writes=[qbk])
            if A_STAGE < 2.55:
                continue
            pT, pTk = pTr.next()
            for c in range(3):
                P.op("tensor", lambda e, pT=pT, qb=qb, c=c: e.transpose(out=pT[:, c, :], in_=qb[:, c * 128:(c + 1) * 128], identity=idb[:]),
                     reads=[qbk, "idb"], writes=[pTk + "_%d" % c])
            for c, (dst, dk) in enumerate(((QTa, "QTa"), (QTb, "QTb"), (KT, "KT"))):
                if c == 1:
                    P.op("vector", lambda e, pT=pT, dst=dst, c=c, t=t: e.tensor_copy(out=dst[:, t * 128:(t + 1) * 128], in_=pT[:, c, :]),
                         reads=[pTk + "_%d" % c], writes=[(dk, t)])
                else:
                    P.op("vector", lambda e, pT=pT, dst=dst, c=c, t=t: e.tensor_copy(out=dst[:, t * 128:(t + 1) * 128], in_=pT[:, c, :]),
                         reads=[pTk + "_%d" % c], writes=[(dk, t)])
            P.op("vector", lambda e, zb=zb, t=t: e.tensor_copy(out=Vaug[:, t, :, 0:64], in_=zb[:, 384:512].rearrange("p (g d) -> p g d", d=64)),
                 reads=[zbk], writes=[("V", t)])
        for cc in range(12 if A_STAGE >= 3 else 0):
            pc, pck = pcr.next()
            for kc in range(8):
                P.op("tensor", lambda e, pc=pc, hT=hT, kc=kc, cc=cc: e.matmul(pc[:], lhsT=w1[:, kc, 512 + cc * 128:512 + (cc + 1) * 128], rhs=hT[:, kc, :],
                                                                       start=(kc == 0), stop=(kc == 7)),
                     reads=[hTk] + w1k, writes=[pck])
            zt, ztk = ztr.next()
            fn = AF.Sigmoid if cc in (8, 9) else AF.Identity
            P.op("scalar", lambda e, zt=zt, pc=pc, cc=cc, fn=fn: e.activation(out=zt[:], in_=pc[:], func=fn, bias=bc[:, cc:cc + 1]),
                 reads=[pck, "bc"], writes=[ztk])
            evs.append(P.dma("sync", lambda e, zt=zt, cc=cc, blk=blk: e.dma_start(out=zc_v[cc * 128:(cc + 1) * 128, blk * 512:(blk + 1) * 512], in_=zt[:]),
                             ztk, reads=[ztk]))
    P.flush(evs)
    cx.close()


def phase_B1(nc, P, T, l, QTa, QTb, KT, Vaug):
    cx = Ctx(nc, P)
    idf, idb = load_consts(cx, P, T["ident"])
    pSr = cx.ring(2, [128, 512], F32, "pS", psum=True)
    pO = [cx.ps([128, 512], F32) for _ in range(4)]
    pTr = cx.ring(1, [128, 2, 512], BF16, "pTa", psum=True)
    PTr = cx.ring(3, [128, 512], BF16, "PT")
    atr = cx.ring(2, [128, 4, 256], BF16, "at")
    aTr = cx.ring(2, [128, 2, 512], BF16, "aT")
    rcr = cx.ring(4, [128, 1], F32, "rc")
    br_v = T["brT_d"].rearrange("(c p) s -> p c s", p=128)
    evs = []
    par = 0
    for qb in range(NB):
        at, atk = atr.next()
        for j, QT in enumerate((QTa, QTb)):
            for g in range(2):
                h = 2 * g + j
                par ^= 1
                qk = [("QTa" if j == 0 else "QTb", qb * 4 + i) for i in range(4)]

                def emitS(kc, g=g, QT=QT, qb=qb, qk=qk):
                    pS, pSk = pSr.next()
                    P.op("tensor", lambda e, pS=pS: e.matmul(pS[:], lhsT=KT[64 * g:64 * g + 64, kc * 128:(kc + 1) * 128],
                                                         rhs=QT[64 * g:64 * g + 64, qb * 512:(qb + 1) * 512], start=True, stop=True),
                         reads=qk + [("KT", kc)], writes=[pSk])
                    return pS, pSk
                cur = emitS(0)
                for kc in range(64):
                    nxt = emitS(kc + 1) if kc < 63 else None
                    pS, pSk = cur
                    PT, PTk = PTr.next()
                    P.op("scalar", lambda e, PT=PT, pS=pS: e.activation(out=PT[:], in_=pS[:], func=AF.Exp), reads=[pSk], writes=[PTk])
                    for qt in range(4):
                        P.op("tensor", lambda e, PT=PT, qt=qt, kc=kc, g=g, par=par: e.matmul(
                            pO[qt][:, par * 128:par * 128 + 65], lhsT=PT[:, qt * 128:(qt + 1) * 128], rhs=Vaug[:, kc, g, 0:65],
                            start=(kc == 0), stop=(kc == 63)),
                            reads=[PTk, ("V", kc), "Vones"], writes=[("pO", qt, par)])
                    cur = nxt
                for qt in range(4):
                    rc, rck = rcr.next()
                    P.op("vector", lambda e, rc=rc, qt=qt, par=par: e.reciprocal(out=rc[:], in_=pO[qt][:, par * 128 + 64:par * 128 + 65]),
                         reads=[("pO", qt, par)], writes=[rck])
                    P.op("vector", lambda e, rc=rc, qt=qt, par=par, at=at, h=h: e.tensor_scalar(
                        out=at[:, qt, h * 64:(h + 1) * 64], in0=pO[qt][:, par * 128:par * 128 + 64], scalar1=rc[:, 0:1], scalar2=None, op0=ALU.mult),
                        reads=[("pO", qt, par), rck], writes=[(atk, qt, h)])
        pT, pTk = pTr.next()
        for qt in range(4):
            for hf in range(2):
                P.op("tensor", lambda e, pT=pT, at=at, qt=qt, hf=hf: e.transpose(out=pT[:, hf, qt * 128:(qt + 1) * 128], in_=at[:, qt, hf * 128:(hf + 1) * 128], identity=idb[:]),
                     reads=[(atk, qt, hh) for hh in range(4)] + ["idb"], writes=[(pTk, qt, hf)])
        aT, aTk = aTr.next()
        P.op("vector", lambda e, aT=aT, pT=pT: e.tensor_copy(out=aT[:], in_=pT[:]), reads=[(pTk, qt, hf) for qt in range(4) for hf in range(2)], writes=[aTk])
        evs.append(P.dma("sync", lambda e, aT=aT, qb=qb: e.dma_start(out=br_v[:, 0:2, qb * 512:(qb + 1) * 512], in_=aT[:]), aTk, reads=[aTk]))
    P.flush(evs)
    cx.close()


def phase_B2a(nc, P, T, l):
    cx = Ctx(nc, P)
    idf, idb = load_consts(cx, P, T["ident"])
    pmisc = cx.ps([128, 512], F32)
    cst = cx.sb([38, 256], F32)
    P.dma("sync", lambda e: e.dma_start(out=cst[0:31, :], in_=T["cf_conv_w"][l]), "c3", writes=[("cst", 0)])
    P.dma("sync", lambda e: e.dma_start(out=cst[31:34, :], in_=T["sc_conv_w"][l]), "c4", writes=[("cst", 1)])
    for i, nm in enumerate(("cf_conv_b", "cf_ln_g", "cf_ln_b", "pool_scale")):
        P.dma("sync", lambda e, i=i, nm=nm: e.dma_start(out=cst[34 + i:35 + i, :], in_=T[nm][l:l + 1, :]), "c5", writes=[("cst", 2 + i)])
    chp = cx.sb([128, 2, 38], F32)
    for c in range(2):
        P.op("tensor", lambda e, c=c: e.transpose(out=pmisc[:, 64 + c * 64:64 + c * 64 + 38], in_=cst[:, c * 128:(c + 1) * 128], identity=idf[0:38, 0:38]),
             reads=[("cst", i) for i in range(6)] + ["idf"], writes=["pm1_%d" % c])
        P.op("vector", lambda e, c=c: e.tensor_copy(out=chp[:, c, :], in_=pmisc[:, 64 + c * 64:64 + c * 64 + 38]), reads=["pm1_%d" % c], writes=[("chp", c)])
    chk = [("chp", 0), ("chp", 1)]
    pwf = cx.sb([128, 2, 128], F32)
    pwbd = cx.sb([128, 2, 128], BF16)
    P.op("vector", lambda e: e.memset(pwf[:], 0.0), writes=["pwf"])
    for gi in range(4):
        c, hh = gi // 2, gi % 2
        P.dma("sync", lambda e, gi=gi, c=c, hh=hh: e.dma_start(out=pwf[hh * 64:(hh + 1) * 64, c, hh * 64:(hh + 1) * 64], in_=T["pool_w"][l][gi]),
              "c6", reads=[], writes=["pwf"])
    P.op("vector", lambda e: e.tensor_copy(out=pwbd[:], in_=pwf[:]), reads=["pwf"], writes=["pwbd"])
    invc = cx.sb([128, 3, 2, 512], F32)
    P.dma("sync", lambda e: e.dma_start(out=invc[:], in_=T["invc"].rearrange("a p c s -> p a c s")), "c7", writes=["invc"])
    onesf = cx.sb([128, 128], F32)
    P.op("vector", lambda e: e.memset(onesf[:], 1.0), writes=["onesf"])

    zchr = cx.ring(2, [128, 12, ZW], F32, "zch")
    brr = cx.ring(2, [128, 6, 512], BF16, "brT")
    cu = cx.sb([128, 2, ZW], F32)
    ag = cx.sb([128, 2, ZW], F32)
    acc = cx.sb([128, 2, 512], F32)
    ysq = cx.sb([128, 2, 512], F32)
    mean = cx.sb([128, 512], F32)
    rstd = cx.sb([128, 512], F32)
    pC = cx.sb([128, ZW], F32)
    pD = cx.sb([128, ZW], F32)
    Wt = cx.sb([128, 2, 512], F32)
    ypool = cx.sb([128, 2, 512], BF16)
    pMr = cx.ring(4, [128, 512], F32, "pM", psum=True)
    zc_v = T["zc_d"].rearrange("(c p) s -> p c s", p=128)
    br_v = T["brT_d"].rearrange("(c p) s -> p c s", p=128)
    evs = []
    for blk in range(NB):
        zch, zk = zchr.next()
        brT, bk = brr.next()
        zks = [(zk, q4) for q4 in range(4)]
        lo = max(0, blk * 512 - HALO)
        hi = min(S, blk * 512 + 512 + HALO)
        off = lo - (blk * 512 - HALO)
        for q4 in range(4):
            P.dma("sync", lambda e, zch=zch, lo=lo, hi=hi, off=off, q4=q4: e.dma_start(out=zch[:, 3 * q4:3 * q4 + 3, off:off + hi - lo], in_=zc_v[:, 3 * q4:3 * q4 + 3, lo:hi]),
                  zk + "_%d" % q4, writes=[(zk, q4)])
        if blk == 0:
            P.op("gpsimd", lambda e, zch=zch: e.memset(zch[:, :, 0:HALO], 0.0), writes=[(zk, 4)])
            zks = zks + [(zk, 4)]
        if blk == NB - 1:
            P.op("gpsimd", lambda e, zch=zch: e.memset(zch[:, :, ZW - HALO:ZW], 0.0), writes=[(zk, 4)])
            zks = zks + [(zk, 4)]
        P.op("gpsimd", lambda e, zch=zch: e.tensor_tensor(out=cu[:], in0=zch[:, 2:4, :], in1=zch[:, 4:6, :], op=ALU.mult), reads=zks, writes=["cu"])
        for c in range(2):
            P.op("vector", lambda e, c=c: e.tensor_scalar(out=acc[:, c, :], in0=cu[:, c, 15:527], scalar1=chp[:, c, 31:32], scalar2=None, op0=ALU.mult),
                 reads=["cu"] + chk, writes=[("acc", c)])
            for k in (1, 2):
                P.op("vector", lambda e, c=c, k=k: e.scalar_tensor_tensor(out=acc[:, c, :], in0=cu[:, c, 15 + k:527 + k], scalar=chp[:, c, 31 + k:32 + k],
                                                                      in1=acc[:, c, :], op0=ALU.mult, op1=ALU.add),
                     reads=["cu", ("acc", c)] + chk, writes=[("acc", c)])
            P.op("vector", lambda e, c=c, zch=zch, brT=brT: e.tensor_tensor(out=brT[:, c, :], in0=acc[:, c, :], in1=zch[:, c, HALO:HALO + 512], op=ALU.mult),
                 reads=[("acc", c)] + zks, writes=[(bk, c)])
        P.op("gpsimd", lambda e, zch=zch: e.tensor_tensor(out=ag[:], in0=zch[:, 6:8, :], in1=zch[:, 8:10, :], op=ALU.mult), reads=zks, writes=["ag"])
        for c in range(2):
            P.op("vector", lambda e, c=c: e.tensor_scalar(out=acc[:, c, :], in0=ag[:, c, 1:513], scalar1=chp[:, c, 0:1], scalar2=chp[:, c, 34:35],
                                                      op0=ALU.mult, op1=ALU.add),
                 reads=["ag"] + chk, writes=[("acc", c)])
            for k in range(1, 31):
                P.op("vector", lambda e, c=c, k=k: e.scalar_tensor_tensor(out=acc[:, c, :], in0=ag[:, c, 1 + k:513 + k], scalar=chp[:, c, k:k + 1],
                                                                      in1=acc[:, c, :], op0=ALU.mult, op1=ALU.add),
                     reads=["ag", ("acc", c)] + chk, writes=[("acc", c)])
        P.op("gpsimd", lambda e: e.tensor_tensor(out=ysq[:], in0=acc[:], in1=acc[:], op=ALU.mult), reads=[("acc", 0), ("acc", 1)], writes=["ysq"])
        pS, pSk = pMr.next()
        pQ, pQk = pMr.next()
        for c in range(2):
            P.op("tensor", lambda e, c=c, pS=pS: e.matmul(pS[:], lhsT=onesf[:], rhs=acc[:, c, :], start=(c == 0), stop=(c == 1)),
                 reads=["onesf", ("acc", c)], writes=[pSk])
        for c in range(2):
            P.op("tensor", lambda e, c=c, pQ=pQ: e.matmul(pQ[:], lhsT=onesf[:], rhs=ysq[:, c, :], start=(c == 0), stop=(c == 1)),
                 reads=["onesf", "ysq"], writes=[pQk])
        P.op("scalar", lambda e, pS=pS: e.activation(out=mean[:], in_=pS[:], func=AF.Identity, scale=1.0 / 256), reads=[pSk], writes=["mean"])
        P.op("gpsimd", lambda e: e.tensor_tensor(out=rstd[:], in0=mean[:], in1=mean[:], op=ALU.mult), reads=["mean"], writes=["rstd"])
        P.op("vector", lambda e, pQ=pQ: e.scalar_tensor_tensor(out=rstd[:], in0=pQ[:], scalar=1.0 / 256, in1=rstd[:], op0=ALU.mult, op1=ALU.subtract),
             reads=[pQk, "rstd"], writes=["rstd"])
        P.op("vector", lambda e: e.tensor_scalar(out=rstd[:], in0=rstd[:], scalar1=LN_EPS, scalar2=None, op0=ALU.add), reads=["rstd"], writes=["rstd"])
        P.op("scalar", lambda e: e.activation(out=rstd[:], in_=rstd[:], func=AF.Sqrt), reads=["rstd"], writes=["rstd"])
        P.op("vector", lambda e: e.reciprocal(out=rstd[:], in_=rstd[:]), reads=["rstd"], writes=["rstd"])
        for c in range(2):
            P.op("vector", lambda e, c=c: e.tensor_tensor(out=acc[:, c, :], in0=acc[:, c, :], in1=mean[:], op=ALU.subtract),
                 reads=[("acc", c), "mean", pSk], writes=[("acc", c)])
            P.op("vector", lambda e, c=c: e.tensor_tensor(out=acc[:, c, :], in0=acc[:, c, :], in1=rstd[:], op=ALU.mult),
                 reads=[("acc", c), "rstd"], writes=[("acc", c)])
            P.op("scalar", lambda e, c=c, brT=brT: e.activation(out=brT[:, 2 + c, :], in_=acc[:, c, :], func=AF.Silu, scale=chp[:, c, 35:36], bias=chp[:, c, 36:37]),
                 reads=[("acc", c)] + chk, writes=[(bk, 2 + c)])
        P.op("gpsimd", lambda e, zch=zch: e.tensor_tensor(out=cu[:, :, 1:ZW], in0=zch[:, 10:12, 0:ZW - 1], in1=zch[:, 10:12, 1:ZW], op=ALU.add),
             reads=zks, writes=["cu"])
        P.op("gpsimd", lambda e: e.tensor_tensor(out=ag[:, :, 2:ZW - 1], in0=cu[:, :, 1:ZW - 2], in1=cu[:, :, 3:ZW], op=ALU.add),
             reads=["cu"], writes=["ag"])
        P.op("gpsimd", lambda e: e.tensor_tensor(out=pC[:, 4:ZW - 3], in0=ag[:, 1, 2:ZW - 5], in1=ag[:, 1, 6:ZW - 1], op=ALU.add),
             reads=["ag"], writes=["pC"])
        P.op("gpsimd", lambda e: e.tensor_tensor(out=pD[:, 8:ZW - 7], in0=pC[:, 4:ZW - 11], in1=pC[:, 12:ZW - 3], op=ALU.add),
             reads=["pC"], writes=["pD"])
        ty = 0 if blk == 0 else (2 if blk == NB - 1 else 1)
        srcs = [(cu, 0, 0), (ag, 0, 1), (pC, None, 0), (pD, None, 1)]
        for gi, (src, cidx, hh) in enumerate(srcs):
            c = gi // 2
            sl = slice(hh * 64, (hh + 1) * 64)
            sap = src[sl, cidx, HALO:HALO + 512] if cidx is not None else src[sl, HALO:HALO + 512]
            P.op("vector", lambda e, sap=sap, sl=sl, c=c, ty=ty: e.tensor_tensor(out=Wt[sl, c, :], in0=sap, in1=invc[sl, ty, c, :], op=ALU.mult),
                 reads=["cu", "ag", "pC", "pD", "invc"], writes=[("Wt", gi)])
        P.op("vector", lambda e, zch=zch: e.tensor_tensor(out=ypool[:], in0=Wt[:], in1=zch[:, 10:12, HALO:HALO + 512], op=ALU.subtract),
             reads=[("Wt", gi) for gi in range(4)] + zks, writes=["ypool"])
        for c in range(2):
            pP, pPk = pMr.next()
            P.op("tensor", lambda e, c=c, pP=pP: e.matmul(pP[:], lhsT=pwbd[:, c, :], rhs=ypool[:, c, :], start=True, stop=True),
                 reads=["pwbd", "ypool"], writes=[pPk])
            P.op("scalar", lambda e, c=c, pP=pP, brT=brT: e.activation(out=brT[:, 4 + c, :], in_=pP[:], func=AF.Identity, scale=chp[:, c, 37:38]),
                 reads=[pPk] + chk, writes=[(bk, 4 + c)])
        evs.append(P.dma("sync", lambda e, brT=brT, blk=blk: e.dma_start(out=br_v[:, 2:8, blk * 512:(blk + 1) * 512], in_=brT[:]), bk,
                         reads=[(bk, i) for i in range(6)]))
    P.flush(evs)
    cx.close()


def phase_B2b(nc, P, T, l, Dall, Wall):
    cx = Ctx(nc, P)
    idf, idb = load_consts(cx, P, T["ident"])
    wv = T["w_in"][l].rearrange("(c p) n -> p c n", p=128)
    wg = cx.sb([128, 8, 4096], BF16)
    for i in range(2):
        P.dma("gpsimd", lambda e, i=i: e.dma_start(out=wg[:, :, i * 2048:(i + 1) * 2048], in_=wv[:, :, 2048 + i * 2048:2048 + (i + 1) * 2048]),
              "wg%d" % i, writes=[("wg", i)])
    wgk = [("wg", 0), ("wg", 1)]
    wbr = cx.sb([128, 8, 1024], BF16)
    P.dma("gpsimd", lambda e: e.dma_start(out=wbr[:], in_=T["w_branch"][l].rearrange("g (c p) n -> p (g c) n", p=128)), "wbr", writes=["wbr"])
    wo = cx.sb([128, 8, 1024], BF16)
    P.dma("gpsimd", lambda e: e.dma_start(out=wo[:], in_=T["w_out"][l].rearrange("(c p) n -> p c n", p=128)), "wo", writes=["wo"])
    wr = cx.sb([128, 8, 32], BF16)
    P.dma("gpsimd", lambda e: e.dma_start(out=wr[:], in_=T["w_router"][l].rearrange("(c p) n -> p c n", p=128)), "wr", writes=["wr"])
    brb = cx.sb([128, 32], F32)
    P.dma("sync", lambda e: e.dma_start(out=brb[:], in_=T["b_router"][l].partition_broadcast(128)), "c3", writes=["brb"])
    gst = cx.sb([32, 128], F32)
    bg = cx.sb([128, 32], F32)
    P.dma("sync", lambda e: e.dma_start(out=gst[:], in_=T["b_in"][l][2048:6144].rearrange("(c p) -> c p", p=128)), "c4", writes=["gst"])
    pmisc = cx.ps([128, 512], F32)
    P.op("tensor", lambda e: e.transpose(out=pmisc[:, 0:32], in_=gst[:], identity=idf[0:32, 0:32]), reads=["gst", "idf"], writes=["pm0"])
    P.op("vector", lambda e: e.tensor_copy(out=bg[:], in_=pmisc[:, 0:32]), reads=["pm0"], writes=["bg"])
    onesb = cx.sb([128, 128], BF16)
    P.op("vector", lambda e: e.memset(onesb[:], 1.0), writes=["onesb"])
    Uf = cx.sb([128, 128], F32)
    Ub = cx.sb([128, 128], BF16)
    P.dma("sync", lambda e: e.dma_start(out=Uf[:], in_=T["utri"][:, :]), "c8", writes=["Uf"])
    P.op("vector", lambda e: e.tensor_copy(out=Ub[:], in_=Uf[:]), reads=["Uf"], writes=["Ub"])
    iotaC = cx.sb([128, 32], F32)
    P.dma("sync", lambda e: e.dma_start(out=iotaC[:], in_=T["iotac"][:, :]), "c9", writes=["iotaC"])
    cnt = cx.sb([128, 32], F32)
    P.op("vector", lambda e: e.memset(cnt[:], 0.0), writes=["cnt"])
    gt, bt = load_ln_params(P, cx, T["ln1_g"][l], T["ln1_b"][l])

    hTr = cx.ring(2, [128, 8, 512], BF16, "hTb")
    brr = cx.ring(2, [128, 8, 512], BF16, "brT")
    gtr = cx.ring(2, [128, 512], BF16, "gate")
    tmr = cx.ring(2, [128, 512], F32, "tmp")
    acr = cx.ring(2, [128, 512], F32, "macc")
    mT = cx.sb([128, 8, 512], BF16)
    pGr = cx.ring(2, [128, 512], F32, "pG", psum=True)
    pJr = cx.ring(2, [128, 512], F32, "pJ", psum=True)
    pMr = cx.ring(2, [128, 512], F32, "pM", psum=True)
    pTr = cx.ring(1, [128, 8, 128], BF16, "pT8", psum=True)
    htr = cx.ring(2, [128, 1024], F32, "ht")
    ofr = cx.ring(2, [128, 1024], F32, "of")
    obr = cx.ring(2, [128, 1024], BF16, "ob")
    h1Tr = cx.ring(2, [128, 8, 128], BF16, "h1T")
    smr = ln_scratch(cx, 1)
    lgr = cx.ring(2, [128, 32], F32, "lg")
    t8r = cx.ring(2, [128, 8], F32, "t8")
    mkr = cx.ring(2, [128, 32], BF16, "mk")
    dfr = cx.ring(2, [128, 32], F32, "df")
    slr = cx.ring(2, [128, 32], F32, "sl")
    s1r = cx.ring(2, [128, 4], F32, "s1")
    dkr = cx.ring(2, [128, 4], F32, "dk")
    hT_v = T["hT_d"].rearrange("(c p) s -> p c s", p=128)
    br_v = T["brT_d"].rearrange("(c p) s -> p c s", p=128)
    Xe = T["Xe_d"]
    evs = []
    for blk in range(NB):
        hT, hTk = hTr.next()
        P.dma("sync", lambda e, hT=hT, blk=blk: e.dma_start(out=hT[:], in_=hT_v[:, :, blk * 512:(blk + 1) * 512]), hTk, writes=[hTk])
        brT, bk = brr.next()
        P.dma("sync", lambda e, brT=brT, blk=blk: e.dma_start(out=brT[:], in_=br_v[:, :, blk * 512:(blk + 1) * 512]), bk, writes=[bk])
        for oc in range(8):
            ma, mak = acr.next()
            for g in range(4):
                pG, pGk = pGr.next()
                for kc in range(8):
                    P.op("tensor", lambda e, pG=pG, kc=kc, g=g, oc=oc, hT=hT: e.matmul(
                        pG[:], lhsT=wg[:, kc, g * 1024 + oc * 128:g * 1024 + (oc + 1) * 128], rhs=hT[:, kc, :], start=(kc == 0), stop=(kc == 7)),
                        reads=[hTk] + wgk, writes=[pGk])
                gate, gtk = gtr.next()
                P.op("scalar", lambda e, gate=gate, pG=pG, g=g, oc=oc: e.activation(out=gate[:], in_=pG[:], func=AF.Sigmoid, bias=bg[:, g * 8 + oc:g * 8 + oc + 1]),
                     reads=[pGk, "bg"], writes=[gtk])
                pJ, pJk = pJr.next()
                for c in range(2):
                    P.op("tensor", lambda e, pJ=pJ, c=c, g=g, oc=oc, brT=brT: e.matmul(
                        pJ[:], lhsT=wbr[:, g * 2 + c, oc * 128:(oc + 1) * 128], rhs=brT[:, g * 2 + c, :], start=(c == 0), stop=(c == 1)),
                        reads=["wbr", bk], writes=[pJk])
                if g == 0:
                    P.op("vector", lambda e, ma=ma, gate=gate, pJ=pJ: e.tensor_tensor(out=ma[:], in0=gate[:], in1=pJ[:], op=ALU.mult),
                         reads=[gtk, pJk], writes=[mak])
                else:
                    tm, tmk = tmr.next()
                    P.op("vector", lambda e, tm=tm, gate=gate, pJ=pJ: e.tensor_tensor(out=tm[:], in0=gate[:], in1=pJ[:], op=ALU.mult),
                         reads=[gtk, pJk], writes=[tmk])
                    if g < 3:
                        P.op("gpsimd", lambda e, ma=ma, tm=tm: e.tensor_tensor(out=ma[:], in0=ma[:], in1=tm[:], op=ALU.add), reads=[mak, tmk], writes=[mak])
                    else:
                        P.op("gpsimd", lambda e, ma=ma, tm=tm, oc=oc: e.tensor_tensor(out=mT[:, oc, :], in0=ma[:], in1=tm[:], op=ALU.add),
                             reads=[mak, tmk], writes=[("mT", oc)])
        for tt in range(4):
            t = blk * 4 + tt
            ht, htk = htr.next()
            P.dma("sync", lambda e, ht=ht, t=t: e.dma_start(out=ht[:], in_=T["h_d"][t * 128:(t + 1) * 128, :]), htk, writes=[htk])
            for hf in range(2):
                pM, pMk = pMr.next()
                for kc in range(8):
                    P.op("tensor", lambda e, pM=pM, kc=kc, tt=tt, hf=hf: e.matmul(
                        pM[:], lhsT=mT[:, kc, tt * 128:(tt + 1) * 128], rhs=wo[:, kc, hf * 512:(hf + 1) * 512], start=(kc == 0), stop=(kc == 7)),
                        reads=[("mT", kc), "wo"], writes=[pMk])
                P.op("vector", lambda e, ht=ht, pM=pM, hf=hf: e.scalar_tensor_tensor(
                    out=ht[:, hf * 512:(hf + 1) * 512], in0=ht[:, hf * 512:(hf + 1) * 512], scalar=DN_ALPHA, in1=pM[:], op0=ALU.mult, op1=ALU.add),
                    reads=[htk, pMk], writes=[htk])
            of, ofk = ofr.next()
            ob, obk = obr.next()
            ln_core(P, cx, ht, htk, gt, bt, of, ofk, ob, obk, smr.next())
            evs.append(P.dma("sync", lambda e, of=of, t=t: e.dma_start(out=T["h1_d"][t * 128:(t + 1) * 128, :], in_=of[:]), ofk, reads=[ofk]))
            pT, pTk = pTr.next()
            for c in range(8):
                P.op("tensor", lambda e, c=c, pT=pT, ob=ob: e.transpose(out=pT[:, c, :], in_=ob[:, c * 128:(c + 1) * 128], identity=idb[:]),
                     reads=[obk, "idb"], writes=[pTk + "_%d" % c])
            h1T, h1Tk = h1Tr.next()
            P.op("vector", lambda e, h1T=h1T, pT=pT: e.tensor_copy(out=h1T[:], in_=pT[:]), reads=[pTk + "_%d" % c for c in range(8)], writes=[h1Tk])
            for kc in range(8):
                P.op("tensor", lambda e, kc=kc, h1T=h1T: e.matmul(pmisc[:, 0:32], lhsT=h1T[:, kc, :], rhs=wr[:, kc, :], start=(kc == 0), stop=(kc == 7)),
                     reads=[h1Tk, "wr"], writes=["pm0"])
            lg, lgk = lgr.next()
            P.op("vector", lambda e, lg=lg: e.tensor_tensor(out=lg[:], in0=pmisc[:, 0:32], in1=brb[:], op=ALU.add), reads=["pm0", "brb"], writes=[lgk])
            t8, t8k = t8r.next()
            P.op("vector", lambda e, t8=t8, lg=lg: e.max(out=t8[:], in_=lg[:]), reads=[lgk], writes=[t8k])
            mk, mkk = mkr.next()
            P.op("vector", lambda e, mk=mk, lg=lg, t8=t8: e.tensor_scalar(out=mk[:], in0=lg[:], scalar1=t8[:, 3:4], scalar2=None, op0=ALU.is_ge),
                 reads=[lgk, t8k], writes=[mkk])
            s1, s1k = s1r.next()
            P.op("vector", lambda e, s1=s1, t8=t8: e.tensor_scalar(out=s1[:, 0:1], in0=t8[:, 0:1], scalar1=-1.0, scalar2=None, op0=ALU.mult),
                 reads=[t8k], writes=[s1k + "n"])
            P.op("scalar", lambda e, s1=s1, t8=t8, t=t: e.activation(out=Wall[:, t, :], in_=t8[:, 0:4], func=AF.Exp, bias=s1[:, 0:1], accum_out=s1[:, 1:2]),
                 reads=[t8k, s1k + "n"], writes=[("Wall", t), s1k + "s"])
            P.op("vector", lambda e, s1=s1: e.reciprocal(out=s1[:, 2:3], in_=s1[:, 1:2]), reads=[s1k + "s"], writes=[s1k + "r"])
            P.op("vector", lambda e, s1=s1, t=t: e.tensor_scalar(out=Wall[:, t, :], in0=Wall[:, t, :], scalar1=s1[:, 2:3], scalar2=None, op0=ALU.mult),
                 reads=[("Wall", t), s1k + "r"], writes=[("Wall", t)])
            P.op("tensor", lambda e, mk=mk: e.matmul(pmisc[:, 64:96], lhsT=Ub[:], rhs=mk[:], start=True, stop=True), reads=["Ub", mkk], writes=["pm1_0"])
            P.op("tensor", lambda e, mk=mk: e.matmul(pmisc[:, 128:160], lhsT=onesb[:], rhs=mk[:], start=True, stop=True), reads=["onesb", mkk], writes=["pm1_1"])
            df, dfk = dfr.next()
            P.op("vector", lambda e, df=df: e.tensor_tensor(out=df[:], in0=pmisc[:, 64:96], in1=cnt[:], op=ALU.add), reads=["pm1_0", "cnt"], writes=[dfk])
            P.op("vector", lambda e: e.tensor_tensor(out=cnt[:], in0=pmisc[:, 128:160], in1=cnt[:], op=ALU.add), reads=["pm1_1", "cnt"], writes=["cnt"])
            sl, slk = slr.next()
            P.op("vector", lambda e, sl=sl, df=df: e.tensor_scalar(out=sl[:], in0=df[:], scalar1=float(CAP), scalar2=1.0e6, op0=ALU.is_ge, op1=ALU.mult),
                 reads=[dfk], writes=[slk])
            P.op("vector", lambda e, sl=sl, df=df: e.tensor_tensor(out=df[:], in0=df[:], in1=sl[:], op=ALU.add), reads=[dfk, slk], writes=[dfk])
            P.op("vector", lambda e, df=df: e.tensor_tensor(out=df[:], in0=df[:], in1=iotaC[:], op=ALU.add), reads=[dfk, "iotaC"], writes=[dfk])
            dk, dkk = dkr.next()
            for k in range(4):
                P.op("vector", lambda e, sl=sl, lg=lg, t8=t8, k=k: e.tensor_scalar(out=sl[:], in0=lg[:], scalar1=t8[:, k:k + 1], scalar2=None, op0=ALU.is_equal),
                     reads=[lgk, t8k, dfk], writes=[slk])
                P.op("vector", lambda e, sl=sl, df=df: e.tensor_tensor(out=sl[:], in0=sl[:], in1=df[:], op=ALU.mult), reads=[slk, dfk], writes=[slk])
                P.op("vector", lambda e, sl=sl, dk=dk, k=k: e.tensor_reduce(out=dk[:, k:k + 1], in_=sl[:], axis=AX.X, op=ALU.add), reads=[slk], writes=[(dkk, k)])
            P.op("vector", lambda e, dk=dk, t=t: e.tensor_copy(out=Dall[:, t, :], in_=dk[:]), reads=[(dkk, k) for k in range(4)], writes=[("Dall", t)])
            for k in range(4):
                evs.append(P.dma("gpsimd", lambda e, ob=ob, t=t, k=k: e.indirect_dma_start(
                    out=Xe[:, :], out_offset=bass.IndirectOffsetOnAxis(ap=Dall[:, t, k:k + 1], axis=0), in_=ob[:], in_offset=None,
                    bounds_check=bc_reg(e, P), oob_is_err=False), obk + "sc", reads=[obk, ("Dall", t)]))
    P.flush(evs)
    cx.close()


def phase_C(nc, P, T, l):
    cx = Ctx(nc, P)
    idf, idb = load_consts(cx, P, T["ident"])
    gst = cx.sb([32, 2048], F32)
    bgu = cx.sb([128, 16, 32], F32)
    P.dma("sync", lambda e: e.dma_start(out=gst[:], in_=T["b_gate_up"][l]), "c1", writes=["gst"])
    pmisc = cx.ps([128, 16, 32], F32)
    for c in range(16):
        P.op("tensor", lambda e, c=c: e.transpose(out=pmisc[:, c, :], in_=gst[:, c * 128:(c + 1) * 128], identity=idf[0:32, 0:32]),
             reads=["gst", "idf"], writes=["pm%d" % c])
    P.op("vector", lambda e: e.tensor_copy(out=bgu[:], in_=pmisc[:]), reads=["pm%d" % c for c in range(16)], writes=["bgu"])
    wgr = cx.ring(2, [128, 8, 2048], BF16, "wgu")
    wdr = cx.ring(2, [128, 8, 1024], BF16, "wdn")
    bdr = cx.ring(2, [128, 1024], F32, "bd")
    xr = cx.ring(3, [128, 1024], BF16, "xs")
    XTr = cx.ring(2, [128, 8, 512], BF16, "XT")
    aTr = cx.ring(2, [128, 8, 512], BF16, "actT")
    pTr = cx.ring(2, [128, 8, 128], BF16, "pT8", psum=True)
    pGr = cx.ring(2, [128, 512], F32, "pG", psum=True)
    pLr = cx.ring(2, [128, 512], F32, "pL", psum=True)
    pYr = cx.ring(1, [128, 512], F32, "pY", psum=True)
    ggr = cx.ring(2, [128, 512], F32, "gg")
    sgr = cx.ring(2, [128, 512], F32, "sg")
    llr = cx.ring(2, [128, 512], F32, "ll")
    yr = cx.ring(2, [128, 1024], F32, "y")
    Xe, Ye = T["Xe_d"], T["Ye_d"]
    evs = []

    def load_w(e_i):
        wgu, wguk = wgr.next()
        wdn, wdnk = wdr.next()
        bd, bdk = bdr.next()
        src = T["w_gate_up"][l][e_i].rearrange("(c p) n -> p c n", p=128)
        for hf in range(2):
            P.dma("gpsimd", lambda e, wgu=wgu, src=src, hf=hf: e.dma_start(out=wgu[:, 4 * hf:4 * hf + 4, :], in_=src[:, 4 * hf:4 * hf + 4, :]),
                  wguk + "_%d" % hf, writes=[(wguk, hf)])
        P.dma("gpsimd", lambda e, wdn=wdn, e_i=e_i: e.dma_start(out=wdn[:], in_=T["w_down"][l][e_i].rearrange("(c p) n -> p c n", p=128)), wdnk, writes=[wdnk])
        P.dma("sync", lambda e, bd=bd, e_i=e_i: e.dma_start(out=bd[:], in_=T["b_down"][l][e_i].partition_broadcast(128)), bdk, writes=[bdk])
        return (wgu, wguk, wdn, wdnk, bd, bdk)

    nxt = load_w(0)
    for ex in range(NE):
        wgu, wguk, wdn, wdnk, bd, bdk = nxt
        if ex + 1 < NE:
            nxt = load_w(ex + 1)
        wk = [(wguk, 0), (wguk, 1)]
        for sb in range(CAP // 512):
            base = ex * CAP + sb * 512
            XT, XTk = XTr.next()
            for st in range(4):
                xs, xsk = xr.next()
                P.dma("sync", lambda e, xs=xs, base=base, st=st: e.dma_start(out=xs[:], in_=Xe[base + st * 128:base + (st + 1) * 128, :]), xsk, writes=[xsk])
                pT, pTk = pTr.next()
                for c in range(8):
                    P.op("tensor", lambda e, c=c, pT=pT, xs=xs: e.transpose(out=pT[:, c, :], in_=xs[:, c * 128:(c + 1) * 128], identity=idb[:]),
                         reads=[xsk, "idb"], writes=[pTk + "_%d" % c])
                P.op("vector", lambda e, XT=XT, pT=pT, st=st: e.tensor_copy(out=XT[:, :, st * 128:(st + 1) * 128], in_=pT[:]),
                     reads=[pTk + "_%d" % c for c in range(8)], writes=[(XTk, st)])
            XTks = [(XTk, st) for st in range(4)]
            aT, aTk = aTr.next()
            for fc in range(8):
                pG, pGk = pGr.next()
                pL, pLk = pLr.next()
                for kc in range(8):
                    P.op("tensor", lambda e, pG=pG, kc=kc, fc=fc, XT=XT, wgu=wgu: e.matmul(pG[:], lhsT=wgu[:, kc, fc * 128:(fc + 1) * 128], rhs=XT[:, kc, :],
                                                                                   start=(kc == 0), stop=(kc == 7)),
                         reads=XTks + wk, writes=[pGk])
                for kc in range(8):
                    P.op("tensor", lambda e, pL=pL, kc=kc, fc=fc, XT=XT, wgu=wgu: e.matmul(pL[:], lhsT=wgu[:, kc, 1024 + fc * 128:1024 + (fc + 1) * 128], rhs=XT[:, kc, :],
                                                                                   start=(kc == 0), stop=(kc == 7)),
                         reads=XTks + wk, writes=[pLk])
                gg, ggk = ggr.next()
                P.op("vector", lambda e, gg=gg, pG=pG, fc=fc, ex=ex: e.tensor_scalar(out=gg[:], in0=pG[:], scalar1=bgu[:, fc, ex:ex + 1], scalar2=7.0, op0=ALU.add, op1=ALU.min),
                     reads=[pGk, "bgu"], writes=[ggk])
                sg, sgk = sgr.next()
                P.op("scalar", lambda e, sg=sg, gg=gg: e.activation(out=sg[:], in_=gg[:], func=AF.Sigmoid, scale=1.702), reads=[ggk], writes=[sgk])
                ll, llk = llr.next()
                P.op("vector", lambda e, ll=ll, pL=pL, fc=fc, ex=ex: e.tensor_scalar(out=ll[:], in0=pL[:], scalar1=bgu[:, 8 + fc, ex:ex + 1], scalar2=-7.0, op0=ALU.add, op1=ALU.max),
                     reads=[pLk, "bgu"], writes=[llk])
                P.op("gpsimd", lambda e, ll=ll: e.tensor_scalar(out=ll[:], in0=ll[:], scalar1=7.0, scalar2=1.0, op0=ALU.min, op1=ALU.add), reads=[llk], writes=[llk])
                P.op("gpsimd", lambda e, gg=gg, sg=sg: e.tensor_tensor(out=gg[:], in0=gg[:], in1=sg[:], op=ALU.mult), reads=[ggk, sgk], writes=[ggk])
                P.op("vector", lambda e, aT=aT, gg=gg, ll=ll, fc=fc: e.tensor_tensor(out=aT[:, fc, :], in0=gg[:], in1=ll[:], op=ALU.mult), reads=[ggk, llk], writes=[(aTk, fc)])
            aTks = [(aTk, fc) for fc in range(8)]
            for st in range(4):
                y, yk = yr.next()
                for hf in range(2):
                    pY, pYk = pYr.next()
                    for fc in range(8):
                        P.op("tensor", lambda e, pY=pY, fc=fc, st=st, hf=hf, aT=aT, wdn=wdn: e.matmul(
                            pY[:], lhsT=aT[:, fc, st * 128:(st + 1) * 128], rhs=wdn[:, fc, hf * 512:(hf + 1) * 512], start=(fc == 0), stop=(fc == 7)),
                            reads=aTks + [wdnk], writes=[pYk])
                    P.op("vector", lambda e, y=y, pY=pY, hf=hf, bd=bd: e.tensor_tensor(out=y[:, hf * 512:(hf + 1) * 512], in0=pY[:], in1=bd[:, hf * 512:(hf + 1) * 512], op=ALU.add),
                         reads=[pYk, bdk], writes=[(yk, hf)])
                evs.append(P.dma("sync", lambda e, y=y, base=base, st=st: e.dma_start(out=Ye[base + st * 128:base + (st + 1) * 128, :], in_=y[:]), yk,
                                 reads=[(yk, 0), (yk, 1)]))
    P.flush(evs)
    cx.close()


def phase_D(nc, P, T, l, Dall, Wall, last):
    cx = Ctx(nc, P)
    idf, idb = load_consts(cx, P, T["ident"])
    gt, bt = load_ln_params(P, cx, T["ln2_g"][l], T["ln2_b"][l])
    ykr = [cx.ring(2, [128, 1024], F32, "yk%d_" % k) for k in range(4)]
    htr = cx.ring(2, [128, 1024], F32, "ht")
    rr = cx.ring(2, [128, 1024], F32, "r")
    ofr = cx.ring(2, [128, 1024], F32, "of")
    obr = cx.ring(2, [128, 1024], BF16, "ob")
    pTr = cx.ring(2, [128, 8, 128], BF16, "pT", psum=True)
    hTr = cx.ring(2, [128, 8, 128], BF16, "hT")
    smr = ln_scratch(cx)
    Ye = T["Ye_d"]
    evs = []
    for t in range(NT):
        ys = []
        for k in range(4):
            yk_, ykk = ykr[k].next()
            P.op("gpsimd", lambda e, yk_=yk_: e.memset(yk_[:], 0.0), writes=[ykk])
            P.dma("gpsimd", lambda e, yk_=yk_, t=t, k=k: e.indirect_dma_start(
                out=yk_[:], out_offset=None, in_=Ye[:, :], in_offset=bass.IndirectOffsetOnAxis(ap=Dall[:, t, k:k + 1], axis=0),
                bounds_check=bc_reg(e, P), oob_is_err=False), ykk, writes=[ykk])
            ys.append((yk_, ykk))
        ht, htk = htr.next()
        P.dma("sync", lambda e, ht=ht, t=t: e.dma_start(out=ht[:], in_=T["h1_d"][t * 128:(t + 1) * 128, :]), htk, writes=[htk])
        r, rk = rr.next()
        P.op("vector", lambda e, r=r, ht=ht: e.tensor_scalar(out=r[:], in0=ht[:], scalar1=DN_ALPHA, scalar2=None, op0=ALU.mult), reads=[htk], writes=[rk])
        for k in range(4):
            yk_, ykk = ys[k]
            P.op("vector", lambda e, r=r, yk_=yk_, t=t, k=k: e.scalar_tensor_tensor(out=r[:], in0=yk_[:], scalar=Wall[:, t, k:k + 1], in1=r[:], op0=ALU.mult, op1=ALU.add),
                 reads=[ykk, rk], writes=[rk])
        of, ofk = ofr.next()
        ob, obk = obr.next()
        ln_core(P, cx, r, rk, gt, bt, of, ofk, None if last else ob, obk, smr.next())
        dst = T["out"] if last else T["h_d"]
        evs.append(P.dma("sync", lambda e, of=of, t=t, dst=dst: e.dma_start(out=dst[t * 128:(t + 1) * 128, :], in_=of[:]), ofk, reads=[ofk]))
        if not last:
            pT, pTk = pTr.next()
            hTt, hTk = hTr.next()
            evs.append(transpose_store_hT(P, ob, obk, pT, pTk, hTt, hTk, idb, T["hT_d"], t, hTk))
    P.flush(evs)
    cx.close()


PARAM_SPECS = [
    ("ln_in_g", (1024,)), ("ln_in_b", (1024,)), ("w_in", (2, 1024, 6144)), ("b_in", (2, 6144)),
    ("q_norm_g", (2, 64)), ("k_norm_g", (2, 64)), ("sc_conv_w", (2, 3, 256)), ("cf_conv_w", (2, 31, 256)),
    ("cf_conv_b", (2, 256)), ("cf_ln_g", (2, 256)), ("cf_ln_b", (2, 256)), ("pool_w", (2, 4, 64, 64)),
    ("pool_scale", (2, 256)), ("w_branch", (2, 4, 256, 1024)), ("w_out", (2, 1024, 1024)),
    ("ln1_g", (2, 1024)), ("ln1_b", (2, 1024)), ("w_router", (2, 1024, 32)), ("b_router", (2, 32)),
    ("w_gate_up", (2, 32, 1024, 2048)), ("b_gate_up", (2, 32, 2048)), ("w_down", (2, 32, 1024, 1024)),
    ("b_down", (2, 32, 1024)), ("ln2_g", (2, 1024)), ("ln2_b", (2, 1024)),
]
CONST_SPECS = [("ident", (128, 128)), ("cos6", (S, 384)), ("sin6", (S, 384)), ("invc", (3, 128, 2, 512)),
               ("utri", (128, 128)), ("iotac", (128, 32))]
SCRATCH = [("h_d", (S, D), F32), ("hT_d", (D, S), BF16), ("zc_d", (1536, S), F32), ("brT_d", (1024, S), BF16),
           ("h1_d", (S, D), F32), ("Xe_d", (NSLOT, D), BF16), ("Ye_d", (NSLOT, D), F32)]


def build_program(upto=99, debug=(), small=False, astage=9):
    global A_STAGE
    A_STAGE = astage
    nc = bass.Bass("TRN2", target_bir_lowering=False)
    T = {}
    T["x"] = nc.dram_tensor("x", [S, D], F32, kind="ExternalInput").ap()
    for nm, shp in PARAM_SPECS + CONST_SPECS:
        if small and nm in ("w_gate_up", "w_down"):
            shp = (2, 1) + tuple(shp[2:])
        T[nm] = nc.dram_tensor(nm, list(shp), F32, kind="ExternalInput").ap()
    T["out"] = nc.dram_tensor("out", [S, D], F32, kind="ExternalOutput").ap()
    for nm, shp, dt in SCRATCH:
        T[nm] = nc.dram_tensor(nm, list(shp), dt, kind="ExternalOutput" if nm in debug else "Internal").ap()
    P = Prog(nc)
    with ExitStack() as top:
        Dall = top.enter_context(nc.sbuf_tensor("Dall", [128, NT, 4], I32))
        Wall = top.enter_context(nc.sbuf_tensor("Wall", [128, NT, 4], F32))
        ph = 0
        phase_ln_in(nc, P, T)
        for l in range(DEPTH):
            if ph >= upto:
                break
            with ExitStack() as att:
                QTa = att.enter_context(nc.sbuf_tensor("QTa%d" % l, [128, S], BF16))
                QTb = att.enter_context(nc.sbuf_tensor("QTb%d" % l, [128, S], BF16))
                KT = att.enter_context(nc.sbuf_tensor("KT%d" % l, [128, S], BF16))
                Vaug = att.enter_context(nc.sbuf_tensor("Vaug%d" % l, [128, NT, 2, 66], BF16))
                phase_A(nc, P, T, l, QTa, QTb, KT, Vaug)
                ph += 1
                if ph >= upto:
                    break
                phase_B1(nc, P, T, l, QTa, QTb, KT, Vaug)
                ph += 1
            if ph >= upto:
                break
            phase_B2a(nc, P, T, l)
            ph += 1
            if ph >= upto:
                break
            phase_B2b(nc, P, T, l, Dall, Wall)
            ph += 1
            if ph >= upto:
                break
            phase_C(nc, P, T, l)
            ph += 1
            if ph >= upto:
                break
            phase_D(nc, P, T, l, Dall, Wall, last=(l == DEPTH - 1))
            ph += 1
    return nc


def host_consts():
    c = {}
    c["ident"] = np.eye(128, dtype=np.float32)
    rows = S // 64
    row = np.repeat(np.arange(rows, dtype=np.float32), 64)
    col = np.tile(np.arange(64, dtype=np.float32), rows)
    inv = (np.float32(10000.0) ** (-np.arange(0, 32, 2, dtype=np.float32) / np.float32(32))).astype(np.float32)
    ar = (row[:, None] * inv).astype(np.float32)
    ac = (col[:, None] * inv).astype(np.float32)
    cr, sr, cc, sc = np.cos(ar), np.sin(ar), np.cos(ac), np.sin(ac)
    cos1 = np.concatenate([cr, cr, cc, cc], axis=1).astype(np.float32)
    sin1 = np.concatenate([-sr, sr, -sc, sc], axis=1).astype(np.float32)
    c["cos6"] = np.ascontiguousarray(np.tile(cos1, (1, 6)))
    c["sin6"] = np.ascontiguousarray(np.tile(sin1, (1, 6)))
    invc = np.zeros((3, 128, 2, 512), np.float32)
    wins = (2, 4, 8, 16)
    for ty, blk in enumerate((0, 1, NB - 1)):
        t = np.arange(blk * 512, blk * 512 + 512)
        for gi, w in enumerate(wins):
            lo = np.maximum(t - w // 2, 0)
            hi = np.minimum(t + (w - 1 - w // 2), S - 1)
            ic = (1.0 / (hi - lo + 1)).astype(np.float32)
            cidx, hh = gi // 2, gi % 2
            invc[ty, hh * 64:(hh + 1) * 64, cidx, :] = ic[None, :]
    c["invc"] = invc
    c["utri"] = np.triu(np.ones((128, 128), np.float32), k=1)
    c["iotac"] = np.tile((np.arange(32, dtype=np.float32) * CAP)[None, :], (128, 1)).astype(np.float32)
    return c


_CACHE = {}


def kernel(**inputs):
    if "nc" not in _CACHE:
        _CACHE["nc"] = build_program()
    nc = _CACHE["nc"]
    consts = host_consts()
    shared = {nm: np.ascontiguousarray(np.asarray(inputs[nm], dtype=np.float32)) for nm, _ in PARAM_SPECS}
    shared.update(consts)
    x = np.asarray(inputs["x"], dtype=np.float32)
    in_maps = []
    for b in range(8):
        m = dict(shared)
        m["x"] = np.ascontiguousarray(x[b])
        in_maps.append(m)
    res = run_bass_kernel_spmd(nc, in_maps, core_ids=list(range(8)))
    return np.stack([np.asarray(r["out"]) for r in res.results], axis=0).astype(np.float32)
```

```python
import math
from contextlib import ExitStack
import numpy as np
import concourse.bass as bass
import concourse.mybir as mybir
from concourse.bass_utils import run_bass_kernel_spmd

F32 = mybir.dt.float32
BF16 = mybir.dt.bfloat16
I32 = mybir.dt.int32
AF = mybir.ActivationFunctionType
ALU = mybir.AluOpType
AX = mybir.AxisListType

ENGS = ("sync", "scalar", "vector", "gpsimd", "tensor")
S = 8192
D = 1024
NT = 64
NB = 16
NE = 32
CAP = 1536
NSLOT = NE * CAP
DEPTH = 2
DN_ALPHA = (2.0 * DEPTH) ** 0.25
LN_EPS = 1e-5
HALO = 16
ZW = 512 + 2 * HALO


class _Probe:
    def __init__(self):
        self.name = None

    def __getattr__(self, nm):
        def f(*a, **k):
            self.name = nm
            return self
        return f


class Prog:
    def __init__(self, nc):
        self.nc = nc
        self.esem = {e: nc.alloc_semaphore("es_" + e) for e in ENGS}
        self.ebase = {e: 0 for e in ENGS}
        self.dsem = {}
        self.nflush = 0
        self._reset()

    def _reset(self):
        self.ops = {e: [] for e in ENGS}
        self.waited = {e: {} for e in ENGS}
        self.last_w = {}
        self.readers = {}
        self.signal = {e: set() for e in ENGS}
        self.pe_mode = None

    def _need(self, eng, ev, waits):
        if ev is None:
            return
        if ev[0] == "e" and ev[1] == eng and eng == "tensor":
            return
        key = (ev[0], ev[1])
        if self.waited[eng].get(key, -1) >= ev[2]:
            return
        waits[key] = max(waits.get(key, -1), ev[2])

    def _deps(self, eng, reads, writes, extra=()):
        waits = {}
        for k in reads:
            self._need(eng, self.last_w.get(k), waits)
        for k in writes:
            self._need(eng, self.last_w.get(k), waits)
            for ev in self.readers.get(k, ()):
                self._need(eng, ev, waits)
        for ev in extra:
            self._need(eng, ev, waits)
        out = []
        for key, v in waits.items():
            self.waited[eng][key] = v
            out.append((key[0], key[1], v))
            if key[0] == "e":
                self.signal[key[1]].add(v)
        return out

    def _commit(self, ev, reads, writes):
        for k in reads:
            self.readers.setdefault(k, []).append(ev)
        for k in writes:
            self.last_w[k] = ev
            self.readers[k] = []

    def op(self, eng, fn, reads=(), writes=(), extra=(), mode=128):
        waits = self._deps(eng, reads, writes, extra)
        idx = len(self.ops[eng])
        if eng == "tensor":
            pr = _Probe()
            fn(pr)
            mode = (pr.name, mode)
            if self.pe_mode is not None and self.pe_mode != mode and idx > 0:
                key = ("e", "tensor")
                if self.waited[eng].get(key, -1) < idx - 1:
                    self.waited[eng][key] = idx - 1
                    waits.append(("e", "tensor", idx - 1))
                    self.signal["tensor"].add(idx - 1)
            self.pe_mode = mode
        self.ops[eng].append(dict(fn=fn, waits=waits, dma=None))
        ev = ("e", eng, idx)
        self._commit(ev, reads, writes)
        return ev

    def dma(self, eng, fn, semkey, reads=(), writes=(), extra=()):
        waits = self._deps(eng, reads, writes, extra)
        sn = "ds_" + semkey
        if sn not in self.dsem:
            self.dsem[sn] = [self.nc.alloc_semaphore(sn), 0]
        st = self.dsem[sn]
        st[1] += 16
        self.ops[eng].append(dict(fn=fn, waits=waits, dma=(st[0], st[1])))
        ev = ("d", sn, st[1])
        self._commit(ev, reads, writes)
        return ev

    def wait(self, eng, evs):
        waits = self._deps(eng, (), (), evs)
        self.ops[eng].append(dict(fn=None, waits=waits, dma=None))

    def flush(self, final_events=()):
        nc = self.nc
        if final_events:
            self.wait("sync", final_events)
        val = {}
        for e in ENGS:
            v = self.ebase[e]
            m = {}
            for idx in sorted(self.signal[e]):
                v += 1
                m[idx] = v
            val[e] = m
            self.ebase[e] = v
        ops, esem, dsem = self.ops, self.esem, self.dsem

        def emit(e, name):
            for idx, o in enumerate(ops[name]):
                for kind, who, v in o["waits"]:
                    if kind == "e":
                        e.wait_ge(esem[who], val[who][v])
                    else:
                        e.wait_ge(dsem[who][0], v)
                if o["fn"] is None:
                    continue
                ins = o["fn"](e)
                if o["dma"] is not None:
                    ins.then_inc(o["dma"][0], 16)
                elif idx in val[name]:
                    ins.then_inc(esem[name], 1)

        with nc.Block() as block:
            @block.sync
            def _(e):
                emit(e, "sync")

            @block.scalar
            def _(e):
                emit(e, "scalar")

            @block.vector
            def _(e):
                emit(e, "vector")

            @block.gpsimd
            def _(e):
                emit(e, "gpsimd")

            @block.tensor
            def _(e):
                emit(e, "tensor")
        self.nflush += 1
        self._reset()


_REG = {}


def bc_reg(e, P):
    key = P.nflush
    if key not in _REG:
        _REG.clear()
        _REG[key] = e.to_reg(NSLOT - 1)
    return _REG[key]


class Ring:
    def __init__(self, items):
        self.items = items
        self.i = -1

    def next(self):
        self.i = (self.i + 1) % len(self.items)
        return self.items[self.i]


class Ctx:
    _uid = [0]

    def __init__(self, nc, P):
        self.nc, self.P = nc, P
        self.stack = ExitStack()
        self.n = 0
        Ctx._uid[0] += 1
        self.uid = Ctx._uid[0]

    def sb(self, shape, dt, name=None):
        self.n += 1
        return self.stack.enter_context(self.nc.sbuf_tensor(name or f"t{self.n}_{self.uid}", list(shape), dt))

    def ps(self, shape, dt, name=None):
        self.n += 1
        return self.stack.enter_context(self.nc.psum_tensor(name or f"p{self.n}_{self.uid}", list(shape), dt))

    def ring(self, n, shape, dt, key, psum=False):
        return Ring([((self.ps if psum else self.sb)(shape, dt), f"{key}{i}") for i in range(n)])

    def close(self):
        self.stack.close()


def load_consts(cx, P, ident_d):
    nc = cx.nc
    idf = cx.sb([128, 128], F32)
    idb = cx.sb([128, 128], BF16)
    P.dma("sync", lambda e: e.dma_start(out=idf[:], in_=ident_d[:, :]), "c0", writes=["idf"])
    P.op("vector", lambda e: e.tensor_copy(out=idb[:], in_=idf[:]), reads=["idf"], writes=["idb"])
    return idf, idb


def ln_core(P, cx, r, rk, gt, bt, of, ofk, ob, obk, sm):
    st, mv, rs, nb, xn, k = sm
    for c in range(2):
        P.op("vector", lambda e, c=c: e.bn_stats(out=st[:, c, :], in_=r[:, c * 512:(c + 1) * 512]),
             reads=[rk], writes=[k + "st%d" % c])
    P.op("vector", lambda e: e.bn_aggr(out=mv[:], in_=st[:].rearrange("p a b -> p (a b)")),
         reads=[k + "st0", k + "st1"], writes=[k + "mv"])
    P.op("vector", lambda e: e.tensor_scalar(out=rs[:], in0=mv[:, 1:2], scalar1=LN_EPS, scalar2=None, op0=ALU.add),
         reads=[k + "mv"], writes=[k + "rs"])
    P.op("scalar", lambda e: e.activation(out=rs[:], in_=rs[:], func=AF.Sqrt), reads=[k + "rs"], writes=[k + "rs"])
    P.op("vector", lambda e: e.reciprocal(out=rs[:], in_=rs[:]), reads=[k + "rs"], writes=[k + "rs"])
    P.op("vector", lambda e: e.tensor_scalar(out=nb[:], in0=mv[:, 0:1], scalar1=rs[:, 0:1], scalar2=-1.0,
                                             op0=ALU.mult, op1=ALU.mult),
         reads=[k + "mv", k + "rs"], writes=[k + "nb"])
    P.op("scalar", lambda e: e.activation(out=xn[:], in_=r[:], func=AF.Identity, scale=rs[:, 0:1], bias=nb[:, 0:1]),
         reads=[rk, k + "rs", k + "nb"], writes=[k + "xn"])
    P.op("vector", lambda e: e.tensor_tensor(out=xn[:], in0=xn[:], in1=gt[:], op=ALU.mult),
         reads=[k + "xn", "lng"], writes=[k + "xn"])
    P.op("gpsimd", lambda e: e.tensor_tensor(out=of[:], in0=xn[:], in1=bt[:], op=ALU.add),
         reads=[k + "xn", "lnb"], writes=[ofk])
    if ob is not None:
        P.op("scalar", lambda e: e.copy(out=ob[:], in_=of[:]), reads=[ofk], writes=[obk])


def ln_scratch(cx, n=2):
    return Ring([(cx.sb([128, 2, 6], F32), cx.sb([128, 2], F32), cx.sb([128, 1], F32), cx.sb([128, 1], F32),
                  cx.sb([128, 1024], F32), f"ln{i}") for i in range(n)])


def load_ln_params(P, cx, g_ap, b_ap):
    gt = cx.sb([128, 1024], F32)
    bt = cx.sb([128, 1024], F32)
    P.dma("sync", lambda e: e.dma_start(out=gt[:], in_=g_ap.partition_broadcast(128)), "c1", writes=["lng"])
    P.dma("sync", lambda e: e.dma_start(out=bt[:], in_=b_ap.partition_broadcast(128)), "c2", writes=["lnb"])
    return gt, bt


def transpose_store_hT(P, hb, hbk, pT, pTk, hTt, hTk, idb, hT_d, t, semkey):
    for c in range(8):
        P.op("tensor", lambda e, c=c: e.transpose(out=pT[:, c, :], in_=hb[:, c * 128:(c + 1) * 128], identity=idb[:]),
             reads=[hbk, "idb"], writes=[pTk + "_%d" % c])
    P.op("vector", lambda e: e.tensor_copy(out=hTt[:], in_=pT[:]), reads=[pTk + "_%d" % c for c in range(8)],
         writes=[hTk])
    return P.dma("sync", lambda e: e.dma_start(
        out=hT_d.rearrange("(c p) s -> p c s", p=128)[:, :, t * 128:(t + 1) * 128], in_=hTt[:]),
        semkey, reads=[hTk])


def phase_ln_in(nc, P, T):
    cx = Ctx(nc, P)
    idf, idb = load_consts(cx, P, T["ident"])
    gt, bt = load_ln_params(P, cx, T["ln_in_g"], T["ln_in_b"])
    xr = cx.ring(2, [128, 1024], F32, "x")
    ofr = cx.ring(2, [128, 1024], F32, "of")
    obr = cx.ring(2, [128, 1024], BF16, "ob")
    pTr = cx.ring(2, [128, 8, 128], BF16, "pT", psum=True)
    hTr = cx.ring(2, [128, 8, 128], BF16, "hT")
    smr = ln_scratch(cx)
    evs = []
    for t in range(NT):
        xt, xk = xr.next()
        P.dma("sync", lambda e, xt=xt, t=t: e.dma_start(out=xt[:], in_=T["x"][t * 128:(t + 1) * 128, :]), xk, writes=[xk])
        of, ofk = ofr.next()
        ob, obk = obr.next()
        ln_core(P, cx, xt, xk, gt, bt, of, ofk, ob, obk, smr.next())
        evs.append(P.dma("sync", lambda e, of=of, t=t: e.dma_start(out=T["h_d"][t * 128:(t + 1) * 128, :], in_=of[:]),
                         ofk, reads=[ofk]))
        pT, pTk = pTr.next()
        hTt, hTk = hTr.next()
        evs.append(transpose_store_hT(P, ob, obk, pT, pTk, hTt, hTk, idb, T["hT_d"], t, hTk))
    P.flush(evs)
    cx.close()


A_STAGE = 9
QPERM = [0, 2, 1, 3]


def phase_A(nc, P, T, l, QTa, QTb, KT, Vaug):
    cx = Ctx(nc, P)
    idf, idb = load_consts(cx, P, T["ident"])
    w_in = T["w_in"][l]
    b_in = T["b_in"][l]
    wv = w_in.rearrange("(c p) n -> p c n", p=128)
    w1 = cx.sb([128, 8, 2048], BF16)
    for s in range(4):
        hs = QPERM[s]
        P.dma("gpsimd", lambda e, s=s, hs=hs: e.dma_start(out=w1[:, :, s * 64:(s + 1) * 64], in_=wv[:, :, hs * 64:(hs + 1) * 64]),
              "w1q", writes=[("w1", s)])
    P.dma("gpsimd", lambda e: e.dma_start(out=w1[:, :, 256:2048], in_=wv[:, :, 256:2048]), "w1r", writes=[("w1", 4)])
    w1k = [("w1", i) for i in range(5)]
    bq = cx.sb([128, 512], F32)
    for s in range(4):
        hs = QPERM[s]
        P.dma("sync", lambda e, s=s, hs=hs: e.dma_start(out=bq[:, s * 64:(s + 1) * 64],
                                                        in_=b_in[hs * 64:(hs + 1) * 64].partition_broadcast(128)),
              "c1", writes=[("bq", s)])
    P.dma("sync", lambda e: e.dma_start(out=bq[:, 256:512], in_=b_in[256:512].partition_broadcast(128)), "c2",
          writes=[("bq", 4)])
    bqk = [("bq", i) for i in range(5)]
    bst = cx.sb([12, 128], F32)
    bc = cx.sb([128, 12], F32)
    P.dma("sync", lambda e: e.dma_start(out=bst[:], in_=b_in[512:2048].rearrange("(c p) -> c p", p=128)), "c3", writes=["bst"])
    pB = cx.ps([128, 512], F32)
    P.op("tensor", lambda e: e.transpose(out=pB[:, 0:12], in_=bst[:], identity=idf[0:12, 0:12]), reads=["bst", "idf"], writes=["pB"], mode=32)
    P.op("vector", lambda e: e.tensor_copy(out=bc[:], in_=pB[:, 0:12]), reads=["pB"], writes=["bc"])
    gq = cx.sb([128, 384], F32)
    for s in range(6):
        src = T["q_norm_g"][l] if s < 4 else T["k_norm_g"][l]
        P.dma("sync", lambda e, s=s, src=src: e.dma_start(out=gq[:, s * 64:(s + 1) * 64], in_=src.partition_broadcast(128)),
              "c4" if s < 4 else "c5", writes=[("gq", s)])
    P.op("vector", lambda e: e.tensor_scalar(out=gq[:, 0:256], in0=gq[:, 0:256], scalar1=0.125, scalar2=None, op0=ALU.mult),
         reads=[("gq", s) for s in range(4)], writes=["gqs"])
    gqk = ["gqs", ("gq", 4), ("gq", 5)]
    P.op("gpsimd", lambda e: e.memset(Vaug[:, :, :, 64:66], 1.0), writes=["Vones"])
    for h in range(4):
        P.op("gpsimd", lambda e, h=h: e.memset(QTa[:, h, :], 0.0), writes=["QTz"])

    hTr = cx.ring(2, [128, 8, 512], BF16, "hTb")
    csr = cx.ring(2, [128, 2, 384], F32, "cs")
    pqr = cx.ring(2, [128, 512], F32, "pq", psum=True)
    pcr = cx.ring(2, [128, 512], F32, "pc", psum=True)
    pTr = cx.ring(2, [128, 8, 128], BF16, "pT3", psum=True)
    zbr = cx.ring(2, [128, 512], F32, "zb")
    sqr = cx.ring(2, [128, 384], F32, "sq")
    ssr = cx.ring(2, [128, 6], F32, "ss")
    xnr = cx.ring(2, [128, 384], F32, "xn")
    t1r = cx.ring(2, [128, 384], F32, "t1")
    t2r = cx.ring(2, [128, 384], F32, "t2")
    qbr = cx.ring(2, [128, 384], BF16, "qb")
    ztr = cx.ring(3, [128, 512], F32, "zt")
    hT_v = T["hT_d"].rearrange("(c p) s -> p c s", p=128)
    zc_v = T["zc_d"]
    evs = []
    for blk in range(NB if A_STAGE >= 1 else 0):
        hT, hTk = hTr.next()
        P.dma("sync", lambda e, hT=hT, blk=blk: e.dma_start(out=hT[:], in_=hT_v[:, :, blk * 512:(blk + 1) * 512]), hTk, writes=[hTk])
        for tt in range(4 if A_STAGE >= 2 else 0):
            t = blk * 4 + tt
            cs, csk = csr.next()
            P.dma("sync", lambda e, cs=cs, t=t: e.dma_start(out=cs[:, 0, :], in_=T["cos6"][t * 128:(t + 1) * 128, :]), csk + "a", writes=[csk + "a"])
            P.dma("sync", lambda e, cs=cs, t=t: e.dma_start(out=cs[:, 1, :], in_=T["sin6"][t * 128:(t + 1) * 128, :]), csk + "b", writes=[csk + "b"])
            pq, pqk = pqr.next()
            for kc in range(8):
                P.op("tensor", lambda e, pq=pq, hT=hT, kc=kc, tt=tt: e.matmul(pq[:], lhsT=hT[:, kc, tt * 128:(tt + 1) * 128], rhs=w1[:, kc, 0:512],
                                                                       start=(kc == 0), stop=(kc == 7)),
                     reads=[hTk] + w1k, writes=[pqk])
            zb, zbk = zbr.next()
            P.op("vector", lambda e, zb=zb, pq=pq: e.tensor_tensor(out=zb[:], in0=pq[:], in1=bq[:], op=ALU.add),
                 reads=[pqk] + bqk, writes=[zbk])
            if A_STAGE < 2.15:
                continue
            sq, sqk = sqr.next()
            P.op("gpsimd", lambda e, sq=sq, zb=zb: e.tensor_tensor(out=sq[:], in0=zb[:, 0:384], in1=zb[:, 0:384], op=ALU.mult),
                 reads=[zbk], writes=[sqk])
            ss, ssk = ssr.next()
            P.op("vector", lambda e, ss=ss, sq=sq: e.tensor_reduce(out=ss[:], in_=sq[:].rearrange("p (h d) -> p h d", d=64), axis=AX.X, op=ALU.add),
                 reads=[sqk], writes=[ssk])
            P.op("vector", lambda e, ss=ss: e.tensor_scalar(out=ss[:], in0=ss[:], scalar1=1.0 / 64, scalar2=1e-6, op0=ALU.mult, op1=ALU.add),
                 reads=[ssk], writes=[ssk])
            P.op("scalar", lambda e, ss=ss: e.activation(out=ss[:], in_=ss[:], func=AF.Sqrt), reads=[ssk], writes=[ssk])
            P.op("vector", lambda e, ss=ss: e.reciprocal(out=ss[:], in_=ss[:]), reads=[ssk], writes=[ssk])
            if A_STAGE < 2.25:
                continue
            xn, xnk = xnr.next()
            P.op("vector", lambda e, xn=xn, zb=zb, ss=ss: e.tensor_tensor(
                out=xn[:].rearrange("p (h d) -> p h d", d=64), in0=zb[:, 0:384].rearrange("p (h d) -> p h d", d=64),
                in1=ss[:, :].unsqueeze(2).to_broadcast([128, 6, 64]), op=ALU.mult), reads=[zbk, ssk], writes=[xnk])
            if A_STAGE < 2.35:
                continue
            P.op("gpsimd", lambda e, xn=xn: e.tensor_tensor(out=xn[:], in0=xn[:], in1=gq[:], op=ALU.mult), reads=[xnk] + gqk, writes=[xnk])
            if A_STAGE < 2.45:
                continue
            t1, t1k = t1r.next()
            P.op("vector", lambda e, t1=t1, xn=xn, cs=cs: e.tensor_tensor(out=t1[:], in0=xn[:], in1=cs[:, 0, :], op=ALU.mult),
                 reads=[xnk, csk + "a"], writes=[t1k])
            t2, t2k = t2r.next()
            xv = xn[:].rearrange("p (a b c) -> p a b c", b=2, c=16)
            sv = cs[:, 1, :].rearrange("p (a b c) -> p a b c", b=2, c=16)
            tv = t2[:].rearrange("p (a b c) -> p a b c", b=2, c=16)
            P.op("gpsimd", lambda e, xv=xv, sv=sv, tv=tv: e.tensor_tensor(out=tv[:, :, 0, :], in0=xv[:, :, 1, :], in1=sv[:, :, 0, :], op=ALU.mult),
                 reads=[xnk, csk + "b"], writes=[t2k + "x"])
            P.op("gpsimd", lambda e, xv=xv, sv=sv, tv=tv: e.tensor_tensor(out=tv[:, :, 1, :], in0=xv[:, :, 0, :], in1=sv[:, :, 1, :], op=ALU.mult),
                 reads=[xnk, csk + "b"], writes=[t2k + "y"])
            qb, qbk = qbr.next()
            P.op("vector", lambda e, qb=qb, t1=t1, t2=t2: e.tensor_tensor(out=qb[:], in0=t1[:], in1=t2[:], op=ALU.add),
                 reads=[t1k, t2k + "x", t2k + "y"], writes=[qbk])
            if A_STAGE < 2.55:
                continue
            pT, pTk = pTr.next()
            for c in range(3):
                P.op("tensor", lambda e, pT=pT, qb=qb, c=c: e.transpose(out=pT[:, c, :], in_=qb[:, c * 128:(c + 1) * 128], identity=idb[:]),
                     reads=[qbk, "idb"], writes=[pTk + "_%d" % c])
            for c in range(2):
                for g in range(2):
                    h = 2 * g + c
                    P.op("vector", lambda e, pT=pT, c=c, g=g, h=h, t=t: e.tensor_copy(out=QTa[64 * g:64 * g + 64, h, t * 128:(t + 1) * 128], in_=pT[64 * g:64 * g + 64, c, :]),
                         reads=[pTk + "_%d" % c, "QTz"], writes=[("QT", h, t)])
            P.op("vector", lambda e, pT=pT, t=t: e.tensor_copy(out=KT[:, t * 128:(t + 1) * 128], in_=pT[:, 2, :]),
                 reads=[pTk + "_2"], writes=[("KT", t)])
            P.op("vector", lambda e, zb=zb, t=t: e.tensor_copy(out=Vaug[:, t, :, 0:64], in_=zb[:, 384:512].rearrange("p (g d) -> p g d", d=64)),
                 reads=[zbk], writes=[("V", t)])
        for cc in range(12 if A_STAGE >= 3 else 0):
            pc, pck = pcr.next()
            for kc in range(8):
                P.op("tensor", lambda e, pc=pc, hT=hT, kc=kc, cc=cc: e.matmul(pc[:], lhsT=w1[:, kc, 512 + cc * 128:512 + (cc + 1) * 128], rhs=hT[:, kc, :],
                                                                       start=(kc == 0), stop=(kc == 7)),
                     reads=[hTk] + w1k, writes=[pck])
            zt, ztk = ztr.next()
            fn = AF.Sigmoid if cc in (8, 9) else AF.Identity
            P.op("scalar", lambda e, zt=zt, pc=pc, cc=cc, fn=fn: e.activation(out=zt[:], in_=pc[:], func=fn, bias=bc[:, cc:cc + 1]),
                 reads=[pck, "bc"], writes=[ztk])
            evs.append(P.dma("sync", lambda e, zt=zt, cc=cc, blk=blk: e.dma_start(out=zc_v[cc * 128:(cc + 1) * 128, blk * 512:(blk + 1) * 512], in_=zt[:]),
                             ztk, reads=[ztk]))
    P.flush(evs)
    cx.close()


def phase_B1(nc, P, T, l, QTa, QTb, KT, Vaug):
    cx = Ctx(nc, P)
    idf, idb = load_consts(cx, P, T["ident"])
    pSr = cx.ring(2, [128, 512], F32, "pS", psum=True)
    pO = [cx.ps([128, 512], F32) for _ in range(4)]
    pTr = cx.ring(1, [128, 2, 512], BF16, "pTa", psum=True)
    PTr = cx.ring(3, [128, 512], BF16, "PT")
    atr = cx.ring(2, [128, 4, 256], BF16, "at")
    aTr = cx.ring(2, [128, 2, 512], BF16, "aT")
    rcr = cx.ring(4, [128, 1], F32, "rc")
    Obr = cx.ring(2, [128, 4, 66], F32, "Ob")
    br_v = T["brT_d"].rearrange("(c p) s -> p c s", p=128)
    evs = []
    par = 0
    for qb in range(NB):
        at, atk = atr.next()
        for h in range(4):
            if True:
                g = h // 2
                par ^= 1
                qk = [("QT", h, qb * 4 + i) for i in range(4)]

                def emitS(kc, h=h, qb=qb, qk=qk):
                    pS, pSk = pSr.next()
                    P.op("tensor", lambda e, pS=pS: e.matmul(pS[:], lhsT=KT[:, kc * 128:(kc + 1) * 128],
                                                         rhs=QTa[:, h, qb * 512:(qb + 1) * 512], start=True, stop=True),
                         reads=qk + [("KT", kc)], writes=[pSk])
                    return pS, pSk
                cur = emitS(0)
                for kc in range(64):
                    nxt = emitS(kc + 1) if kc < 63 else None
                    pS, pSk = cur
                    PT, PTk = PTr.next()
                    P.op("scalar", lambda e, PT=PT, pS=pS: e.activation(out=PT[:], in_=pS[:], func=AF.Exp), reads=[pSk], writes=[PTk])
                    for qt in range(4):
                        P.op("tensor", lambda e, PT=PT, qt=qt, kc=kc, g=g: e.matmul(
                            pO[qt][:, 0:65], lhsT=PT[:, qt * 128:(qt + 1) * 128], rhs=Vaug[:, kc, g, 0:65],
                            start=(kc == 0), stop=(kc == 63)),
                            reads=[PTk, ("V", kc), "Vones"], writes=[("pO", qt)])
                    cur = nxt
                Ob, Obk = Obr.next()
                for qt in range(4):
                    P.op("vector", lambda e, Ob=Ob, qt=qt: e.tensor_copy(out=Ob[:, qt, :], in_=pO[qt][:, 0:66]),
                         reads=[("pO", qt)], writes=[(Obk, qt)])
                for qt in range(4):
                    rc, rck = rcr.next()
                    P.op("vector", lambda e, rc=rc, qt=qt, Ob=Ob: e.reciprocal(out=rc[:], in_=Ob[:, qt, 64:65]),
                         reads=[(Obk, qt)], writes=[rck])
                    P.op("vector", lambda e, rc=rc, qt=qt, Ob=Ob, at=at, h=h: e.tensor_scalar(
                        out=at[:, qt, h * 64:(h + 1) * 64], in0=Ob[:, qt, 0:64], scalar1=rc[:, 0:1], scalar2=None, op0=ALU.mult),
                        reads=[(Obk, qt), rck], writes=[(atk, qt, h)])
        pT, pTk = pTr.next()
        for qt in range(4):
            for hf in range(2):
                P.op("tensor", lambda e, pT=pT, at=at, qt=qt, hf=hf: e.transpose(out=pT[:, hf, qt * 128:(qt + 1) * 128], in_=at[:, qt, hf * 128:(hf + 1) * 128], identity=idb[:]),
                     reads=[(atk, qt, hh) for hh in range(4)] + ["idb"], writes=[(pTk, qt, hf)])
        aT, aTk = aTr.next()
        P.op("vector", lambda e, aT=aT, pT=pT: e.tensor_copy(out=aT[:], in_=pT[:]), reads=[(pTk, qt, hf) for qt in range(4) for hf in range(2)], writes=[aTk])
        evs.append(P.dma("sync", lambda e, aT=aT, qb=qb: e.dma_start(out=br_v[:, 0:2, qb * 512:(qb + 1) * 512], in_=aT[:]), aTk, reads=[aTk]))
    P.flush(evs)
    cx.close()


def phase_B2a(nc, P, T, l):
    cx = Ctx(nc, P)
    idf, idb = load_consts(cx, P, T["ident"])
    pmisc = cx.ps([128, 512], F32)
    cst = cx.sb([38, 256], F32)
    P.dma("sync", lambda e: e.dma_start(out=cst[0:31, :], in_=T["cf_conv_w"][l]), "c3", writes=[("cst", 0)])
    P.dma("sync", lambda e: e.dma_start(out=cst[31:34, :], in_=T["sc_conv_w"][l]), "c4", writes=[("cst", 1)])
    for i, nm in enumerate(("cf_conv_b", "cf_ln_g", "cf_ln_b", "pool_scale")):
        P.dma("sync", lambda e, i=i, nm=nm: e.dma_start(out=cst[34 + i:35 + i, :], in_=T[nm][l:l + 1, :]), "c5", writes=[("cst", 2 + i)])
    chp = cx.sb([128, 2, 38], F32)
    for c in range(2):
        P.op("tensor", lambda e, c=c: e.transpose(out=pmisc[:, 64 + c * 64:64 + c * 64 + 38], in_=cst[:, c * 128:(c + 1) * 128], identity=idf[0:38, 0:38]),
             reads=[("cst", i) for i in range(6)] + ["idf"], writes=["pmisc"], mode=64)
        P.op("vector", lambda e, c=c: e.tensor_copy(out=chp[:, c, :], in_=pmisc[:, 64 + c * 64:64 + c * 64 + 38]), reads=["pmisc"], writes=[("chp", c)])
    chk = [("chp", 0), ("chp", 1)]
    pwf = cx.sb([128, 2, 128], F32)
    pwbd = cx.sb([128, 2, 128], BF16)
    P.op("vector", lambda e: e.memset(pwf[:], 0.0), writes=["pwf"])
    for gi in range(4):
        c, hh = gi // 2, gi % 2
        P.dma("sync", lambda e, gi=gi, c=c, hh=hh: e.dma_start(out=pwf[hh * 64:(hh + 1) * 64, c, hh * 64:(hh + 1) * 64], in_=T["pool_w"][l][gi]),
              "c6", reads=[], writes=["pwf"])
    P.op("vector", lambda e: e.tensor_copy(out=pwbd[:], in_=pwf[:]), reads=["pwf"], writes=["pwbd"])
    invc = cx.sb([128, 3, 2, 512], F32)
    P.dma("sync", lambda e: e.dma_start(out=invc[:], in_=T["invc"].rearrange("a p c s -> p a c s")), "c7", writes=["invc"])
    onesf = cx.sb([128, 128], F32)
    P.op("vector", lambda e: e.memset(onesf[:], 1.0), writes=["onesf"])

    zchr = cx.ring(2, [128, 12, ZW], F32, "zch")
    brr = cx.ring(2, [128, 6, 512], BF16, "brT")
    cu = cx.sb([128, 2, ZW], F32)
    ag = cx.sb([128, 2, ZW], F32)
    acc = cx.sb([128, 2, 512], F32)
    ysq = cx.sb([128, 2, 512], F32)
    mean = cx.sb([128, 512], F32)
    rstd = cx.sb([128, 512], F32)
    pC = cx.sb([128, ZW], F32)
    pD = cx.sb([128, ZW], F32)
    Wt = cx.sb([128, 2, 512], F32)
    ypool = cx.sb([128, 2, 512], BF16)
    pMr = cx.ring(4, [128, 512], F32, "pM", psum=True)
    zc_v = T["zc_d"].rearrange("(c p) s -> p c s", p=128)
    br_v = T["brT_d"].rearrange("(c p) s -> p c s", p=128)
    evs = []
    for blk in range(NB):
        zch, zk = zchr.next()
        brT, bk = brr.next()
        zks = [(zk, q4) for q4 in range(4)]
        lo = max(0, blk * 512 - HALO)
        hi = min(S, blk * 512 + 512 + HALO)
        off = lo - (blk * 512 - HALO)
        for q4 in range(4):
            P.dma("sync", lambda e, zch=zch, lo=lo, hi=hi, off=off, q4=q4: e.dma_start(out=zch[:, 3 * q4:3 * q4 + 3, off:off + hi - lo], in_=zc_v[:, 3 * q4:3 * q4 + 3, lo:hi]),
                  zk + "_%d" % q4, writes=[(zk, q4)])
        if blk == 0:
            P.op("gpsimd", lambda e, zch=zch: e.memset(zch[:, :, 0:HALO], 0.0), writes=[(zk, 4)])
            zks = zks + [(zk, 4)]
        if blk == NB - 1:
            P.op("gpsimd", lambda e, zch=zch: e.memset(zch[:, :, ZW - HALO:ZW], 0.0), writes=[(zk, 4)])
            zks = zks + [(zk, 4)]
        P.op("gpsimd", lambda e, zch=zch: e.tensor_tensor(out=cu[:], in0=zch[:, 2:4, :], in1=zch[:, 4:6, :], op=ALU.mult), reads=zks, writes=["cu"])
        for c in range(2):
            P.op("vector", lambda e, c=c: e.tensor_scalar(out=acc[:, c, :], in0=cu[:, c, 15:527], scalar1=chp[:, c, 31:32], scalar2=None, op0=ALU.mult),
                 reads=["cu"] + chk, writes=[("acc", c)])
            for k in (1, 2):
                P.op("vector", lambda e, c=c, k=k: e.scalar_tensor_tensor(out=acc[:, c, :], in0=cu[:, c, 15 + k:527 + k], scalar=chp[:, c, 31 + k:32 + k],
                                                                      in1=acc[:, c, :], op0=ALU.mult, op1=ALU.add),
                     reads=["cu", ("acc", c)] + chk, writes=[("acc", c)])
            P.op("vector", lambda e, c=c, zch=zch, brT=brT: e.tensor_tensor(out=brT[:, c, :], in0=acc[:, c, :], in1=zch[:, c, HALO:HALO + 512], op=ALU.mult),
                 reads=[("acc", c)] + zks, writes=[(bk, c)])
        P.op("gpsimd", lambda e, zch=zch: e.tensor_tensor(out=ag[:], in0=zch[:, 6:8, :], in1=zch[:, 8:10, :], op=ALU.mult), reads=zks, writes=["ag"])
        for c in range(2):
            P.op("vector", lambda e, c=c: e.tensor_scalar(out=acc[:, c, :], in0=ag[:, c, 1:513], scalar1=chp[:, c, 0:1], scalar2=chp[:, c, 34:35],
                                                      op0=ALU.mult, op1=ALU.add),
                 reads=["ag"] + chk, writes=[("acc", c)])
            for k in range(1, 31):
                P.op("vector", lambda e, c=c, k=k: e.scalar_tensor_tensor(out=acc[:, c, :], in0=ag[:, c, 1 + k:513 + k], scalar=chp[:, c, k:k + 1],
                                                                      in1=acc[:, c, :], op0=ALU.mult, op1=ALU.add),
                     reads=["ag", ("acc", c)] + chk, writes=[("acc", c)])
        P.op("gpsimd", lambda e: e.tensor_tensor(out=ysq[:], in0=acc[:], in1=acc[:], op=ALU.mult), reads=[("acc", 0), ("acc", 1)], writes=["ysq"])
        pS, pSk = pMr.next()
        pQ, pQk = pMr.next()
        for c in range(2):
            P.op("tensor", lambda e, c=c, pS=pS: e.matmul(pS[:], lhsT=onesf[:], rhs=acc[:, c, :], start=(c == 0), stop=(c == 1)),
                 reads=["onesf", ("acc", c)], writes=[pSk])
        for c in range(2):
            P.op("tensor", lambda e, c=c, pQ=pQ: e.matmul(pQ[:], lhsT=onesf[:], rhs=ysq[:, c, :], start=(c == 0), stop=(c == 1)),
                 reads=["onesf", "ysq"], writes=[pQk])
        P.op("scalar", lambda e, pS=pS: e.activation(out=mean[:], in_=pS[:], func=AF.Identity, scale=1.0 / 256), reads=[pSk], writes=["mean"])
        P.op("gpsimd", lambda e: e.tensor_tensor(out=rstd[:], in0=mean[:], in1=mean[:], op=ALU.mult), reads=["mean"], writes=["rstd"])
        P.op("vector", lambda e, pQ=pQ: e.scalar_tensor_tensor(out=rstd[:], in0=pQ[:], scalar=1.0 / 256, in1=rstd[:], op0=ALU.mult, op1=ALU.subtract),
             reads=[pQk, "rstd"], writes=["rstd"])
        P.op("vector", lambda e: e.tensor_scalar(out=rstd[:], in0=rstd[:], scalar1=LN_EPS, scalar2=None, op0=ALU.add), reads=["rstd"], writes=["rstd"])
        P.op("scalar", lambda e: e.activation(out=rstd[:], in_=rstd[:], func=AF.Sqrt), reads=["rstd"], writes=["rstd"])
        P.op("vector", lambda e: e.reciprocal(out=rstd[:], in_=rstd[:]), reads=["rstd"], writes=["rstd"])
        for c in range(2):
            P.op("vector", lambda e, c=c: e.tensor_tensor(out=acc[:, c, :], in0=acc[:, c, :], in1=mean[:], op=ALU.subtract),
                 reads=[("acc", c), "mean", pSk], writes=[("acc", c)])
            P.op("vector", lambda e, c=c: e.tensor_tensor(out=acc[:, c, :], in0=acc[:, c, :], in1=rstd[:], op=ALU.mult),
                 reads=[("acc", c), "rstd"], writes=[("acc", c)])
            P.op("scalar", lambda e, c=c, brT=brT: e.activation(out=brT[:, 2 + c, :], in_=acc[:, c, :], func=AF.Silu, scale=chp[:, c, 35:36], bias=chp[:, c, 36:37]),
                 reads=[("acc", c)] + chk, writes=[(bk, 2 + c)])
        P.op("gpsimd", lambda e, zch=zch: e.tensor_tensor(out=cu[:, :, 1:ZW], in0=zch[:, 10:12, 0:ZW - 1], in1=zch[:, 10:12, 1:ZW], op=ALU.add),
             reads=zks, writes=["cu"])
        P.op("gpsimd", lambda e: e.tensor_tensor(out=ag[:, :, 2:ZW - 1], in0=cu[:, :, 1:ZW - 2], in1=cu[:, :, 3:ZW], op=ALU.add),
             reads=["cu"], writes=["ag"])
        P.op("gpsimd", lambda e: e.tensor_tensor(out=pC[:, 4:ZW - 3], in0=ag[:, 1, 2:ZW - 5], in1=ag[:, 1, 6:ZW - 1], op=ALU.add),
             reads=["ag"], writes=["pC"])
        P.op("gpsimd", lambda e: e.tensor_tensor(out=pD[:, 8:ZW - 7], in0=pC[:, 4:ZW - 11], in1=pC[:, 12:ZW - 3], op=ALU.add),
             reads=["pC"], writes=["pD"])
        ty = 0 if blk == 0 else (2 if blk == NB - 1 else 1)
        srcs = [(cu, 0, 0), (ag, 0, 1), (pC, None, 0), (pD, None, 1)]
        for gi, (src, cidx, hh) in enumerate(srcs):
            c = gi // 2
            sl = slice(hh * 64, (hh + 1) * 64)
            sap = src[sl, cidx, HALO:HALO + 512] if cidx is not None else src[sl, HALO:HALO + 512]
            P.op("vector", lambda e, sap=sap, sl=sl, c=c, ty=ty: e.tensor_tensor(out=Wt[sl, c, :], in0=sap, in1=invc[sl, ty, c, :], op=ALU.mult),
                 reads=["cu", "ag", "pC", "pD", "invc"], writes=[("Wt", gi)])
        P.op("vector", lambda e, zch=zch: e.tensor_tensor(out=ypool[:], in0=Wt[:], in1=zch[:, 10:12, HALO:HALO + 512], op=ALU.subtract),
             reads=[("Wt", gi) for gi in range(4)] + zks, writes=["ypool"])
        for c in range(2):
            pP, pPk = pMr.next()
            P.op("tensor", lambda e, c=c, pP=pP: e.matmul(pP[:], lhsT=pwbd[:, c, :], rhs=ypool[:, c, :], start=True, stop=True),
                 reads=["pwbd", "ypool"], writes=[pPk])
            P.op("scalar", lambda e, c=c, pP=pP, brT=brT: e.activation(out=brT[:, 4 + c, :], in_=pP[:], func=AF.Identity, scale=chp[:, c, 37:38]),
                 reads=[pPk] + chk, writes=[(bk, 4 + c)])
        evs.append(P.dma("sync", lambda e, brT=brT, blk=blk: e.dma_start(out=br_v[:, 2:8, blk * 512:(blk + 1) * 512], in_=brT[:]), bk,
                         reads=[(bk, i) for i in range(6)]))
    P.flush(evs)
    cx.close()


def phase_B2b(nc, P, T, l, Dall, Wall):
    cx = Ctx(nc, P)
    idf, idb = load_consts(cx, P, T["ident"])
    wv = T["w_in"][l].rearrange("(c p) n -> p c n", p=128)
    wg = cx.sb([128, 8, 4096], BF16)
    for i in range(2):
        P.dma("gpsimd", lambda e, i=i: e.dma_start(out=wg[:, :, i * 2048:(i + 1) * 2048], in_=wv[:, :, 2048 + i * 2048:2048 + (i + 1) * 2048]),
              "wg%d" % i, writes=[("wg", i)])
    wgk = [("wg", 0), ("wg", 1)]
    wbr = cx.sb([128, 8, 1024], BF16)
    P.dma("gpsimd", lambda e: e.dma_start(out=wbr[:], in_=T["w_branch"][l].rearrange("g (c p) n -> p (g c) n", p=128)), "wbr", writes=["wbr"])
    wo = cx.sb([128, 8, 1024], BF16)
    P.dma("gpsimd", lambda e: e.dma_start(out=wo[:], in_=T["w_out"][l].rearrange("(c p) n -> p c n", p=128)), "wo", writes=["wo"])
    wr = cx.sb([128, 8, 32], BF16)
    P.dma("gpsimd", lambda e: e.dma_start(out=wr[:], in_=T["w_router"][l].rearrange("(c p) n -> p c n", p=128)), "wr", writes=["wr"])
    brb = cx.sb([128, 32], F32)
    P.dma("sync", lambda e: e.dma_start(out=brb[:], in_=T["b_router"][l].partition_broadcast(128)), "c3", writes=["brb"])
    gst = cx.sb([32, 128], F32)
    bg = cx.sb([128, 32], F32)
    P.dma("sync", lambda e: e.dma_start(out=gst[:], in_=T["b_in"][l][2048:6144].rearrange("(c p) -> c p", p=128)), "c4", writes=["gst"])
    pmisc = cx.ps([128, 512], F32)
    P.op("tensor", lambda e: e.transpose(out=pmisc[:, 0:32], in_=gst[:], identity=idf[0:32, 0:32]), reads=["gst", "idf"], writes=["pmisc"], mode=32)
    P.op("vector", lambda e: e.tensor_copy(out=bg[:], in_=pmisc[:, 0:32]), reads=["pmisc"], writes=["bg"])
    onesb = cx.sb([128, 128], BF16)
    P.op("vector", lambda e: e.memset(onesb[:], 1.0), writes=["onesb"])
    Uf = cx.sb([128, 128], F32)
    Ub = cx.sb([128, 128], BF16)
    P.dma("sync", lambda e: e.dma_start(out=Uf[:], in_=T["utri"][:, :]), "c8", writes=["Uf"])
    P.op("vector", lambda e: e.tensor_copy(out=Ub[:], in_=Uf[:]), reads=["Uf"], writes=["Ub"])
    iotaC = cx.sb([128, 32], F32)
    P.dma("sync", lambda e: e.dma_start(out=iotaC[:], in_=T["iotac"][:, :]), "c9", writes=["iotaC"])
    cnt = cx.sb([128, 32], F32)
    P.op("vector", lambda e: e.memset(cnt[:], 0.0), writes=["cnt"])
    gt, bt = load_ln_params(P, cx, T["ln1_g"][l], T["ln1_b"][l])

    hTr = cx.ring(2, [128, 8, 512], BF16, "hTb")
    brr = cx.ring(2, [128, 8, 512], BF16, "brT")
    gtr = cx.ring(2, [128, 512], BF16, "gate")
    tmr = cx.ring(2, [128, 512], F32, "tmp")
    acr = cx.ring(2, [128, 512], F32, "macc")
    mT = cx.sb([128, 8, 512], BF16)
    pGr = cx.ring(2, [128, 512], F32, "pG", psum=True)
    pJr = cx.ring(2, [128, 512], F32, "pJ", psum=True)
    pMr = cx.ring(2, [128, 512], F32, "pM", psum=True)
    pTr = cx.ring(1, [128, 8, 128], BF16, "pT8", psum=True)
    htr = cx.ring(2, [128, 1024], F32, "ht")
    ofr = cx.ring(2, [128, 1024], F32, "of")
    obr = cx.ring(2, [128, 1024], BF16, "ob")
    h1Tr = cx.ring(2, [128, 8, 128], BF16, "h1T")
    smr = ln_scratch(cx, 1)
    lgr = cx.ring(2, [128, 32], F32, "lg")
    t8r = cx.ring(2, [128, 8], F32, "t8")
    mkr = cx.ring(2, [128, 32], BF16, "mk")
    dfr = cx.ring(2, [128, 32], F32, "df")
    slr = cx.ring(2, [128, 32], F32, "sl")
    s1r = cx.ring(2, [128, 4], F32, "s1")
    dkr = cx.ring(2, [128, 4], F32, "dk")
    hT_v = T["hT_d"].rearrange("(c p) s -> p c s", p=128)
    br_v = T["brT_d"].rearrange("(c p) s -> p c s", p=128)
    Xe = T["Xe_d"]
    evs = []
    for blk in range(NB):
        hT, hTk = hTr.next()
        P.dma("sync", lambda e, hT=hT, blk=blk: e.dma_start(out=hT[:], in_=hT_v[:, :, blk * 512:(blk + 1) * 512]), hTk, writes=[hTk])
        brT, bk = brr.next()
        P.dma("sync", lambda e, brT=brT, blk=blk: e.dma_start(out=brT[:], in_=br_v[:, :, blk * 512:(blk + 1) * 512]), bk, writes=[bk])
        for oc in range(8):
            ma, mak = acr.next()
            for g in range(4):
                pG, pGk = pGr.next()
                for kc in range(8):
                    P.op("tensor", lambda e, pG=pG, kc=kc, g=g, oc=oc, hT=hT: e.matmul(
                        pG[:], lhsT=wg[:, kc, g * 1024 + oc * 128:g * 1024 + (oc + 1) * 128], rhs=hT[:, kc, :], start=(kc == 0), stop=(kc == 7)),
                        reads=[hTk] + wgk, writes=[pGk])
                gate, gtk = gtr.next()
                P.op("scalar", lambda e, gate=gate, pG=pG, g=g, oc=oc: e.activation(out=gate[:], in_=pG[:], func=AF.Sigmoid, bias=bg[:, g * 8 + oc:g * 8 + oc + 1]),
                     reads=[pGk, "bg"], writes=[gtk])
                pJ, pJk = pJr.next()
                for c in range(2):
                    P.op("tensor", lambda e, pJ=pJ, c=c, g=g, oc=oc, brT=brT: e.matmul(
                        pJ[:], lhsT=wbr[:, g * 2 + c, oc * 128:(oc + 1) * 128], rhs=brT[:, g * 2 + c, :], start=(c == 0), stop=(c == 1)),
                        reads=["wbr", bk], writes=[pJk])
                if g == 0:
                    P.op("vector", lambda e, ma=ma, gate=gate, pJ=pJ: e.tensor_tensor(out=ma[:], in0=gate[:], in1=pJ[:], op=ALU.mult),
                         reads=[gtk, pJk], writes=[mak])
                else:
                    tm, tmk = tmr.next()
                    P.op("vector", lambda e, tm=tm, gate=gate, pJ=pJ: e.tensor_tensor(out=tm[:], in0=gate[:], in1=pJ[:], op=ALU.mult),
                         reads=[gtk, pJk], writes=[tmk])
                    if g < 3:
                        P.op("gpsimd", lambda e, ma=ma, tm=tm: e.tensor_tensor(out=ma[:], in0=ma[:], in1=tm[:], op=ALU.add), reads=[mak, tmk], writes=[mak])
                    else:
                        P.op("gpsimd", lambda e, ma=ma, tm=tm, oc=oc: e.tensor_tensor(out=mT[:, oc, :], in0=ma[:], in1=tm[:], op=ALU.add),
                             reads=[mak, tmk], writes=[("mT", oc)])
        for tt in range(4):
            t = blk * 4 + tt
            ht, htk = htr.next()
            P.dma("sync", lambda e, ht=ht, t=t: e.dma_start(out=ht[:], in_=T["h_d"][t * 128:(t + 1) * 128, :]), htk, writes=[htk])
            for hf in range(2):
                pM, pMk = pMr.next()
                for kc in range(8):
                    P.op("tensor", lambda e, pM=pM, kc=kc, tt=tt, hf=hf: e.matmul(
                        pM[:], lhsT=mT[:, kc, tt * 128:(tt + 1) * 128], rhs=wo[:, kc, hf * 512:(hf + 1) * 512], start=(kc == 0), stop=(kc == 7)),
                        reads=[("mT", kc), "wo"], writes=[pMk])
                P.op("vector", lambda e, ht=ht, pM=pM, hf=hf: e.scalar_tensor_tensor(
                    out=ht[:, hf * 512:(hf + 1) * 512], in0=ht[:, hf * 512:(hf + 1) * 512], scalar=DN_ALPHA, in1=pM[:], op0=ALU.mult, op1=ALU.add),
                    reads=[htk, pMk], writes=[htk])
            of, ofk = ofr.next()
            ob, obk = obr.next()
            ln_core(P, cx, ht, htk, gt, bt, of, ofk, ob, obk, smr.next())
            evs.append(P.dma("sync", lambda e, of=of, t=t: e.dma_start(out=T["h1_d"][t * 128:(t + 1) * 128, :], in_=of[:]), ofk, reads=[ofk]))
            pT, pTk = pTr.next()
            for c in range(8):
                P.op("tensor", lambda e, c=c, pT=pT, ob=ob: e.transpose(out=pT[:, c, :], in_=ob[:, c * 128:(c + 1) * 128], identity=idb[:]),
                     reads=[obk, "idb"], writes=[pTk + "_%d" % c])
            h1T, h1Tk = h1Tr.next()
            P.op("vector", lambda e, h1T=h1T, pT=pT: e.tensor_copy(out=h1T[:], in_=pT[:]), reads=[pTk + "_%d" % c for c in range(8)], writes=[h1Tk])
            for kc in range(8):
                P.op("tensor", lambda e, kc=kc, h1T=h1T: e.matmul(pmisc[:, 0:32], lhsT=h1T[:, kc, :], rhs=wr[:, kc, :], start=(kc == 0), stop=(kc == 7)),
                     reads=[h1Tk, "wr"], writes=["pmisc"])
            lg, lgk = lgr.next()
            P.op("vector", lambda e, lg=lg: e.tensor_tensor(out=lg[:], in0=pmisc[:, 0:32], in1=brb[:], op=ALU.add), reads=["pmisc", "brb"], writes=[lgk])
            t8, t8k = t8r.next()
            P.op("vector", lambda e, t8=t8, lg=lg: e.max(out=t8[:], in_=lg[:]), reads=[lgk], writes=[t8k])
            mk, mkk = mkr.next()
            P.op("vector", lambda e, mk=mk, lg=lg, t8=t8: e.tensor_scalar(out=mk[:], in0=lg[:], scalar1=t8[:, 3:4], scalar2=None, op0=ALU.is_ge),
                 reads=[lgk, t8k], writes=[mkk])
            s1, s1k = s1r.next()
            P.op("vector", lambda e, s1=s1, t8=t8: e.tensor_scalar(out=s1[:, 0:1], in0=t8[:, 0:1], scalar1=-1.0, scalar2=None, op0=ALU.mult),
                 reads=[t8k], writes=[s1k + "n"])
            P.op("scalar", lambda e, s1=s1, t8=t8, t=t: e.activation(out=Wall[:, t, :], in_=t8[:, 0:4], func=AF.Exp, bias=s1[:, 0:1], accum_out=s1[:, 1:2]),
                 reads=[t8k, s1k + "n"], writes=[("Wall", t), s1k + "s"])
            P.op("vector", lambda e, s1=s1: e.reciprocal(out=s1[:, 2:3], in_=s1[:, 1:2]), reads=[s1k + "s"], writes=[s1k + "r"])
            P.op("vector", lambda e, s1=s1, t=t: e.tensor_scalar(out=Wall[:, t, :], in0=Wall[:, t, :], scalar1=s1[:, 2:3], scalar2=None, op0=ALU.mult),
                 reads=[("Wall", t), s1k + "r"], writes=[("Wall", t)])
            P.op("tensor", lambda e, mk=mk: e.matmul(pmisc[:, 64:96], lhsT=Ub[:], rhs=mk[:], start=True, stop=True), reads=["Ub", mkk], writes=["pmisc"])
            P.op("tensor", lambda e, mk=mk: e.matmul(pmisc[:, 128:160], lhsT=onesb[:], rhs=mk[:], start=True, stop=True), reads=["onesb", mkk], writes=["pmisc"])
            df, dfk = dfr.next()
            P.op("vector", lambda e, df=df: e.tensor_tensor(out=df[:], in0=pmisc[:, 64:96], in1=cnt[:], op=ALU.add), reads=["pmisc", "cnt"], writes=[dfk])
            P.op("vector", lambda e: e.tensor_tensor(out=cnt[:], in0=pmisc[:, 128:160], in1=cnt[:], op=ALU.add), reads=["pmisc", "cnt"], writes=["cnt"])
            sl, slk = slr.next()
            P.op("vector", lambda e, sl=sl, df=df: e.tensor_scalar(out=sl[:], in0=df[:], scalar1=float(CAP), scalar2=1.0e6, op0=ALU.is_ge, op1=ALU.mult),
                 reads=[dfk], writes=[slk])
            P.op("vector", lambda e, sl=sl, df=df: e.tensor_tensor(out=df[:], in0=df[:], in1=sl[:], op=ALU.add), reads=[dfk, slk], writes=[dfk])
            P.op("vector", lambda e, df=df: e.tensor_tensor(out=df[:], in0=df[:], in1=iotaC[:], op=ALU.add), reads=[dfk, "iotaC"], writes=[dfk])
            dk, dkk = dkr.next()
            for k in range(4):
                P.op("vector", lambda e, sl=sl, lg=lg, t8=t8, k=k: e.tensor_scalar(out=sl[:], in0=lg[:], scalar1=t8[:, k:k + 1], scalar2=None, op0=ALU.is_equal),
                     reads=[lgk, t8k, dfk], writes=[slk])
                P.op("vector", lambda e, sl=sl, df=df: e.tensor_tensor(out=sl[:], in0=sl[:], in1=df[:], op=ALU.mult), reads=[slk, dfk], writes=[slk])
                P.op("vector", lambda e, sl=sl, dk=dk, k=k: e.tensor_reduce(out=dk[:, k:k + 1], in_=sl[:], axis=AX.X, op=ALU.add), reads=[slk], writes=[(dkk, k)])
            P.op("vector", lambda e, dk=dk, t=t: e.tensor_copy(out=Dall[:, t, :], in_=dk[:]), reads=[(dkk, k) for k in range(4)], writes=[("Dall", t)])
            for k in range(4):
                evs.append(P.dma("gpsimd", lambda e, ob=ob, t=t, k=k: e.indirect_dma_start(
                    out=Xe[:, :], out_offset=bass.IndirectOffsetOnAxis(ap=Dall[:, t, k:k + 1], axis=0), in_=ob[:], in_offset=None,
                    bounds_check=bc_reg(e, P), oob_is_err=False), obk + "sc", reads=[obk, ("Dall", t)]))
    P.flush(evs)
    cx.close()


def phase_C(nc, P, T, l):
    cx = Ctx(nc, P)
    idf, idb = load_consts(cx, P, T["ident"])
    gst = cx.sb([32, 2048], F32)
    bgu = cx.sb([128, 16, 32], F32)
    P.dma("sync", lambda e: e.dma_start(out=gst[:], in_=T["b_gate_up"][l]), "c1", writes=["gst"])
    pmisc = cx.ps([128, 16, 32], F32)
    for c in range(16):
        P.op("tensor", lambda e, c=c: e.transpose(out=pmisc[:, c, :], in_=gst[:, c * 128:(c + 1) * 128], identity=idf[0:32, 0:32]),
             reads=["gst", "idf"], writes=["pmisc"], mode=32)
    P.op("vector", lambda e: e.tensor_copy(out=bgu[:], in_=pmisc[:]), reads=["pmisc"], writes=["bgu"])
    wgr = cx.ring(2, [128, 8, 2048], BF16, "wgu")
    wdr = cx.ring(2, [128, 8, 1024], BF16, "wdn")
    bdr = cx.ring(2, [128, 1024], F32, "bd")
    xr = cx.ring(3, [128, 1024], BF16, "xs")
    XTr = cx.ring(2, [128, 8, 512], BF16, "XT")
    aTr = cx.ring(2, [128, 8, 512], BF16, "actT")
    pTr = cx.ring(2, [128, 8, 128], BF16, "pT8", psum=True)
    pGr = cx.ring(2, [128, 512], F32, "pG", psum=True)
    pLr = cx.ring(2, [128, 512], F32, "pL", psum=True)
    pYr = cx.ring(1, [128, 512], F32, "pY", psum=True)
    ggr = cx.ring(2, [128, 512], F32, "gg")
    sgr = cx.ring(2, [128, 512], F32, "sg")
    llr = cx.ring(2, [128, 512], F32, "ll")
    yr = cx.ring(2, [128, 1024], F32, "y")
    Xe, Ye = T["Xe_d"], T["Ye_d"]
    evs = []

    def load_w(e_i):
        wgu, wguk = wgr.next()
        wdn, wdnk = wdr.next()
        bd, bdk = bdr.next()
        src = T["w_gate_up"][l][e_i].rearrange("(c p) n -> p c n", p=128)
        for hf in range(2):
            P.dma("gpsimd", lambda e, wgu=wgu, src=src, hf=hf: e.dma_start(out=wgu[:, 4 * hf:4 * hf + 4, :], in_=src[:, 4 * hf:4 * hf + 4, :]),
                  wguk + "_%d" % hf, writes=[(wguk, hf)])
        P.dma("gpsimd", lambda e, wdn=wdn, e_i=e_i: e.dma_start(out=wdn[:], in_=T["w_down"][l][e_i].rearrange("(c p) n -> p c n", p=128)), wdnk, writes=[wdnk])
        P.dma("sync", lambda e, bd=bd, e_i=e_i: e.dma_start(out=bd[:], in_=T["b_down"][l][e_i].partition_broadcast(128)), bdk, writes=[bdk])
        return (wgu, wguk, wdn, wdnk, bd, bdk)

    nxt = load_w(0)
    for ex in range(NE):
        wgu, wguk, wdn, wdnk, bd, bdk = nxt
        if ex + 1 < NE:
            nxt = load_w(ex + 1)
        wk = [(wguk, 0), (wguk, 1)]
        for sb in range(CAP // 512):
            base = ex * CAP + sb * 512
            XT, XTk = XTr.next()
            for st in range(4):
                xs, xsk = xr.next()
                P.dma("sync", lambda e, xs=xs, base=base, st=st: e.dma_start(out=xs[:], in_=Xe[base + st * 128:base + (st + 1) * 128, :]), xsk, writes=[xsk])
                pT, pTk = pTr.next()
                for c in range(8):
                    P.op("tensor", lambda e, c=c, pT=pT, xs=xs: e.transpose(out=pT[:, c, :], in_=xs[:, c * 128:(c + 1) * 128], identity=idb[:]),
                         reads=[xsk, "idb"], writes=[pTk + "_%d" % c])
                P.op("vector", lambda e, XT=XT, pT=pT, st=st: e.tensor_copy(out=XT[:, :, st * 128:(st + 1) * 128], in_=pT[:]),
                     reads=[pTk + "_%d" % c for c in range(8)], writes=[(XTk, st)])
            XTks = [(XTk, st) for st in range(4)]
            aT, aTk = aTr.next()
            for fc in range(8):
                pG, pGk = pGr.next()
                pL, pLk = pLr.next()
                for kc in range(8):
                    P.op("tensor", lambda e, pG=pG, kc=kc, fc=fc, XT=XT, wgu=wgu: e.matmul(pG[:], lhsT=wgu[:, kc, fc * 128:(fc + 1) * 128], rhs=XT[:, kc, :],
                                                                                   start=(kc == 0), stop=(kc == 7)),
                         reads=XTks + wk, writes=[pGk])
                for kc in range(8):
                    P.op("tensor", lambda e, pL=pL, kc=kc, fc=fc, XT=XT, wgu=wgu: e.matmul(pL[:], lhsT=wgu[:, kc, 1024 + fc * 128:1024 + (fc + 1) * 128], rhs=XT[:, kc, :],
                                                                                   start=(kc == 0), stop=(kc == 7)),
                         reads=XTks + wk, writes=[pLk])
                gg, ggk = ggr.next()
                P.op("vector", lambda e, gg=gg, pG=pG, fc=fc, ex=ex: e.tensor_scalar(out=gg[:], in0=pG[:], scalar1=bgu[:, fc, ex:ex + 1], scalar2=7.0, op0=ALU.add, op1=ALU.min),
                     reads=[pGk, "bgu"], writes=[ggk])
                sg, sgk = sgr.next()
                P.op("scalar", lambda e, sg=sg, gg=gg: e.activation(out=sg[:], in_=gg[:], func=AF.Sigmoid, scale=1.702), reads=[ggk], writes=[sgk])
                ll, llk = llr.next()
                P.op("scalar", lambda e, ll=ll, pL=pL, fc=fc, ex=ex: e.activation(out=ll[:], in_=pL[:], func=AF.Identity, bias=bgu[:, 8 + fc, ex:ex + 1]),
                     reads=[pLk, "bgu"], writes=[llk])
                P.op("gpsimd", lambda e, ll=ll: e.tensor_scalar(out=ll[:], in0=ll[:], scalar1=7.0, scalar2=-7.0, op0=ALU.min, op1=ALU.max), reads=[llk], writes=[llk])
                P.op("gpsimd", lambda e, gg=gg, sg=sg: e.tensor_tensor(out=gg[:], in0=gg[:], in1=sg[:], op=ALU.mult), reads=[ggk, sgk], writes=[ggk])
                P.op("vector", lambda e, aT=aT, gg=gg, ll=ll, fc=fc: e.scalar_tensor_tensor(out=aT[:, fc, :], in0=ll[:], scalar=1.0, in1=gg[:], op0=ALU.add, op1=ALU.mult),
                     reads=[ggk, llk], writes=[(aTk, fc)])
            aTks = [(aTk, fc) for fc in range(8)]
            for st in range(4):
                y, yk = yr.next()
                for hf in range(2):
                    pY, pYk = pYr.next()
                    for fc in range(8):
                        P.op("tensor", lambda e, pY=pY, fc=fc, st=st, hf=hf, aT=aT, wdn=wdn: e.matmul(
                            pY[:], lhsT=aT[:, fc, st * 128:(st + 1) * 128], rhs=wdn[:, fc, hf * 512:(hf + 1) * 512], start=(fc == 0), stop=(fc == 7)),
                            reads=aTks + [wdnk], writes=[pYk])
                    P.op("vector", lambda e, y=y, pY=pY, hf=hf, bd=bd: e.tensor_tensor(out=y[:, hf * 512:(hf + 1) * 512], in0=pY[:], in1=bd[:, hf * 512:(hf + 1) * 512], op=ALU.add),
                         reads=[pYk, bdk], writes=[(yk, hf)])
                evs.append(P.dma("sync", lambda e, y=y, base=base, st=st: e.dma_start(out=Ye[base + st * 128:base + (st + 1) * 128, :], in_=y[:]), yk,
                                 reads=[(yk, 0), (yk, 1)]))
    P.flush(evs)
    cx.close()


def phase_D(nc, P, T, l, Dall, Wall, last):
    cx = Ctx(nc, P)
    idf, idb = load_consts(cx, P, T["ident"])
    gt, bt = load_ln_params(P, cx, T["ln2_g"][l], T["ln2_b"][l])
    ykr = [cx.ring(2, [128, 1024], F32, "yk%d_" % k) for k in range(4)]
    htr = cx.ring(2, [128, 1024], F32, "ht")
    rr = cx.ring(2, [128, 1024], F32, "r")
    ofr = cx.ring(2, [128, 1024], F32, "of")
    obr = cx.ring(2, [128, 1024], BF16, "ob")
    pTr = cx.ring(2, [128, 8, 128], BF16, "pT", psum=True)
    hTr = cx.ring(2, [128, 8, 128], BF16, "hT")
    smr = ln_scratch(cx)
    Ye = T["Ye_d"]
    evs = []
    for t in range(NT):
        ys = []
        for k in range(4):
            yk_, ykk = ykr[k].next()
            P.op("gpsimd", lambda e, yk_=yk_: e.memset(yk_[:], 0.0), writes=[ykk])
            P.dma("gpsimd", lambda e, yk_=yk_, t=t, k=k: e.indirect_dma_start(
                out=yk_[:], out_offset=None, in_=Ye[:, :], in_offset=bass.IndirectOffsetOnAxis(ap=Dall[:, t, k:k + 1], axis=0),
                bounds_check=bc_reg(e, P), oob_is_err=False), ykk, writes=[ykk])
            ys.append((yk_, ykk))
        ht, htk = htr.next()
        P.dma("sync", lambda e, ht=ht, t=t: e.dma_start(out=ht[:], in_=T["h1_d"][t * 128:(t + 1) * 128, :]), htk, writes=[htk])
        r, rk = rr.next()
        P.op("vector", lambda e, r=r, ht=ht: e.tensor_scalar(out=r[:], in0=ht[:], scalar1=DN_ALPHA, scalar2=None, op0=ALU.mult), reads=[htk], writes=[rk])
        for k in range(4):
            yk_, ykk = ys[k]
            P.op("vector", lambda e, r=r, yk_=yk_, t=t, k=k: e.scalar_tensor_tensor(out=r[:], in0=yk_[:], scalar=Wall[:, t, k:k + 1], in1=r[:], op0=ALU.mult, op1=ALU.add),
                 reads=[ykk, rk], writes=[rk])
        of, ofk = ofr.next()
        ob, obk = obr.next()
        ln_core(P, cx, r, rk, gt, bt, of, ofk, None if last else ob, obk, smr.next())
        dst = T["out"] if last else T["h_d"]
        evs.append(P.dma("sync", lambda e, of=of, t=t, dst=dst: e.dma_start(out=dst[t * 128:(t + 1) * 128, :], in_=of[:]), ofk, reads=[ofk]))
        if not last:
            pT, pTk = pTr.next()
            hTt, hTk = hTr.next()
            evs.append(transpose_store_hT(P, ob, obk, pT, pTk, hTt, hTk, idb, T["hT_d"], t, hTk))
    P.flush(evs)
    cx.close()


PARAM_SPECS = [
    ("ln_in_g", (1024,)), ("ln_in_b", (1024,)), ("w_in", (2, 1024, 6144)), ("b_in", (2, 6144)),
    ("q_norm_g", (2, 64)), ("k_norm_g", (2, 64)), ("sc_conv_w", (2, 3, 256)), ("cf_conv_w", (2, 31, 256)),
    ("cf_conv_b", (2, 256)), ("cf_ln_g", (2, 256)), ("cf_ln_b", (2, 256)), ("pool_w", (2, 4, 64, 64)),
    ("pool_scale", (2, 256)), ("w_branch", (2, 4, 256, 1024)), ("w_out", (2, 1024, 1024)),
    ("ln1_g", (2, 1024)), ("ln1_b", (2, 1024)), ("w_router", (2, 1024, 32)), ("b_router", (2, 32)),
    ("w_gate_up", (2, 32, 1024, 2048)), ("b_gate_up", (2, 32, 2048)), ("w_down", (2, 32, 1024, 1024)),
    ("b_down", (2, 32, 1024)), ("ln2_g", (2, 1024)), ("ln2_b", (2, 1024)),
]
CONST_SPECS = [("ident", (128, 128)), ("cos6", (S, 384)), ("sin6", (S, 384)), ("invc", (3, 128, 2, 512)),
               ("utri", (128, 128)), ("iotac", (128, 32))]
SCRATCH = [("h_d", (S, D), F32), ("hT_d", (D, S), BF16), ("zc_d", (1536, S), F32), ("brT_d", (1024, S), BF16),
           ("h1_d", (S, D), F32), ("Xe_d", (NSLOT, D), BF16), ("Ye_d", (NSLOT, D), F32)]


def build_program(upto=99, debug=(), small=False, astage=9):
    global A_STAGE
    A_STAGE = astage
    nc = bass.Bass("TRN2", target_bir_lowering=False)
    T = {}
    T["x"] = nc.dram_tensor("x", [S, D], F32, kind="ExternalInput").ap()
    for nm, shp in PARAM_SPECS + CONST_SPECS:
        if small and nm in ("w_gate_up", "w_down"):
            shp = (2, 1) + tuple(shp[2:])
        T[nm] = nc.dram_tensor(nm, list(shp), F32, kind="ExternalInput").ap()
    T["out"] = nc.dram_tensor("out", [S, D], F32, kind="ExternalOutput").ap()
    for nm, shp, dt in SCRATCH:
        T[nm] = nc.dram_tensor(nm, list(shp), dt, kind="ExternalOutput" if nm in debug else "Internal").ap()
    P = Prog(nc)
    with ExitStack() as top:
        Dall = top.enter_context(nc.sbuf_tensor("Dall", [128, NT, 4], I32))
        Wall = top.enter_context(nc.sbuf_tensor("Wall", [128, NT, 4], F32))
        ph = 0
        phase_ln_in(nc, P, T)
        for l in range(DEPTH):
            if ph >= upto:
                break
            with ExitStack() as att:
                QTa = att.enter_context(nc.sbuf_tensor("QT%d" % l, [128, 4, S], BF16))
                QTb = None
                KT = att.enter_context(nc.sbuf_tensor("KT%d" % l, [128, S], BF16))
                Vaug = att.enter_context(nc.sbuf_tensor("Vaug%d" % l, [128, NT, 2, 66], BF16))
                phase_A(nc, P, T, l, QTa, QTb, KT, Vaug)
                ph += 1
                if ph >= upto:
                    break
                phase_B1(nc, P, T, l, QTa, QTb, KT, Vaug)
                ph += 1
            if ph >= upto:
                break
            phase_B2a(nc, P, T, l)
            ph += 1
            if ph >= upto:
                break
            phase_B2b(nc, P, T, l, Dall, Wall)
            ph += 1
            if ph >= upto:
                break
            phase_C(nc, P, T, l)
            ph += 1
            if ph >= upto:
                break
            phase_D(nc, P, T, l, Dall, Wall, last=(l == DEPTH - 1))
            ph += 1
    return nc


def host_consts():
    c = {}
    c["ident"] = np.eye(128, dtype=np.float32)
    rows = S // 64
    row = np.repeat(np.arange(rows, dtype=np.float32), 64)
    col = np.tile(np.arange(64, dtype=np.float32), rows)
    inv = (np.float32(10000.0) ** (-np.arange(0, 32, 2, dtype=np.float32) / np.float32(32))).astype(np.float32)
    ar = (row[:, None] * inv).astype(np.float32)
    ac = (col[:, None] * inv).astype(np.float32)
    cr, sr, cc, sc = np.cos(ar), np.sin(ar), np.cos(ac), np.sin(ac)
    cos1 = np.concatenate([cr, cr, cc, cc], axis=1).astype(np.float32)
    sin1 = np.concatenate([-sr, sr, -sc, sc], axis=1).astype(np.float32)
    c["cos6"] = np.ascontiguousarray(np.tile(cos1, (1, 6)))
    c["sin6"] = np.ascontiguousarray(np.tile(sin1, (1, 6)))
    invc = np.zeros((3, 128, 2, 512), np.float32)
    wins = (2, 4, 8, 16)
    for ty, blk in enumerate((0, 1, NB - 1)):
        t = np.arange(blk * 512, blk * 512 + 512)
        for gi, w in enumerate(wins):
            lo = np.maximum(t - w // 2, 0)
            hi = np.minimum(t + (w - 1 - w // 2), S - 1)
            ic = (1.0 / (hi - lo + 1)).astype(np.float32)
            cidx, hh = gi // 2, gi % 2
            invc[ty, hh * 64:(hh + 1) * 64, cidx, :] = ic[None, :]
    c["invc"] = invc
    c["utri"] = np.triu(np.ones((128, 128), np.float32), k=1)
    c["iotac"] = np.tile((np.arange(32, dtype=np.float32) * CAP)[None, :], (128, 1)).astype(np.float32)
    return c


_CACHE = {}


def kernel(**inputs):
    if "nc" not in _CACHE:
        _CACHE["nc"] = build_program()
    nc = _CACHE["nc"]
    consts = host_consts()
    shared = {nm: np.ascontiguousarray(np.asarray(inputs[nm], dtype=np.float32)) for nm, _ in PARAM_SPECS}
    shared.update(consts)
    x = np.asarray(inputs["x"], dtype=np.float32)
    in_maps = []
    for b in range(8):
        m = dict(shared)
        m["x"] = np.ascontiguousarray(x[b])
        in_maps.append(m)
    res = run_bass_kernel_spmd(nc, in_maps, core_ids=list(range(8)))
    return np.stack([np.asarray(r["out"]) for r in res.results], axis=0).astype(np.float32)
```

```python
import math
from contextlib import ExitStack
import numpy as np
import concourse.bass as bass
import concourse.mybir as mybir
from concourse.bass_utils import run_bass_kernel_spmd

F32 = mybir.dt.float32
BF16 = mybir.dt.bfloat16
I32 = mybir.dt.int32
AF = mybir.ActivationFunctionType
ALU = mybir.AluOpType
AX = mybir.AxisListType

ENGS = ("sync", "scalar", "vector", "gpsimd", "tensor")
S = 8192
D = 1024
NT = 64
NB = 16
NE = 32
CAP = 1536
NSLOT = NE * CAP
DEPTH = 2
DN_ALPHA = (2.0 * DEPTH) ** 0.25
LN_EPS = 1e-5
HALO = 16
ZW = 512 + 2 * HALO


class _Probe:
    def __init__(self):
        self.name = None

    def __getattr__(self, nm):
        def f(*a, **k):
            self.name = nm
            return self
        return f


class Prog:
    def __init__(self, nc):
        self.nc = nc
        self.esem = {e: nc.alloc_semaphore("es_" + e) for e in ENGS}
        self.ebase = {e: 0 for e in ENGS}
        self.dsem = {}
        self.nflush = 0
        self._reset()

    def _reset(self):
        self.ops = {e: [] for e in ENGS}
        self.waited = {e: {} for e in ENGS}
        self.last_w = {}
        self.readers = {}
        self.signal = {e: set() for e in ENGS}
        self.pe_mode = None

    def _need(self, eng, ev, waits):
        if ev is None:
            return
        if ev[0] == "e" and ev[1] == eng and eng == "tensor":
            return
        key = (ev[0], ev[1])
        if self.waited[eng].get(key, -1) >= ev[2]:
            return
        waits[key] = max(waits.get(key, -1), ev[2])

    def _deps(self, eng, reads, writes, extra=()):
        waits = {}
        for k in reads:
            self._need(eng, self.last_w.get(k), waits)
        for k in writes:
            self._need(eng, self.last_w.get(k), waits)
            for ev in self.readers.get(k, ()):
                self._need(eng, ev, waits)
        for ev in extra:
            self._need(eng, ev, waits)
        out = []
        for key, v in waits.items():
            self.waited[eng][key] = v
            out.append((key[0], key[1], v))
            if key[0] == "e":
                self.signal[key[1]].add(v)
        return out

    def _commit(self, ev, reads, writes):
        for k in reads:
            self.readers.setdefault(k, []).append(ev)
        for k in writes:
            self.last_w[k] = ev
            self.readers[k] = []

    def op(self, eng, fn, reads=(), writes=(), extra=(), mode=128):
        waits = self._deps(eng, reads, writes, extra)
        idx = len(self.ops[eng])
        if eng == "tensor":
            pr = _Probe()
            fn(pr)
            mode = (pr.name, mode)
            if self.pe_mode is not None and self.pe_mode != mode and idx > 0:
                key = ("e", "tensor")
                if self.waited[eng].get(key, -1) < idx - 1:
                    self.waited[eng][key] = idx - 1
                    waits.append(("e", "tensor", idx - 1))
                    self.signal["tensor"].add(idx - 1)
            self.pe_mode = mode
        self.ops[eng].append(dict(fn=fn, waits=waits, dma=None))
        ev = ("e", eng, idx)
        self._commit(ev, reads, writes)
        return ev

    def dma(self, eng, fn, semkey, reads=(), writes=(), extra=()):
        waits = self._deps(eng, reads, writes, extra)
        sn = "ds_" + semkey
        if sn not in self.dsem:
            self.dsem[sn] = [self.nc.alloc_semaphore(sn), 0]
        st = self.dsem[sn]
        st[1] += 16
        self.ops[eng].append(dict(fn=fn, waits=waits, dma=(st[0], st[1])))
        ev = ("d", sn, st[1])
        self._commit(ev, reads, writes)
        return ev

    def wait(self, eng, evs):
        waits = self._deps(eng, (), (), evs)
        self.ops[eng].append(dict(fn=None, waits=waits, dma=None))

    def flush(self, final_events=()):
        nc = self.nc
        if final_events:
            self.wait("sync", final_events)
        val = {}
        for e in ENGS:
            v = self.ebase[e]
            m = {}
            for idx in sorted(self.signal[e]):
                v += 1
                m[idx] = v
            val[e] = m
            self.ebase[e] = v
        ops, esem, dsem = self.ops, self.esem, self.dsem

        def emit(e, name):
            for idx, o in enumerate(ops[name]):
                for kind, who, v in o["waits"]:
                    if kind == "e":
                        e.wait_ge(esem[who], val[who][v])
                    else:
                        e.wait_ge(dsem[who][0], v)
                if o["fn"] is None:
                    continue
                ins = o["fn"](e)
                if o["dma"] is not None:
                    ins.then_inc(o["dma"][0], 16)
                elif idx in val[name]:
                    ins.then_inc(esem[name], 1)

        with nc.Block() as block:
            @block.sync
            def _(e):
                emit(e, "sync")

            @block.scalar
            def _(e):
                emit(e, "scalar")

            @block.vector
            def _(e):
                emit(e, "vector")

            @block.gpsimd
            def _(e):
                emit(e, "gpsimd")

            @block.tensor
            def _(e):
                emit(e, "tensor")
        self.nflush += 1
        self._reset()


_REG = {}


def bc_reg(e, P):
    key = P.nflush
    if key not in _REG:
        _REG.clear()
        _REG[key] = e.to_reg(NSLOT - 1)
    return _REG[key]


class Ring:
    def __init__(self, items):
        self.items = items
        self.i = -1

    def next(self):
        self.i = (self.i + 1) % len(self.items)
        return self.items[self.i]


class Ctx:
    _uid = [0]

    def __init__(self, nc, P):
        self.nc, self.P = nc, P
        self.stack = ExitStack()
        self.n = 0
        Ctx._uid[0] += 1
        self.uid = Ctx._uid[0]

    def sb(self, shape, dt, name=None):
        self.n += 1
        return self.stack.enter_context(self.nc.sbuf_tensor(name or f"t{self.n}_{self.uid}", list(shape), dt))

    def ps(self, shape, dt, name=None):
        self.n += 1
        return self.stack.enter_context(self.nc.psum_tensor(name or f"p{self.n}_{self.uid}", list(shape), dt))

    def ring(self, n, shape, dt, key, psum=False):
        return Ring([((self.ps if psum else self.sb)(shape, dt), f"{key}{i}") for i in range(n)])

    def close(self):
        self.stack.close()


def load_consts(cx, P, ident_d):
    nc = cx.nc
    idf = cx.sb([128, 128], F32)
    idb = cx.sb([128, 128], BF16)
    P.dma("sync", lambda e: e.dma_start(out=idf[:], in_=ident_d[:, :]), "c0", writes=["idf"])
    P.op("vector", lambda e: e.tensor_copy(out=idb[:], in_=idf[:]), reads=["idf"], writes=["idb"])
    return idf, idb


def ln_core(P, cx, r, rk, gt, bt, of, ofk, ob, obk, sm):
    st, mv, rs, nb, xn, k = sm
    for c in range(2):
        P.op("vector", lambda e, c=c: e.bn_stats(out=st[:, c, :], in_=r[:, c * 512:(c + 1) * 512]),
             reads=[rk], writes=[k + "st%d" % c])
    P.op("vector", lambda e: e.bn_aggr(out=mv[:], in_=st[:].rearrange("p a b -> p (a b)")),
         reads=[k + "st0", k + "st1"], writes=[k + "mv"])
    P.op("vector", lambda e: e.tensor_scalar(out=rs[:], in0=mv[:, 1:2], scalar1=LN_EPS, scalar2=None, op0=ALU.add),
         reads=[k + "mv"], writes=[k + "rs"])
    P.op("scalar", lambda e: e.activation(out=rs[:], in_=rs[:], func=AF.Sqrt), reads=[k + "rs"], writes=[k + "rs"])
    P.op("vector", lambda e: e.reciprocal(out=rs[:], in_=rs[:]), reads=[k + "rs"], writes=[k + "rs"])
    P.op("vector", lambda e: e.tensor_scalar(out=nb[:], in0=mv[:, 0:1], scalar1=rs[:, 0:1], scalar2=-1.0,
                                             op0=ALU.mult, op1=ALU.mult),
         reads=[k + "mv", k + "rs"], writes=[k + "nb"])
    P.op("scalar", lambda e: e.activation(out=xn[:], in_=r[:], func=AF.Identity, scale=rs[:, 0:1], bias=nb[:, 0:1]),
         reads=[rk, k + "rs", k + "nb"], writes=[k + "xn"])
    P.op("vector", lambda e: e.tensor_tensor(out=xn[:], in0=xn[:], in1=gt[:], op=ALU.mult),
         reads=[k + "xn", "lng"], writes=[k + "xn"])
    P.op("gpsimd", lambda e: e.tensor_tensor(out=of[:], in0=xn[:], in1=bt[:], op=ALU.add),
         reads=[k + "xn", "lnb"], writes=[ofk])
    if ob is not None:
        P.op("scalar", lambda e: e.copy(out=ob[:], in_=of[:]), reads=[ofk], writes=[obk])


def ln_scratch(cx, n=2):
    return Ring([(cx.sb([128, 2, 6], F32), cx.sb([128, 2], F32), cx.sb([128, 1], F32), cx.sb([128, 1], F32),
                  cx.sb([128, 1024], F32), f"ln{i}") for i in range(n)])


def load_ln_params(P, cx, g_ap, b_ap):
    gt = cx.sb([128, 1024], F32)
    bt = cx.sb([128, 1024], F32)
    P.dma("sync", lambda e: e.dma_start(out=gt[:], in_=g_ap.partition_broadcast(128)), "c1", writes=["lng"])
    P.dma("sync", lambda e: e.dma_start(out=bt[:], in_=b_ap.partition_broadcast(128)), "c2", writes=["lnb"])
    return gt, bt


def transpose_store_hT(P, hb, hbk, pT, pTk, hTt, hTk, idb, hT_d, t, semkey):
    for c in range(8):
        P.op("tensor", lambda e, c=c: e.transpose(out=pT[:, c, :], in_=hb[:, c * 128:(c + 1) * 128], identity=idb[:]),
             reads=[hbk, "idb"], writes=[pTk + "_%d" % c])
    P.op("vector", lambda e: e.tensor_copy(out=hTt[:], in_=pT[:]), reads=[pTk + "_%d" % c for c in range(8)],
         writes=[hTk])
    return P.dma("sync", lambda e: e.dma_start(
        out=hT_d.rearrange("(c p) s -> p c s", p=128)[:, :, t * 128:(t + 1) * 128], in_=hTt[:]),
        semkey, reads=[hTk])


def phase_ln_in(nc, P, T):
    cx = Ctx(nc, P)
    idf, idb = load_consts(cx, P, T["ident"])
    gt, bt = load_ln_params(P, cx, T["ln_in_g"], T["ln_in_b"])
    xr = cx.ring(2, [128, 1024], F32, "x")
    ofr = cx.ring(2, [128, 1024], F32, "of")
    obr = cx.ring(2, [128, 1024], BF16, "ob")
    pTr = cx.ring(2, [128, 8, 128], BF16, "pT", psum=True)
    hTr = cx.ring(2, [128, 8, 128], BF16, "hT")
    smr = ln_scratch(cx)
    evs = []
    for t in range(NT):
        xt, xk = xr.next()
        P.dma("sync", lambda e, xt=xt, t=t: e.dma_start(out=xt[:], in_=T["x"][t * 128:(t + 1) * 128, :]), xk, writes=[xk])
        of, ofk = ofr.next()
        ob, obk = obr.next()
        ln_core(P, cx, xt, xk, gt, bt, of, ofk, ob, obk, smr.next())
        evs.append(P.dma("sync", lambda e, of=of, t=t: e.dma_start(out=T["h_d"][t * 128:(t + 1) * 128, :], in_=of[:]),
                         ofk, reads=[ofk]))
        pT, pTk = pTr.next()
        hTt, hTk = hTr.next()
        evs.append(transpose_store_hT(P, ob, obk, pT, pTk, hTt, hTk, idb, T["hT_d"], t, hTk))
    P.flush(evs)
    cx.close()


A_STAGE = 9
QPERM = [0, 2, 1, 3]


def phase_A(nc, P, T, l, QTa, QTb, KT, Vaug):
    cx = Ctx(nc, P)
    idf, idb = load_consts(cx, P, T["ident"])
    w_in = T["w_in"][l]
    b_in = T["b_in"][l]
    wv = w_in.rearrange("(c p) n -> p c n", p=128)
    w1 = cx.sb([128, 8, 2048], BF16)
    for s in range(4):
        hs = QPERM[s]
        P.dma("gpsimd", lambda e, s=s, hs=hs: e.dma_start(out=w1[:, :, s * 64:(s + 1) * 64], in_=wv[:, :, hs * 64:(hs + 1) * 64]),
              "w1q", writes=[("w1", s)])
    P.dma("gpsimd", lambda e: e.dma_start(out=w1[:, :, 256:2048], in_=wv[:, :, 256:2048]), "w1r", writes=[("w1", 4)])
    w1k = [("w1", i) for i in range(5)]
    bq = cx.sb([128, 512], F32)
    for s in range(4):
        hs = QPERM[s]
        P.dma("sync", lambda e, s=s, hs=hs: e.dma_start(out=bq[:, s * 64:(s + 1) * 64],
                                                        in_=b_in[hs * 64:(hs + 1) * 64].partition_broadcast(128)),
              "c1", writes=[("bq", s)])
    P.dma("sync", lambda e: e.dma_start(out=bq[:, 256:512], in_=b_in[256:512].partition_broadcast(128)), "c2",
          writes=[("bq", 4)])
    bqk = [("bq", i) for i in range(5)]
    bst = cx.sb([12, 128], F32)
    bc = cx.sb([128, 12], F32)
    P.dma("sync", lambda e: e.dma_start(out=bst[:], in_=b_in[512:2048].rearrange("(c p) -> c p", p=128)), "c3", writes=["bst"])
    pB = cx.ps([128, 512], F32)
    P.op("tensor", lambda e: e.transpose(out=pB[:, 0:12], in_=bst[:], identity=idf[0:12, 0:12]), reads=["bst", "idf"], writes=["pB"], mode=32)
    P.op("vector", lambda e: e.tensor_copy(out=bc[:], in_=pB[:, 0:12]), reads=["pB"], writes=["bc"])
    gq = cx.sb([128, 384], F32)
    for s in range(6):
        src = T["q_norm_g"][l] if s < 4 else T["k_norm_g"][l]
        P.dma("sync", lambda e, s=s, src=src: e.dma_start(out=gq[:, s * 64:(s + 1) * 64], in_=src.partition_broadcast(128)),
              "c4" if s < 4 else "c5", writes=[("gq", s)])
    P.op("vector", lambda e: e.tensor_scalar(out=gq[:, 0:256], in0=gq[:, 0:256], scalar1=0.125, scalar2=None, op0=ALU.mult),
         reads=[("gq", s) for s in range(4)], writes=["gqs"])
    gqk = ["gqs", ("gq", 4), ("gq", 5)]
    P.op("gpsimd", lambda e: e.memset(Vaug[:, :, :, 64:66], 1.0), writes=["Vones"])
    for h in range(4):
        P.op("gpsimd", lambda e, h=h: e.memset(QTa[:, h, :], 0.0), writes=["QTz"])

    hTr = cx.ring(2, [128, 8, 512], BF16, "hTb")
    csr = cx.ring(2, [128, 2, 384], F32, "cs")
    pqr = cx.ring(2, [128, 512], F32, "pq", psum=True)
    pcr = cx.ring(2, [128, 512], F32, "pc", psum=True)
    pTr = cx.ring(2, [128, 8, 128], BF16, "pT3", psum=True)
    zbr = cx.ring(2, [128, 512], F32, "zb")
    sqr = cx.ring(2, [128, 384], F32, "sq")
    ssr = cx.ring(2, [128, 6], F32, "ss")
    xnr = cx.ring(2, [128, 384], F32, "xn")
    t1r = cx.ring(2, [128, 384], F32, "t1")
    t2r = cx.ring(2, [128, 384], F32, "t2")
    qbr = cx.ring(2, [128, 384], BF16, "qb")
    ztr = cx.ring(3, [128, 512], F32, "zt")
    hT_v = T["hT_d"].rearrange("(c p) s -> p c s", p=128)
    zc_v = T["zc_d"]
    evs = []
    for blk in range(NB if A_STAGE >= 1 else 0):
        hT, hTk = hTr.next()
        P.dma("sync", lambda e, hT=hT, blk=blk: e.dma_start(out=hT[:], in_=hT_v[:, :, blk * 512:(blk + 1) * 512]), hTk, writes=[hTk])
        for tt in range(4 if A_STAGE >= 2 else 0):
            t = blk * 4 + tt
            cs, csk = csr.next()
            P.dma("sync", lambda e, cs=cs, t=t: e.dma_start(out=cs[:, 0, :], in_=T["cos6"][t * 128:(t + 1) * 128, :]), csk + "a", writes=[csk + "a"])
            P.dma("sync", lambda e, cs=cs, t=t: e.dma_start(out=cs[:, 1, :], in_=T["sin6"][t * 128:(t + 1) * 128, :]), csk + "b", writes=[csk + "b"])
            pq, pqk = pqr.next()
            for kc in range(8):
                P.op("tensor", lambda e, pq=pq, hT=hT, kc=kc, tt=tt: e.matmul(pq[:], lhsT=hT[:, kc, tt * 128:(tt + 1) * 128], rhs=w1[:, kc, 0:512],
                                                                       start=(kc == 0), stop=(kc == 7)),
                     reads=[hTk] + w1k, writes=[pqk])
            zb, zbk = zbr.next()
            P.op("vector", lambda e, zb=zb, pq=pq: e.tensor_tensor(out=zb[:], in0=pq[:], in1=bq[:], op=ALU.add),
                 reads=[pqk] + bqk, writes=[zbk])
            if A_STAGE < 2.15:
                continue
            sq, sqk = sqr.next()
            P.op("gpsimd", lambda e, sq=sq, zb=zb: e.tensor_tensor(out=sq[:], in0=zb[:, 0:384], in1=zb[:, 0:384], op=ALU.mult),
                 reads=[zbk], writes=[sqk])
            ss, ssk = ssr.next()
            P.op("vector", lambda e, ss=ss, sq=sq: e.tensor_reduce(out=ss[:], in_=sq[:].rearrange("p (h d) -> p h d", d=64), axis=AX.X, op=ALU.add),
                 reads=[sqk], writes=[ssk])
            P.op("vector", lambda e, ss=ss: e.tensor_scalar(out=ss[:], in0=ss[:], scalar1=1.0 / 64, scalar2=1e-6, op0=ALU.mult, op1=ALU.add),
                 reads=[ssk], writes=[ssk])
            P.op("scalar", lambda e, ss=ss: e.activation(out=ss[:], in_=ss[:], func=AF.Sqrt), reads=[ssk], writes=[ssk])
            P.op("vector", lambda e, ss=ss: e.reciprocal(out=ss[:], in_=ss[:]), reads=[ssk], writes=[ssk])
            if A_STAGE < 2.25:
                continue
            xn, xnk = xnr.next()
            P.op("vector", lambda e, xn=xn, zb=zb, ss=ss: e.tensor_tensor(
                out=xn[:].rearrange("p (h d) -> p h d", d=64), in0=zb[:, 0:384].rearrange("p (h d) -> p h d", d=64),
                in1=ss[:, :].unsqueeze(2).to_broadcast([128, 6, 64]), op=ALU.mult), reads=[zbk, ssk], writes=[xnk])
            if A_STAGE < 2.35:
                continue
            P.op("gpsimd", lambda e, xn=xn: e.tensor_tensor(out=xn[:], in0=xn[:], in1=gq[:], op=ALU.mult), reads=[xnk] + gqk, writes=[xnk])
            if A_STAGE < 2.45:
                continue
            t1, t1k = t1r.next()
            P.op("vector", lambda e, t1=t1, xn=xn, cs=cs: e.tensor_tensor(out=t1[:], in0=xn[:], in1=cs[:, 0, :], op=ALU.mult),
                 reads=[xnk, csk + "a"], writes=[t1k])
            t2, t2k = t2r.next()
            xv = xn[:].rearrange("p (a b c) -> p a b c", b=2, c=16)
            sv = cs[:, 1, :].rearrange("p (a b c) -> p a b c", b=2, c=16)
            tv = t2[:].rearrange("p (a b c) -> p a b c", b=2, c=16)
            P.op("gpsimd", lambda e, xv=xv, sv=sv, tv=tv: e.tensor_tensor(out=tv[:, :, 0, :], in0=xv[:, :, 1, :], in1=sv[:, :, 0, :], op=ALU.mult),
                 reads=[xnk, csk + "b"], writes=[t2k + "x"])
            P.op("gpsimd", lambda e, xv=xv, sv=sv, tv=tv: e.tensor_tensor(out=tv[:, :, 1, :], in0=xv[:, :, 0, :], in1=sv[:, :, 1, :], op=ALU.mult),
                 reads=[xnk, csk + "b"], writes=[t2k + "y"])
            qb, qbk = qbr.next()
            P.op("vector", lambda e, qb=qb, t1=t1, t2=t2: e.tensor_tensor(out=qb[:], in0=t1[:], in1=t2[:], op=ALU.add),
                 reads=[t1k, t2k + "x", t2k + "y"], writes=[qbk])
            if A_STAGE < 2.55:
                continue
            pT, pTk = pTr.next()
            for c in range(3):
                P.op("tensor", lambda e, pT=pT, qb=qb, c=c: e.transpose(out=pT[:, c, :], in_=qb[:, c * 128:(c + 1) * 128], identity=idb[:]),
                     reads=[qbk, "idb"], writes=[pTk + "_%d" % c])
            for c in range(2):
                for g in range(2):
                    h = 2 * g + c
                    P.op("vector", lambda e, pT=pT, c=c, g=g, h=h, t=t: e.tensor_copy(out=QTa[64 * g:64 * g + 64, h, t * 128:(t + 1) * 128], in_=pT[64 * g:64 * g + 64, c, :]),
                         reads=[pTk + "_%d" % c, "QTz"], writes=[("QT", h, t)])
            P.op("vector", lambda e, pT=pT, t=t: e.tensor_copy(out=KT[:, t * 128:(t + 1) * 128], in_=pT[:, 2, :]),
                 reads=[pTk + "_2"], writes=[("KT", t)])
            P.op("vector", lambda e, zb=zb, t=t: e.tensor_copy(out=Vaug[:, t, :, 0:64], in_=zb[:, 384:512].rearrange("p (g d) -> p g d", d=64)),
                 reads=[zbk], writes=[("V", t)])
        for cc in range(12 if A_STAGE >= 3 else 0):
            pc, pck = pcr.next()
            for kc in range(8):
                P.op("tensor", lambda e, pc=pc, hT=hT, kc=kc, cc=cc: e.matmul(pc[:], lhsT=w1[:, kc, 512 + cc * 128:512 + (cc + 1) * 128], rhs=hT[:, kc, :],
                                                                       start=(kc == 0), stop=(kc == 7)),
                     reads=[hTk] + w1k, writes=[pck])
            zt, ztk = ztr.next()
            fn = AF.Sigmoid if cc in (8, 9) else AF.Identity
            P.op("scalar", lambda e, zt=zt, pc=pc, cc=cc, fn=fn: e.activation(out=zt[:], in_=pc[:], func=fn, bias=bc[:, cc:cc + 1]),
                 reads=[pck, "bc"], writes=[ztk])
            evs.append(P.dma("sync", lambda e, zt=zt, cc=cc, blk=blk: e.dma_start(out=zc_v[cc * 128:(cc + 1) * 128, blk * 512:(blk + 1) * 512], in_=zt[:]),
                             ztk, reads=[ztk]))
    P.flush(evs)
    cx.close()


def phase_B1(nc, P, T, l, QTa, QTb, KT, Vaug):
    cx = Ctx(nc, P)
    idf, idb = load_consts(cx, P, T["ident"])
    pSr = cx.ring(2, [128, 512], F32, "pS", psum=True)
    pO = [cx.ps([128, 512], F32) for _ in range(4)]
    pTr = cx.ring(1, [128, 2, 512], BF16, "pTa", psum=True)
    PTr = cx.ring(3, [128, 512], BF16, "PT")
    atr = cx.ring(2, [128, 4, 256], BF16, "at")
    aTr = cx.ring(2, [128, 2, 512], BF16, "aT")
    rcr = cx.ring(4, [128, 1], F32, "rc")
    Obr = cx.ring(2, [128, 4, 66], F32, "Ob")
    br_v = T["brT_d"].rearrange("(c p) s -> p c s", p=128)
    evs = []
    par = 0
    for qb in range(NB):
        at, atk = atr.next()
        for h in range(4):
            if True:
                g = h // 2
                par ^= 1
                qk = [("QT", h, qb * 4 + i) for i in range(4)]

                def emitS(kc, h=h, qb=qb, qk=qk):
                    pS, pSk = pSr.next()
                    P.op("tensor", lambda e, pS=pS: e.matmul(pS[:], lhsT=KT[:, kc * 128:(kc + 1) * 128],
                                                         rhs=QTa[:, h, qb * 512:(qb + 1) * 512], start=True, stop=True),
                         reads=qk + [("KT", kc)], writes=[pSk])
                    return pS, pSk
                cur = emitS(0)
                for kc in range(64):
                    nxt = emitS(kc + 1) if kc < 63 else None
                    pS, pSk = cur
                    PT, PTk = PTr.next()
                    P.op("scalar", lambda e, PT=PT, pS=pS: e.activation(out=PT[:], in_=pS[:], func=AF.Exp), reads=[pSk], writes=[PTk])
                    for qt in range(4):
                        P.op("tensor", lambda e, PT=PT, qt=qt, kc=kc, g=g: e.matmul(
                            pO[qt][:, 0:65], lhsT=PT[:, qt * 128:(qt + 1) * 128], rhs=Vaug[:, kc, g, 0:65],
                            start=(kc == 0), stop=(kc == 63)),
                            reads=[PTk, ("V", kc), "Vones"], writes=[("pO", qt)])
                    cur = nxt
                Ob, Obk = Obr.next()
                for qt in range(4):
                    P.op("vector", lambda e, Ob=Ob, qt=qt: e.tensor_copy(out=Ob[:, qt, :], in_=pO[qt][:, 0:66]),
                         reads=[("pO", qt)], writes=[(Obk, qt)])
                for qt in range(4):
                    rc, rck = rcr.next()
                    P.op("vector", lambda e, rc=rc, qt=qt, Ob=Ob: e.reciprocal(out=rc[:], in_=Ob[:, qt, 64:65]),
                         reads=[(Obk, qt)], writes=[rck])
                    P.op("vector", lambda e, rc=rc, qt=qt, Ob=Ob, at=at, h=h: e.tensor_scalar(
                        out=at[:, qt, h * 64:(h + 1) * 64], in0=Ob[:, qt, 0:64], scalar1=rc[:, 0:1], scalar2=None, op0=ALU.mult),
                        reads=[(Obk, qt), rck], writes=[(atk, qt, h)])
        pT, pTk = pTr.next()
        for qt in range(4):
            for hf in range(2):
                P.op("tensor", lambda e, pT=pT, at=at, qt=qt, hf=hf: e.transpose(out=pT[:, hf, qt * 128:(qt + 1) * 128], in_=at[:, qt, hf * 128:(hf + 1) * 128], identity=idb[:]),
                     reads=[(atk, qt, hh) for hh in range(4)] + ["idb"], writes=[(pTk, qt, hf)])
        aT, aTk = aTr.next()
        P.op("vector", lambda e, aT=aT, pT=pT: e.tensor_copy(out=aT[:], in_=pT[:]), reads=[(pTk, qt, hf) for qt in range(4) for hf in range(2)], writes=[aTk])
        evs.append(P.dma("sync", lambda e, aT=aT, qb=qb: e.dma_start(out=br_v[:, 0:2, qb * 512:(qb + 1) * 512], in_=aT[:]), aTk, reads=[aTk]))
    P.flush(evs)
    cx.close()


def phase_B2a(nc, P, T, l):
    cx = Ctx(nc, P)
    idf, idb = load_consts(cx, P, T["ident"])
    pmisc = cx.ps([128, 512], F32)
    cst = cx.sb([38, 256], F32)
    P.dma("sync", lambda e: e.dma_start(out=cst[0:31, :], in_=T["cf_conv_w"][l]), "c3", writes=[("cst", 0)])
    P.dma("sync", lambda e: e.dma_start(out=cst[31:34, :], in_=T["sc_conv_w"][l]), "c4", writes=[("cst", 1)])
    for i, nm in enumerate(("cf_conv_b", "cf_ln_g", "cf_ln_b", "pool_scale")):
        P.dma("sync", lambda e, i=i, nm=nm: e.dma_start(out=cst[34 + i:35 + i, :], in_=T[nm][l:l + 1, :]), "c5", writes=[("cst", 2 + i)])
    chp = cx.sb([128, 2, 38], F32)
    for c in range(2):
        P.op("tensor", lambda e, c=c: e.transpose(out=pmisc[:, 64 + c * 64:64 + c * 64 + 38], in_=cst[:, c * 128:(c + 1) * 128], identity=idf[0:38, 0:38]),
             reads=[("cst", i) for i in range(6)] + ["idf"], writes=["pmisc"], mode=64)
        P.op("vector", lambda e, c=c: e.tensor_copy(out=chp[:, c, :], in_=pmisc[:, 64 + c * 64:64 + c * 64 + 38]), reads=["pmisc"], writes=[("chp", c)])
    chk = [("chp", 0), ("chp", 1)]
    pwf = cx.sb([128, 2, 128], F32)
    pwbd = cx.sb([128, 2, 128], BF16)
    P.op("vector", lambda e: e.memset(pwf[:], 0.0), writes=["pwf"])
    for gi in range(4):
        c, hh = gi // 2, gi % 2
        P.dma("sync", lambda e, gi=gi, c=c, hh=hh: e.dma_start(out=pwf[hh * 64:(hh + 1) * 64, c, hh * 64:(hh + 1) * 64], in_=T["pool_w"][l][gi]),
              "c6", reads=[], writes=["pwf"])
    P.op("vector", lambda e: e.tensor_copy(out=pwbd[:], in_=pwf[:]), reads=["pwf"], writes=["pwbd"])
    invc = cx.sb([128, 3, 2, 512], F32)
    P.dma("sync", lambda e: e.dma_start(out=invc[:], in_=T["invc"].rearrange("a p c s -> p a c s")), "c7", writes=["invc"])
    onesf = cx.sb([128, 128], F32)
    P.op("vector", lambda e: e.memset(onesf[:], 1.0), writes=["onesf"])

    zchr = cx.ring(2, [128, 12, ZW], F32, "zch")
    brr = cx.ring(2, [128, 6, 512], BF16, "brT")
    cu = cx.sb([128, 2, ZW], F32)
    ag = cx.sb([128, 2, ZW], F32)
    acc = cx.sb([128, 2, 512], F32)
    ysq = cx.sb([128, 2, 512], F32)
    mean = cx.sb([128, 512], F32)
    rstd = cx.sb([128, 512], F32)
    pC = cx.sb([128, ZW], F32)
    pD = cx.sb([128, ZW], F32)
    Wt = cx.sb([128, 2, 512], F32)
    ypool = cx.sb([128, 2, 512], BF16)
    pMr = cx.ring(4, [128, 512], F32, "pM", psum=True)
    zc_v = T["zc_d"].rearrange("(c p) s -> p c s", p=128)
    br_v = T["brT_d"].rearrange("(c p) s -> p c s", p=128)
    evs = []
    for blk in range(NB):
        zch, zk = zchr.next()
        brT, bk = brr.next()
        zks = [(zk, q4) for q4 in range(4)]
        lo = max(0, blk * 512 - HALO)
        hi = min(S, blk * 512 + 512 + HALO)
        off = lo - (blk * 512 - HALO)
        for q4 in range(4):
            P.dma("sync", lambda e, zch=zch, lo=lo, hi=hi, off=off, q4=q4: e.dma_start(out=zch[:, 3 * q4:3 * q4 + 3, off:off + hi - lo], in_=zc_v[:, 3 * q4:3 * q4 + 3, lo:hi]),
                  zk + "_%d" % q4, writes=[(zk, q4)])
        if blk == 0:
            P.op("gpsimd", lambda e, zch=zch: e.memset(zch[:, :, 0:HALO], 0.0), writes=[(zk, 4)])
            zks = zks + [(zk, 4)]
        if blk == NB - 1:
            P.op("gpsimd", lambda e, zch=zch: e.memset(zch[:, :, ZW - HALO:ZW], 0.0), writes=[(zk, 4)])
            zks = zks + [(zk, 4)]
        P.op("gpsimd", lambda e, zch=zch: e.tensor_tensor(out=cu[:], in0=zch[:, 2:4, :], in1=zch[:, 4:6, :], op=ALU.mult), reads=zks, writes=["cu"])
        for c in range(2):
            P.op("vector", lambda e, c=c: e.tensor_scalar(out=acc[:, c, :], in0=cu[:, c, 15:527], scalar1=chp[:, c, 31:32], scalar2=None, op0=ALU.mult),
                 reads=["cu"] + chk, writes=[("acc", c)])
            for k in (1, 2):
                P.op("vector", lambda e, c=c, k=k: e.scalar_tensor_tensor(out=acc[:, c, :], in0=cu[:, c, 15 + k:527 + k], scalar=chp[:, c, 31 + k:32 + k],
                                                                      in1=acc[:, c, :], op0=ALU.mult, op1=ALU.add),
                     reads=["cu", ("acc", c)] + chk, writes=[("acc", c)])
            P.op("vector", lambda e, c=c, zch=zch, brT=brT: e.tensor_tensor(out=brT[:, c, :], in0=acc[:, c, :], in1=zch[:, c, HALO:HALO + 512], op=ALU.mult),
                 reads=[("acc", c)] + zks, writes=[(bk, c)])
        P.op("gpsimd", lambda e, zch=zch: e.tensor_tensor(out=ag[:], in0=zch[:, 6:8, :], in1=zch[:, 8:10, :], op=ALU.mult), reads=zks, writes=["ag"])
        for c in range(2):
            P.op("vector", lambda e, c=c: e.tensor_scalar(out=acc[:, c, :], in0=ag[:, c, 1:513], scalar1=chp[:, c, 0:1], scalar2=chp[:, c, 34:35],
                                                      op0=ALU.mult, op1=ALU.add),
                 reads=["ag"] + chk, writes=[("acc", c)])
            for k in range(1, 31):
                P.op("vector", lambda e, c=c, k=k: e.scalar_tensor_tensor(out=acc[:, c, :], in0=ag[:, c, 1 + k:513 + k], scalar=chp[:, c, k:k + 1],
                                                                      in1=acc[:, c, :], op0=ALU.mult, op1=ALU.add),
                     reads=["ag", ("acc", c)] + chk, writes=[("acc", c)])
        P.op("gpsimd", lambda e: e.tensor_tensor(out=ysq[:], in0=acc[:], in1=acc[:], op=ALU.mult), reads=[("acc", 0), ("acc", 1)], writes=["ysq"])
        pS, pSk = pMr.next()
        pQ, pQk = pMr.next()
        for c in range(2):
            P.op("tensor", lambda e, c=c, pS=pS: e.matmul(pS[:], lhsT=onesf[:], rhs=acc[:, c, :], start=(c == 0), stop=(c == 1)),
                 reads=["onesf", ("acc", c)], writes=[pSk])
        for c in range(2):
            P.op("tensor", lambda e, c=c, pQ=pQ: e.matmul(pQ[:], lhsT=onesf[:], rhs=ysq[:, c, :], start=(c == 0), stop=(c == 1)),
                 reads=["onesf", "ysq"], writes=[pQk])
        P.op("scalar", lambda e, pS=pS: e.activation(out=mean[:], in_=pS[:], func=AF.Identity, scale=1.0 / 256), reads=[pSk], writes=["mean"])
        P.op("gpsimd", lambda e: e.tensor_tensor(out=rstd[:], in0=mean[:], in1=mean[:], op=ALU.mult), reads=["mean"], writes=["rstd"])
        P.op("vector", lambda e, pQ=pQ: e.scalar_tensor_tensor(out=rstd[:], in0=pQ[:], scalar=1.0 / 256, in1=rstd[:], op0=ALU.mult, op1=ALU.subtract),
             reads=[pQk, "rstd"], writes=["rstd"])
        P.op("vector", lambda e: e.tensor_scalar(out=rstd[:], in0=rstd[:], scalar1=LN_EPS, scalar2=None, op0=ALU.add), reads=["rstd"], writes=["rstd"])
        P.op("scalar", lambda e: e.activation(out=rstd[:], in_=rstd[:], func=AF.Sqrt), reads=["rstd"], writes=["rstd"])
        P.op("vector", lambda e: e.reciprocal(out=rstd[:], in_=rstd[:]), reads=["rstd"], writes=["rstd"])
        for c in range(2):
            P.op("vector", lambda e, c=c: e.tensor_tensor(out=acc[:, c, :], in0=acc[:, c, :], in1=mean[:], op=ALU.subtract),
                 reads=[("acc", c), "mean", pSk], writes=[("acc", c)])
            P.op("vector", lambda e, c=c: e.tensor_tensor(out=acc[:, c, :], in0=acc[:, c, :], in1=rstd[:], op=ALU.mult),
                 reads=[("acc", c), "rstd"], writes=[("acc", c)])
            P.op("scalar", lambda e, c=c, brT=brT: e.activation(out=brT[:, 2 + c, :], in_=acc[:, c, :], func=AF.Silu, scale=chp[:, c, 35:36], bias=chp[:, c, 36:37]),
                 reads=[("acc", c)] + chk, writes=[(bk, 2 + c)])
        P.op("gpsimd", lambda e, zch=zch: e.tensor_tensor(out=cu[:, :, 1:ZW], in0=zch[:, 10:12, 0:ZW - 1], in1=zch[:, 10:12, 1:ZW], op=ALU.add),
             reads=zks, writes=["cu"])
        P.op("gpsimd", lambda e: e.tensor_tensor(out=ag[:, :, 2:ZW - 1], in0=cu[:, :, 1:ZW - 2], in1=cu[:, :, 3:ZW], op=ALU.add),
             reads=["cu"], writes=["ag"])
        P.op("gpsimd", lambda e: e.tensor_tensor(out=pC[:, 4:ZW - 3], in0=ag[:, 1, 2:ZW - 5], in1=ag[:, 1, 6:ZW - 1], op=ALU.add),
             reads=["ag"], writes=["pC"])
        P.op("gpsimd", lambda e: e.tensor_tensor(out=pD[:, 8:ZW - 7], in0=pC[:, 4:ZW - 11], in1=pC[:, 12:ZW - 3], op=ALU.add),
             reads=["pC"], writes=["pD"])
        ty = 0 if blk == 0 else (2 if blk == NB - 1 else 1)
        srcs = [(cu, 0, 0), (ag, 0, 1), (pC, None, 0), (pD, None, 1)]
        for gi, (src, cidx, hh) in enumerate(srcs):
            c = gi // 2
            sl = slice(hh * 64, (hh + 1) * 64)
            sap = src[sl, cidx, HALO:HALO + 512] if cidx is not None else src[sl, HALO:HALO + 512]
            P.op("vector", lambda e, sap=sap, sl=sl, c=c, ty=ty: e.tensor_tensor(out=Wt[sl, c, :], in0=sap, in1=invc[sl, ty, c, :], op=ALU.mult),
                 reads=["cu", "ag", "pC", "pD", "invc"], writes=[("Wt", gi)])
        P.op("vector", lambda e, zch=zch: e.tensor_tensor(out=ypool[:], in0=Wt[:], in1=zch[:, 10:12, HALO:HALO + 512], op=ALU.subtract),
             reads=[("Wt", gi) for gi in range(4)] + zks, writes=["ypool"])
        for c in range(2):
            pP, pPk = pMr.next()
            P.op("tensor", lambda e, c=c, pP=pP: e.matmul(pP[:], lhsT=pwbd[:, c, :], rhs=ypool[:, c, :], start=True, stop=True),
                 reads=["pwbd", "ypool"], writes=[pPk])
            P.op("scalar", lambda e, c=c, pP=pP, brT=brT: e.activation(out=brT[:, 4 + c, :], in_=pP[:], func=AF.Identity, scale=chp[:, c, 37:38]),
                 reads=[pPk] + chk, writes=[(bk, 4 + c)])
        evs.append(P.dma("sync", lambda e, brT=brT, blk=blk: e.dma_start(out=br_v[:, 2:8, blk * 512:(blk + 1) * 512], in_=brT[:]), bk,
                         reads=[(bk, i) for i in range(6)]))
    P.flush(evs)
    cx.close()


def phase_B2b(nc, P, T, l, Dall, Wall):
    cx = Ctx(nc, P)
    idf, idb = load_consts(cx, P, T["ident"])
    wv = T["w_in"][l].rearrange("(c p) n -> p c n", p=128)
    wg = cx.sb([128, 8, 4096], BF16)
    for i in range(2):
        P.dma("gpsimd", lambda e, i=i: e.dma_start(out=wg[:, :, i * 2048:(i + 1) * 2048], in_=wv[:, :, 2048 + i * 2048:2048 + (i + 1) * 2048]),
              "wg%d" % i, writes=[("wg", i)])
    wgk = [("wg", 0), ("wg", 1)]
    wbr = cx.sb([128, 8, 1024], BF16)
    P.dma("gpsimd", lambda e: e.dma_start(out=wbr[:], in_=T["w_branch"][l].rearrange("g (c p) n -> p (g c) n", p=128)), "wbr", writes=["wbr"])
    wo = cx.sb([128, 8, 1024], BF16)
    P.dma("gpsimd", lambda e: e.dma_start(out=wo[:], in_=T["w_out"][l].rearrange("(c p) n -> p c n", p=128)), "wo", writes=["wo"])
    wr = cx.sb([128, 8, 32], BF16)
    P.dma("gpsimd", lambda e: e.dma_start(out=wr[:], in_=T["w_router"][l].rearrange("(c p) n -> p c n", p=128)), "wr", writes=["wr"])
    brb = cx.sb([128, 32], F32)
    P.dma("sync", lambda e: e.dma_start(out=brb[:], in_=T["b_router"][l].partition_broadcast(128)), "c3", writes=["brb"])
    gst = cx.sb([32, 128], F32)
    bg = cx.sb([128, 32], F32)
    P.dma("sync", lambda e: e.dma_start(out=gst[:], in_=T["b_in"][l][2048:6144].rearrange("(c p) -> c p", p=128)), "c4", writes=["gst"])
    pmisc = cx.ps([128, 512], F32)
    P.op("tensor", lambda e: e.transpose(out=pmisc[:, 0:32], in_=gst[:], identity=idf[0:32, 0:32]), reads=["gst", "idf"], writes=["pmisc"], mode=32)
    P.op("vector", lambda e: e.tensor_copy(out=bg[:], in_=pmisc[:, 0:32]), reads=["pmisc"], writes=["bg"])
    onesb = cx.sb([128, 128], BF16)
    P.op("vector", lambda e: e.memset(onesb[:], 1.0), writes=["onesb"])
    Uf = cx.sb([128, 128], F32)
    Ub = cx.sb([128, 128], BF16)
    P.dma("sync", lambda e: e.dma_start(out=Uf[:], in_=T["utri"][:, :]), "c8", writes=["Uf"])
    P.op("vector", lambda e: e.tensor_copy(out=Ub[:], in_=Uf[:]), reads=["Uf"], writes=["Ub"])
    iotaC = cx.sb([128, 32], F32)
    P.dma("sync", lambda e: e.dma_start(out=iotaC[:], in_=T["iotac"][:, :]), "c9", writes=["iotaC"])
    cnt = cx.sb([128, 32], F32)
    P.op("vector", lambda e: e.memset(cnt[:], 0.0), writes=["cnt"])
    gt, bt = load_ln_params(P, cx, T["ln1_g"][l], T["ln1_b"][l])

    hTr = cx.ring(2, [128, 8, 512], BF16, "hTb")
    brr = cx.ring(2, [128, 8, 512], BF16, "brT")
    gtr = cx.ring(2, [128, 512], BF16, "gate")
    tmr = cx.ring(2, [128, 512], F32, "tmp")
    acr = cx.ring(2, [128, 512], F32, "macc")
    mT = cx.sb([128, 8, 512], BF16)
    pGr = cx.ring(2, [128, 512], F32, "pG", psum=True)
    pJr = cx.ring(2, [128, 512], F32, "pJ", psum=True)
    pMr = cx.ring(2, [128, 512], F32, "pM", psum=True)
    pTr = cx.ring(1, [128, 8, 128], BF16, "pT8", psum=True)
    htr = cx.ring(2, [128, 1024], F32, "ht")
    ofr = cx.ring(2, [128, 1024], F32, "of")
    obr = cx.ring(2, [128, 1024], BF16, "ob")
    h1Tr = cx.ring(2, [128, 8, 128], BF16, "h1T")
    smr = ln_scratch(cx, 1)
    lgr = cx.ring(2, [128, 32], F32, "lg")
    t8r = cx.ring(2, [128, 8], F32, "t8")
    mkr = cx.ring(2, [128, 32], BF16, "mk")
    dfr = cx.ring(2, [128, 32], F32, "df")
    slr = cx.ring(2, [128, 32], F32, "sl")
    s1r = cx.ring(2, [128, 4], F32, "s1")
    dkr = cx.ring(2, [128, 4], F32, "dk")
    hT_v = T["hT_d"].rearrange("(c p) s -> p c s", p=128)
    br_v = T["brT_d"].rearrange("(c p) s -> p c s", p=128)
    Xe = T["Xe_d"]
    evs = []
    for blk in range(NB):
        hT, hTk = hTr.next()
        P.dma("sync", lambda e, hT=hT, blk=blk: e.dma_start(out=hT[:], in_=hT_v[:, :, blk * 512:(blk + 1) * 512]), hTk, writes=[hTk])
        brT, bk = brr.next()
        P.dma("sync", lambda e, brT=brT, blk=blk: e.dma_start(out=brT[:], in_=br_v[:, :, blk * 512:(blk + 1) * 512]), bk, writes=[bk])
        for oc in range(8):
            ma, mak = acr.next()
            for g in range(4):
                pG, pGk = pGr.next()
                for kc in range(8):
                    P.op("tensor", lambda e, pG=pG, kc=kc, g=g, oc=oc, hT=hT: e.matmul(
                        pG[:], lhsT=wg[:, kc, g * 1024 + oc * 128:g * 1024 + (oc + 1) * 128], rhs=hT[:, kc, :], start=(kc == 0), stop=(kc == 7)),
                        reads=[hTk] + wgk, writes=[pGk])
                gate, gtk = gtr.next()
                P.op("scalar", lambda e, gate=gate, pG=pG, g=g, oc=oc: e.activation(out=gate[:], in_=pG[:], func=AF.Sigmoid, bias=bg[:, g * 8 + oc:g * 8 + oc + 1]),
                     reads=[pGk, "bg"], writes=[gtk])
                pJ, pJk = pJr.next()
                for c in range(2):
                    P.op("tensor", lambda e, pJ=pJ, c=c, g=g, oc=oc, brT=brT: e.matmul(
                        pJ[:], lhsT=wbr[:, g * 2 + c, oc * 128:(oc + 1) * 128], rhs=brT[:, g * 2 + c, :], start=(c == 0), stop=(c == 1)),
                        reads=["wbr", bk], writes=[pJk])
                if g == 0:
                    P.op("vector", lambda e, ma=ma, gate=gate, pJ=pJ: e.tensor_tensor(out=ma[:], in0=gate[:], in1=pJ[:], op=ALU.mult),
                         reads=[gtk, pJk], writes=[mak])
                else:
                    tm, tmk = tmr.next()
                    P.op("vector", lambda e, tm=tm, gate=gate, pJ=pJ: e.tensor_tensor(out=tm[:], in0=gate[:], in1=pJ[:], op=ALU.mult),
                         reads=[gtk, pJk], writes=[tmk])
                    if g < 3:
                        P.op("gpsimd", lambda e, ma=ma, tm=tm: e.tensor_tensor(out=ma[:], in0=ma[:], in1=tm[:], op=ALU.add), reads=[mak, tmk], writes=[mak])
                    else:
                        P.op("gpsimd", lambda e, ma=ma, tm=tm, oc=oc: e.tensor_tensor(out=mT[:, oc, :], in0=ma[:], in1=tm[:], op=ALU.add),
                             reads=[mak, tmk], writes=[("mT", oc)])
        for tt in range(4):
            t = blk * 4 + tt
            ht, htk = htr.next()
            P.dma("sync", lambda e, ht=ht, t=t: e.dma_start(out=ht[:], in_=T["h_d"][t * 128:(t + 1) * 128, :]), htk, writes=[htk])
            for hf in range(2):
                pM, pMk = pMr.next()
                for kc in range(8):
                    P.op("tensor", lambda e, pM=pM, kc=kc, tt=tt, hf=hf: e.matmul(
                        pM[:], lhsT=mT[:, kc, tt * 128:(tt + 1) * 128], rhs=wo[:, kc, hf * 512:(hf + 1) * 512], start=(kc == 0), stop=(kc == 7)),
                        reads=[("mT", kc), "wo"], writes=[pMk])
                P.op("vector", lambda e, ht=ht, pM=pM, hf=hf: e.scalar_tensor_tensor(
                    out=ht[:, hf * 512:(hf + 1) * 512], in0=ht[:, hf * 512:(hf + 1) * 512], scalar=DN_ALPHA, in1=pM[:], op0=ALU.mult, op1=ALU.add),
                    reads=[htk, pMk], writes=[htk])
            of, ofk = ofr.next()
            ob, obk = obr.next()
            ln_core(P, cx, ht, htk, gt, bt, of, ofk, ob, obk, smr.next())
            evs.append(P.dma("sync", lambda e, of=of, t=t: e.dma_start(out=T["h1_d"][t * 128:(t + 1) * 128, :], in_=of[:]), ofk, reads=[ofk]))
            pT, pTk = pTr.next()
            for c in range(8):
                P.op("tensor", lambda e, c=c, pT=pT, ob=ob: e.transpose(out=pT[:, c, :], in_=ob[:, c * 128:(c + 1) * 128], identity=idb[:]),
                     reads=[obk, "idb"], writes=[pTk + "_%d" % c])
            h1T, h1Tk = h1Tr.next()
            P.op("vector", lambda e, h1T=h1T, pT=pT: e.tensor_copy(out=h1T[:], in_=pT[:]), reads=[pTk + "_%d" % c for c in range(8)], writes=[h1Tk])
            for kc in range(8):
                P.op("tensor", lambda e, kc=kc, h1T=h1T: e.matmul(pmisc[:, 0:32], lhsT=h1T[:, kc, :], rhs=wr[:, kc, :], start=(kc == 0), stop=(kc == 7)),
                     reads=[h1Tk, "wr"], writes=["pmisc"])
            lg, lgk = lgr.next()
            P.op("vector", lambda e, lg=lg: e.tensor_tensor(out=lg[:], in0=pmisc[:, 0:32], in1=brb[:], op=ALU.add), reads=["pmisc", "brb"], writes=[lgk])
            t8, t8k = t8r.next()
            P.op("vector", lambda e, t8=t8, lg=lg: e.max(out=t8[:], in_=lg[:]), reads=[lgk], writes=[t8k])
            mk, mkk = mkr.next()
            P.op("vector", lambda e, mk=mk, lg=lg, t8=t8: e.tensor_scalar(out=mk[:], in0=lg[:], scalar1=t8[:, 3:4], scalar2=None, op0=ALU.is_ge),
                 reads=[lgk, t8k], writes=[mkk])
            s1, s1k = s1r.next()
            P.op("vector", lambda e, s1=s1, t8=t8: e.tensor_scalar(out=s1[:, 0:1], in0=t8[:, 0:1], scalar1=-1.0, scalar2=None, op0=ALU.mult),
                 reads=[t8k], writes=[s1k + "n"])
            P.op("scalar", lambda e, s1=s1, t8=t8, t=t: e.activation(out=Wall[:, t, :], in_=t8[:, 0:4], func=AF.Exp, bias=s1[:, 0:1], accum_out=s1[:, 1:2]),
                 reads=[t8k, s1k + "n"], writes=[("Wall", t), s1k + "s"])
            P.op("vector", lambda e, s1=s1: e.reciprocal(out=s1[:, 2:3], in_=s1[:, 1:2]), reads=[s1k + "s"], writes=[s1k + "r"])
            P.op("vector", lambda e, s1=s1, t=t: e.tensor_scalar(out=Wall[:, t, :], in0=Wall[:, t, :], scalar1=s1[:, 2:3], scalar2=None, op0=ALU.mult),
                 reads=[("Wall", t), s1k + "r"], writes=[("Wall", t)])
            P.op("tensor", lambda e, mk=mk: e.matmul(pmisc[:, 64:96], lhsT=Ub[:], rhs=mk[:], start=True, stop=True), reads=["Ub", mkk], writes=["pmisc"])
            P.op("tensor", lambda e, mk=mk: e.matmul(pmisc[:, 128:160], lhsT=onesb[:], rhs=mk[:], start=True, stop=True), reads=["onesb", mkk], writes=["pmisc"])
            df, dfk = dfr.next()
            P.op("vector", lambda e, df=df: e.tensor_tensor(out=df[:], in0=pmisc[:, 64:96], in1=cnt[:], op=ALU.add), reads=["pmisc", "cnt"], writes=[dfk])
            P.op("vector", lambda e: e.tensor_tensor(out=cnt[:], in0=pmisc[:, 128:160], in1=cnt[:], op=ALU.add), reads=["pmisc", "cnt"], writes=["cnt"])
            sl, slk = slr.next()
            P.op("vector", lambda e, sl=sl, df=df: e.tensor_scalar(out=sl[:], in0=df[:], scalar1=float(CAP), scalar2=1.0e6, op0=ALU.is_ge, op1=ALU.mult),
                 reads=[dfk], writes=[slk])
            P.op("vector", lambda e, sl=sl, df=df: e.tensor_tensor(out=df[:], in0=df[:], in1=sl[:], op=ALU.add), reads=[dfk, slk], writes=[dfk])
            P.op("vector", lambda e, df=df: e.tensor_tensor(out=df[:], in0=df[:], in1=iotaC[:], op=ALU.add), reads=[dfk, "iotaC"], writes=[dfk])
            dk, dkk = dkr.next()
            for k in range(4):
                P.op("vector", lambda e, sl=sl, lg=lg, t8=t8, k=k: e.tensor_scalar(out=sl[:], in0=lg[:], scalar1=t8[:, k:k + 1], scalar2=None, op0=ALU.is_equal),
                     reads=[lgk, t8k, dfk], writes=[slk])
                P.op("vector", lambda e, sl=sl, df=df: e.tensor_tensor(out=sl[:], in0=sl[:], in1=df[:], op=ALU.mult), reads=[slk, dfk], writes=[slk])
                P.op("vector", lambda e, sl=sl, dk=dk, k=k: e.tensor_reduce(out=dk[:, k:k + 1], in_=sl[:], axis=AX.X, op=ALU.add), reads=[slk], writes=[(dkk, k)])
            P.op("vector", lambda e, dk=dk, t=t: e.tensor_copy(out=Dall[:, t, :], in_=dk[:]), reads=[(dkk, k) for k in range(4)], writes=[("Dall", t)])
            for k in range(4):
                evs.append(P.dma("gpsimd", lambda e, ob=ob, t=t, k=k: e.indirect_dma_start(
                    out=Xe[:, :], out_offset=bass.IndirectOffsetOnAxis(ap=Dall[:, t, k:k + 1], axis=0), in_=ob[:], in_offset=None,
                    bounds_check=bc_reg(e, P), oob_is_err=False), obk + "sc", reads=[obk, ("Dall", t)]))
    P.flush(evs)
    cx.close()


def phase_C(nc, P, T, l):
    cx = Ctx(nc, P)
    idf, idb = load_consts(cx, P, T["ident"])
    gst = cx.sb([32, 2048], F32)
    bgu = cx.sb([128, 16, 32], F32)
    P.dma("sync", lambda e: e.dma_start(out=gst[:], in_=T["b_gate_up"][l]), "c1", writes=["gst"])
    pmisc = cx.ps([128, 16, 32], F32)
    for c in range(16):
        P.op("tensor", lambda e, c=c: e.transpose(out=pmisc[:, c, :], in_=gst[:, c * 128:(c + 1) * 128], identity=idf[0:32, 0:32]),
             reads=["gst", "idf"], writes=["pmisc"], mode=32)
    P.op("vector", lambda e: e.tensor_copy(out=bgu[:], in_=pmisc[:]), reads=["pmisc"], writes=["bgu"])
    wgr = cx.ring(2, [128, 8, 2048], BF16, "wgu")
    wdr = cx.ring(2, [128, 8, 1024], BF16, "wdn")
    bdr = cx.ring(2, [128, 1024], F32, "bd")
    xr = cx.ring(4, [128, 1024], BF16, "xs")
    XTr = cx.ring(2, [128, 8, 512], BF16, "XT")
    aTr = cx.ring(2, [128, 8, 512], BF16, "actT")
    pTr = cx.ring(1, [128, 8, 128], BF16, "pT8", psum=True)
    pGr = cx.ring(2, [128, 512], F32, "pG", psum=True)
    pLr = cx.ring(2, [128, 512], F32, "pL", psum=True)
    pYr = cx.ring(2, [128, 512], F32, "pY", psum=True)
    ggr = cx.ring(3, [128, 512], F32, "gg")
    sgr = cx.ring(3, [128, 512], F32, "sg")
    llr = cx.ring(3, [128, 512], F32, "ll")
    yr = cx.ring(2, [128, 1024], F32, "y")
    Xe, Ye = T["Xe_d"], T["Ye_d"]
    evs = []

    def load_w(e_i):
        wgu, wguk = wgr.next()
        wdn, wdnk = wdr.next()
        bd, bdk = bdr.next()
        src = T["w_gate_up"][l][e_i].rearrange("(c p) n -> p c n", p=128)
        for hf in range(2):
            P.dma("gpsimd", lambda e, wgu=wgu, src=src, hf=hf: e.dma_start(out=wgu[:, 4 * hf:4 * hf + 4, :], in_=src[:, 4 * hf:4 * hf + 4, :]),
                  wguk + "_%d" % hf, writes=[(wguk, hf)])
        P.dma("gpsimd", lambda e, wdn=wdn, e_i=e_i: e.dma_start(out=wdn[:], in_=T["w_down"][l][e_i].rearrange("(c p) n -> p c n", p=128)), wdnk, writes=[wdnk])
        P.dma("sync", lambda e, bd=bd, e_i=e_i: e.dma_start(out=bd[:], in_=T["b_down"][l][e_i].partition_broadcast(128)), bdk, writes=[bdk])
        return (wgu, wguk, wdn, wdnk, bd, bdk)

    NBLK = NE * (CAP // 512)
    BPE = CAP // 512
    W = {}
    W[0] = load_w(0)
    blkst = {}

    def PREP(b):
        base = b * 512
        XT, XTk = XTr.next()
        for st in range(4):
            xs, xsk = xr.next()
            P.dma("sync", lambda e, xs=xs, base=base, st=st: e.dma_start(out=xs[:], in_=Xe[base + st * 128:base + (st + 1) * 128, :]), xsk, writes=[xsk])
            pT, pTk = pTr.next()
            for c in range(8):
                P.op("tensor", lambda e, c=c, pT=pT, xs=xs: e.transpose(out=pT[:, c, :], in_=xs[:, c * 128:(c + 1) * 128], identity=idb[:]),
                     reads=[xsk, "idb"], writes=[pTk + "_%d" % c])
            P.op("vector", lambda e, XT=XT, pT=pT, st=st: e.tensor_copy(out=XT[:, :, st * 128:(st + 1) * 128], in_=pT[:]),
                 reads=[pTk + "_%d" % c for c in range(8)], writes=[(XTk, st)])
        aT, aTk = aTr.next()
        blkst[b] = dict(XT=XT, XTk=XTk, aT=aT, aTk=aTk, pend=None)

    def GU(b, fcs):
        ex = b // BPE
        if b % BPE == 0 and fcs[0] == 2 and ex + 1 < NE:
            W[ex + 1] = load_w(ex + 1)
        wgu, wguk, wdn, wdnk, bd, bdk = W[ex]
        wk = [(wguk, 0), (wguk, 1)]
        st_ = blkst[b]
        XT, XTk, aT, aTk = st_["XT"], st_["XTk"], st_["aT"], st_["aTk"]
        XTks = [(XTk, st) for st in range(4)]
        for fc in fcs:
            pG, pGk = pGr.next()
            pL, pLk = pLr.next()
            for kc in range(8):
                P.op("tensor", lambda e, pG=pG, kc=kc, fc=fc, XT=XT, wgu=wgu: e.matmul(pG[:], lhsT=wgu[:, kc, fc * 128:(fc + 1) * 128], rhs=XT[:, kc, :],
                                                                               start=(kc == 0), stop=(kc == 7)),
                     reads=XTks + wk, writes=[pGk])
            for kc in range(8):
                P.op("tensor", lambda e, pL=pL, kc=kc, fc=fc, XT=XT, wgu=wgu: e.matmul(pL[:], lhsT=wgu[:, kc, 1024 + fc * 128:1024 + (fc + 1) * 128], rhs=XT[:, kc, :],
                                                                               start=(kc == 0), stop=(kc == 7)),
                     reads=XTks + wk, writes=[pLk])
            gg, ggk = ggr.next()
            sg, sgk = sgr.next()
            ll, llk = llr.next()
            P.op("scalar", lambda e, ll=ll, pL=pL, fc=fc, ex=ex: e.activation(out=ll[:], in_=pL[:], func=AF.Identity, bias=bgu[:, 8 + fc, ex:ex + 1]),
                 reads=[pLk, "bgu"], writes=[llk])
            P.op("vector", lambda e, gg=gg, pG=pG, fc=fc, ex=ex: e.tensor_scalar(out=gg[:], in0=pG[:], scalar1=bgu[:, fc, ex:ex + 1], scalar2=7.0, op0=ALU.add, op1=ALU.min),
                 reads=[pGk, "bgu"], writes=[ggk])
            P.op("scalar", lambda e, sg=sg, gg=gg: e.activation(out=sg[:], in_=gg[:], func=AF.Sigmoid, scale=1.702), reads=[ggk], writes=[sgk])
            P.op("gpsimd", lambda e, ll=ll: e.tensor_scalar(out=ll[:], in0=ll[:], scalar1=7.0, scalar2=-7.0, op0=ALU.min, op1=ALU.max), reads=[llk], writes=[llk])
            if st_["pend"] is not None:
                st_["pend"]()

            def fin(gg=gg, ggk=ggk, sg=sg, sgk=sgk, ll=ll, llk=llk, fc=fc, aT=aT, aTk=aTk):
                P.op("gpsimd", lambda e: e.tensor_tensor(out=gg[:], in0=gg[:], in1=sg[:], op=ALU.mult), reads=[ggk, sgk], writes=[ggk])
                P.op("vector", lambda e: e.scalar_tensor_tensor(out=aT[:, fc, :], in0=ll[:], scalar=1.0, in1=gg[:], op0=ALU.add, op1=ALU.mult),
                     reads=[ggk, llk], writes=[(aTk, fc)])
            st_["pend"] = fin
        if fcs[-1] == 7:
            st_["pend"]()
            st_["pend"] = None

    def DOWN(b):
        ex = b // BPE
        base = b * 512
        wgu, wguk, wdn, wdnk, bd, bdk = W[ex]
        st_ = blkst.pop(b)
        aT, aTk = st_["aT"], st_["aTk"]
        aTks = [(aTk, fc) for fc in range(8)]
        for st in range(4):
            y, yk = yr.next()
            for hf in range(2):
                pY, pYk = pYr.next()
                for fc in range(8):
                    P.op("tensor", lambda e, pY=pY, fc=fc, st=st, hf=hf, aT=aT, wdn=wdn: e.matmul(
                        pY[:], lhsT=aT[:, fc, st * 128:(st + 1) * 128], rhs=wdn[:, fc, hf * 512:(hf + 1) * 512], start=(fc == 0), stop=(fc == 7)),
                        reads=aTks + [wdnk], writes=[pYk])
                if hf == 0:
                    P.op("vector", lambda e, y=y, pY=pY, hf=hf, bd=bd: e.tensor_tensor(out=y[:, hf * 512:(hf + 1) * 512], in0=pY[:], in1=bd[:, hf * 512:(hf + 1) * 512], op=ALU.add),
                         reads=[pYk, bdk], writes=[(yk, hf)])
                else:
                    P.op("scalar", lambda e, y=y, pY=pY, hf=hf: e.activation(out=y[:, hf * 512:(hf + 1) * 512], in_=pY[:], func=AF.Identity),
                         reads=[pYk], writes=[(yk, "t")])
                    P.op("gpsimd", lambda e, y=y, hf=hf, bd=bd: e.tensor_tensor(out=y[:, hf * 512:(hf + 1) * 512], in0=y[:, hf * 512:(hf + 1) * 512], in1=bd[:, hf * 512:(hf + 1) * 512], op=ALU.add),
                         reads=[(yk, "t"), bdk], writes=[(yk, hf)])
            evs.append(P.dma("sync", lambda e, y=y, base=base, st=st: e.dma_start(out=Ye[base + st * 128:base + (st + 1) * 128, :], in_=y[:]), yk,
                             reads=[(yk, 0), (yk, 1)]))

    PREP(0)
    for b in range(NBLK + 1):
        if b < NBLK:
            GU(b, [0, 1])
        if b >= 1:
            DOWN(b - 1)
        if b < NBLK:
            GU(b, [2, 3])
            if b + 1 < NBLK:
                PREP(b + 1)
            GU(b, [4, 5, 6, 7])
    P.flush(evs)
    cx.close()


def phase_D(nc, P, T, l, Dall, Wall, last):
    cx = Ctx(nc, P)
    idf, idb = load_consts(cx, P, T["ident"])
    gt, bt = load_ln_params(P, cx, T["ln2_g"][l], T["ln2_b"][l])
    ykr = [cx.ring(2, [128, 1024], F32, "yk%d_" % k) for k in range(4)]
    htr = cx.ring(2, [128, 1024], F32, "ht")
    rr = cx.ring(2, [128, 1024], F32, "r")
    ofr = cx.ring(2, [128, 1024], F32, "of")
    obr = cx.ring(2, [128, 1024], BF16, "ob")
    pTr = cx.ring(2, [128, 8, 128], BF16, "pT", psum=True)
    hTr = cx.ring(2, [128, 8, 128], BF16, "hT")
    smr = ln_scratch(cx)
    Ye = T["Ye_d"]
    evs = []
    for t in range(NT):
        ys = []
        for k in range(4):
            yk_, ykk = ykr[k].next()
            P.op("gpsimd", lambda e, yk_=yk_: e.memset(yk_[:], 0.0), writes=[ykk])
            P.dma("gpsimd", lambda e, yk_=yk_, t=t, k=k: e.indirect_dma_start(
                out=yk_[:], out_offset=None, in_=Ye[:, :], in_offset=bass.IndirectOffsetOnAxis(ap=Dall[:, t, k:k + 1], axis=0),
                bounds_check=bc_reg(e, P), oob_is_err=False), ykk, writes=[ykk])
            ys.append((yk_, ykk))
        ht, htk = htr.next()
        P.dma("sync", lambda e, ht=ht, t=t: e.dma_start(out=ht[:], in_=T["h1_d"][t * 128:(t + 1) * 128, :]), htk, writes=[htk])
        r, rk = rr.next()
        P.op("vector", lambda e, r=r, ht=ht: e.tensor_scalar(out=r[:], in0=ht[:], scalar1=DN_ALPHA, scalar2=None, op0=ALU.mult), reads=[htk], writes=[rk])
        for k in range(4):
            yk_, ykk = ys[k]
            P.op("vector", lambda e, r=r, yk_=yk_, t=t, k=k: e.scalar_tensor_tensor(out=r[:], in0=yk_[:], scalar=Wall[:, t, k:k + 1], in1=r[:], op0=ALU.mult, op1=ALU.add),
                 reads=[ykk, rk], writes=[rk])
        of, ofk = ofr.next()
        ob, obk = obr.next()
        ln_core(P, cx, r, rk, gt, bt, of, ofk, None if last else ob, obk, smr.next())
        dst = T["out"] if last else T["h_d"]
        evs.append(P.dma("sync", lambda e, of=of, t=t, dst=dst: e.dma_start(out=dst[t * 128:(t + 1) * 128, :], in_=of[:]), ofk, reads=[ofk]))
        if not last:
            pT, pTk = pTr.next()
            hTt, hTk = hTr.next()
            evs.append(transpose_store_hT(P, ob, obk, pT, pTk, hTt, hTk, idb, T["hT_d"], t, hTk))
    P.flush(evs)
    cx.close()


PARAM_SPECS = [
    ("ln_in_g", (1024,)), ("ln_in_b", (1024,)), ("w_in", (2, 1024, 6144)), ("b_in", (2, 6144)),
    ("q_norm_g", (2, 64)), ("k_norm_g", (2, 64)), ("sc_conv_w", (2, 3, 256)), ("cf_conv_w", (2, 31, 256)),
    ("cf_conv_b", (2, 256)), ("cf_ln_g", (2, 256)), ("cf_ln_b", (2, 256)), ("pool_w", (2, 4, 64, 64)),
    ("pool_scale", (2, 256)), ("w_branch", (2, 4, 256, 1024)), ("w_out", (2, 1024, 1024)),
    ("ln1_g", (2, 1024)), ("ln1_b", (2, 1024)), ("w_router", (2, 1024, 32)), ("b_router", (2, 32)),
    ("w_gate_up", (2, 32, 1024, 2048)), ("b_gate_up", (2, 32, 2048)), ("w_down", (2, 32, 1024, 1024)),
    ("b_down", (2, 32, 1024)), ("ln2_g", (2, 1024)), ("ln2_b", (2, 1024)),
]
CONST_SPECS = [("ident", (128, 128)), ("cos6", (S, 384)), ("sin6", (S, 384)), ("invc", (3, 128, 2, 512)),
               ("utri", (128, 128)), ("iotac", (128, 32))]
SCRATCH = [("h_d", (S, D), F32), ("hT_d", (D, S), BF16), ("zc_d", (1536, S), F32), ("brT_d", (1024, S), BF16),
           ("h1_d", (S, D), F32), ("Xe_d", (NSLOT, D), BF16), ("Ye_d", (NSLOT, D), F32)]


def build_program(upto=99, debug=(), small=False, astage=9):
    global A_STAGE
    A_STAGE = astage
    nc = bass.Bass("TRN2", target_bir_lowering=False)
    T = {}
    T["x"] = nc.dram_tensor("x", [S, D], F32, kind="ExternalInput").ap()
    for nm, shp in PARAM_SPECS + CONST_SPECS:
        if small and nm in ("w_gate_up", "w_down"):
            shp = (2, 1) + tuple(shp[2:])
        T[nm] = nc.dram_tensor(nm, list(shp), F32, kind="ExternalInput").ap()
    T["out"] = nc.dram_tensor("out", [S, D], F32, kind="ExternalOutput").ap()
    for nm, shp, dt in SCRATCH:
        T[nm] = nc.dram_tensor(nm, list(shp), dt, kind="ExternalOutput" if nm in debug else "Internal").ap()
    P = Prog(nc)
    with ExitStack() as top:
        Dall = top.enter_context(nc.sbuf_tensor("Dall", [128, NT, 4], I32))
        Wall = top.enter_context(nc.sbuf_tensor("Wall", [128, NT, 4], F32))
        ph = 0
        phase_ln_in(nc, P, T)
        for l in range(DEPTH):
            if ph >= upto:
                break
            with ExitStack() as att:
                QTa = att.enter_context(nc.sbuf_tensor("QT%d" % l, [128, 4, S], BF16))
                QTb = None
                KT = att.enter_context(nc.sbuf_tensor("KT%d" % l, [128, S], BF16))
                Vaug = att.enter_context(nc.sbuf_tensor("Vaug%d" % l, [128, NT, 2, 66], BF16))
                phase_A(nc, P, T, l, QTa, QTb, KT, Vaug)
                ph += 1
                if ph >= upto:
                    break
                phase_B1(nc, P, T, l, QTa, QTb, KT, Vaug)
                ph += 1
            if ph >= upto:
                break
            phase_B2a(nc, P, T, l)
            ph += 1
            if ph >= upto:
                break
            phase_B2b(nc, P, T, l, Dall, Wall)
            ph += 1
            if ph >= upto:
                break
            phase_C(nc, P, T, l)
            ph += 1
            if ph >= upto:
                break
            phase_D(nc, P, T, l, Dall, Wall, last=(l == DEPTH - 1))
            ph += 1
    return nc


def host_consts():
    c = {}
    c["ident"] = np.eye(128, dtype=np.float32)
    rows = S // 64
    row = np.repeat(np.arange(rows, dtype=np.float32), 64)
    col = np.tile(np.arange(64, dtype=np.float32), rows)
    inv = (np.float32(10000.0) ** (-np.arange(0, 32, 2, dtype=np.float32) / np.float32(32))).astype(np.float32)
    ar = (row[:, None] * inv).astype(np.float32)
    ac = (col[:, None] * inv).astype(np.float32)
    cr, sr, cc, sc = np.cos(ar), np.sin(ar), np.cos(ac), np.sin(ac)
    cos1 = np.concatenate([cr, cr, cc, cc], axis=1).astype(np.float32)
    sin1 = np.concatenate([-sr, sr, -sc, sc], axis=1).astype(np.float32)
    c["cos6"] = np.ascontiguousarray(np.tile(cos1, (1, 6)))
    c["sin6"] = np.ascontiguousarray(np.tile(sin1, (1, 6)))
    invc = np.zeros((3, 128, 2, 512), np.float32)
    wins = (2, 4, 8, 16)
    for ty, blk in enumerate((0, 1, NB - 1)):
        t = np.arange(blk * 512, blk * 512 + 512)
        for gi, w in enumerate(wins):
            lo = np.maximum(t - w // 2, 0)
            hi = np.minimum(t + (w - 1 - w // 2), S - 1)
            ic = (1.0 / (hi - lo + 1)).astype(np.float32)
            cidx, hh = gi // 2, gi % 2
            invc[ty, hh * 64:(hh + 1) * 64, cidx, :] = ic[None, :]
    c["invc"] = invc
    c["utri"] = np.triu(np.ones((128, 128), np.float32), k=1)
    c["iotac"] = np.tile((np.arange(32, dtype=np.float32) * CAP)[None, :], (128, 1)).astype(np.float32)
    return c


_CACHE = {}


def kernel(**inputs):
    if "nc" not in _CACHE:
        _CACHE["nc"] = build_program()
    nc = _CACHE["nc"]
    consts = host_consts()
    shared = {nm: np.ascontiguousarray(np.asarray(inputs[nm], dtype=np.float32)) for nm, _ in PARAM_SPECS}
    shared.update(consts)
    x = np.asarray(inputs["x"], dtype=np.float32)
    in_maps = []
    for b in range(8):
        m = dict(shared)
        m["x"] = np.ascontiguousarray(x[b])
        in_maps.append(m)
    res = run_bass_kernel_spmd(nc, in_maps, core_ids=list(range(8)))
    return np.stack([np.asarray(r["out"]) for r in res.results], axis=0).astype(np.float32)
```

```python
import math
from contextlib import ExitStack
import numpy as np
import concourse.bass as bass
import concourse.mybir as mybir
from concourse.bass_utils import run_bass_kernel_spmd

F32 = mybir.dt.float32
BF16 = mybir.dt.bfloat16
I32 = mybir.dt.int32
AF = mybir.ActivationFunctionType
ALU = mybir.AluOpType
AX = mybir.AxisListType

ENGS = ("sync", "scalar", "vector", "gpsimd", "tensor")
S = 8192
D = 1024
NT = 64
NB = 16
NE = 32
CAP = 1536
NSLOT = NE * CAP
DEPTH = 2
DN_ALPHA = (2.0 * DEPTH) ** 0.25
LN_EPS = 1e-5
HALO = 16
ZW = 512 + 2 * HALO


class _Probe:
    def __init__(self):
        self.name = None

    def __getattr__(self, nm):
        def f(*a, **k):
            self.name = nm
            return self
        return f


class Prog:
    def __init__(self, nc):
        self.nc = nc
        self.esem = {e: nc.alloc_semaphore("es_" + e) for e in ENGS}
        self.ebase = {e: 0 for e in ENGS}
        self.dsem = {}
        self.nflush = 0
        self._reset()

    def _reset(self):
        self.ops = {e: [] for e in ENGS}
        self.waited = {e: {} for e in ENGS}
        self.last_w = {}
        self.readers = {}
        self.signal = {e: set() for e in ENGS}
        self.pe_mode = None

    def _need(self, eng, ev, waits):
        if ev is None:
            return
        if ev[0] == "e" and ev[1] == eng and eng == "tensor":
            return
        key = (ev[0], ev[1])
        if self.waited[eng].get(key, -1) >= ev[2]:
            return
        waits[key] = max(waits.get(key, -1), ev[2])

    def _deps(self, eng, reads, writes, extra=()):
        waits = {}
        for k in reads:
            self._need(eng, self.last_w.get(k), waits)
        for k in writes:
            self._need(eng, self.last_w.get(k), waits)
            for ev in self.readers.get(k, ()):
                self._need(eng, ev, waits)
        for ev in extra:
            self._need(eng, ev, waits)
        out = []
        for key, v in waits.items():
            self.waited[eng][key] = v
            out.append((key[0], key[1], v))
            if key[0] == "e":
                self.signal[key[1]].add(v)
        return out

    def _commit(self, ev, reads, writes):
        for k in reads:
            self.readers.setdefault(k, []).append(ev)
        for k in writes:
            self.last_w[k] = ev
            self.readers[k] = []

    def op(self, eng, fn, reads=(), writes=(), extra=(), mode=128):
        waits = self._deps(eng, reads, writes, extra)
        idx = len(self.ops[eng])
        if eng == "tensor":
            pr = _Probe()
            fn(pr)
            mode = (pr.name, mode)
            if self.pe_mode is not None and self.pe_mode != mode and idx > 0:
                key = ("e", "tensor")
                if self.waited[eng].get(key, -1) < idx - 1:
                    self.waited[eng][key] = idx - 1
                    waits.append(("e", "tensor", idx - 1))
                    self.signal["tensor"].add(idx - 1)
            self.pe_mode = mode
        self.ops[eng].append(dict(fn=fn, waits=waits, dma=None))
        ev = ("e", eng, idx)
        self._commit(ev, reads, writes)
        return ev

    def dma(self, eng, fn, semkey, reads=(), writes=(), extra=()):
        waits = self._deps(eng, reads, writes, extra)
        sn = "ds_" + semkey
        if sn not in self.dsem:
            self.dsem[sn] = [self.nc.alloc_semaphore(sn), 0]
        st = self.dsem[sn]
        st[1] += 16
        self.ops[eng].append(dict(fn=fn, waits=waits, dma=(st[0], st[1])))
        ev = ("d", sn, st[1])
        self._commit(ev, reads, writes)
        return ev

    def wait(self, eng, evs):
        waits = self._deps(eng, (), (), evs)
        self.ops[eng].append(dict(fn=None, waits=waits, dma=None))

    def flush(self, final_events=()):
        nc = self.nc
        if final_events:
            self.wait("sync", final_events)
        val = {}
        for e in ENGS:
            v = self.ebase[e]
            m = {}
            for idx in sorted(self.signal[e]):
                v += 1
                m[idx] = v
            val[e] = m
            self.ebase[e] = v
        ops, esem, dsem = self.ops, self.esem, self.dsem

        def emit(e, name):
            for idx, o in enumerate(ops[name]):
                for kind, who, v in o["waits"]:
                    if kind == "e":
                        e.wait_ge(esem[who], val[who][v])
                    else:
                        e.wait_ge(dsem[who][0], v)
                if o["fn"] is None:
                    continue
                ins = o["fn"](e)
                if o["dma"] is not None:
                    ins.then_inc(o["dma"][0], 16)
                elif idx in val[name]:
                    ins.then_inc(esem[name], 1)

        with nc.Block() as block:
            @block.sync
            def _(e):
                emit(e, "sync")

            @block.scalar
            def _(e):
                emit(e, "scalar")

            @block.vector
            def _(e):
                emit(e, "vector")

            @block.gpsimd
            def _(e):
                emit(e, "gpsimd")

            @block.tensor
            def _(e):
                emit(e, "tensor")
        self.nflush += 1
        self._reset()


_REG = {}


def bc_reg(e, P):
    key = P.nflush
    if key not in _REG:
        _REG.clear()
        _REG[key] = e.to_reg(NSLOT - 1)
    return _REG[key]


class Ring:
    def __init__(self, items):
        self.items = items
        self.i = -1

    def next(self):
        self.i = (self.i + 1) % len(self.items)
        return self.items[self.i]


class Ctx:
    _uid = [0]

    def __init__(self, nc, P):
        self.nc, self.P = nc, P
        self.stack = ExitStack()
        self.n = 0
        Ctx._uid[0] += 1
        self.uid = Ctx._uid[0]

    def sb(self, shape, dt, name=None):
        self.n += 1
        return self.stack.enter_context(self.nc.sbuf_tensor(name or f"t{self.n}_{self.uid}", list(shape), dt))

    def ps(self, shape, dt, name=None):
        self.n += 1
        return self.stack.enter_context(self.nc.psum_tensor(name or f"p{self.n}_{self.uid}", list(shape), dt))

    def ring(self, n, shape, dt, key, psum=False):
        return Ring([((self.ps if psum else self.sb)(shape, dt), f"{key}{i}") for i in range(n)])

    def close(self):
        self.stack.close()


def load_consts(cx, P, ident_d):
    nc = cx.nc
    idf = cx.sb([128, 128], F32)
    idb = cx.sb([128, 128], BF16)
    P.dma("sync", lambda e: e.dma_start(out=idf[:], in_=ident_d[:, :]), "c0", writes=["idf"])
    P.op("vector", lambda e: e.tensor_copy(out=idb[:], in_=idf[:]), reads=["idf"], writes=["idb"])
    return idf, idb


def ln_core(P, cx, r, rk, gt, bt, of, ofk, ob, obk, sm, add_eng="gpsimd"):
    st, mv, rs, nb, xn, k = sm
    for c in range(2):
        P.op("vector", lambda e, c=c: e.bn_stats(out=st[:, c, :], in_=r[:, c * 512:(c + 1) * 512]),
             reads=[rk], writes=[k + "st%d" % c])
    P.op("vector", lambda e: e.bn_aggr(out=mv[:], in_=st[:].rearrange("p a b -> p (a b)")),
         reads=[k + "st0", k + "st1"], writes=[k + "mv"])
    P.op("vector", lambda e: e.tensor_scalar(out=rs[:], in0=mv[:, 1:2], scalar1=LN_EPS, scalar2=None, op0=ALU.add),
         reads=[k + "mv"], writes=[k + "rs"])
    P.op("scalar", lambda e: e.activation(out=rs[:], in_=rs[:], func=AF.Sqrt), reads=[k + "rs"], writes=[k + "rs"])
    P.op("vector", lambda e: e.reciprocal(out=rs[:], in_=rs[:]), reads=[k + "rs"], writes=[k + "rs"])
    P.op("vector", lambda e: e.tensor_scalar(out=nb[:], in0=mv[:, 0:1], scalar1=rs[:, 0:1], scalar2=-1.0,
                                             op0=ALU.mult, op1=ALU.mult),
         reads=[k + "mv", k + "rs"], writes=[k + "nb"])
    P.op("scalar", lambda e: e.activation(out=xn[:], in_=r[:], func=AF.Identity, scale=rs[:, 0:1], bias=nb[:, 0:1]),
         reads=[rk, k + "rs", k + "nb"], writes=[k + "xn"])
    P.op("vector", lambda e: e.tensor_tensor(out=xn[:], in0=xn[:], in1=gt[:], op=ALU.mult),
         reads=[k + "xn", "lng"], writes=[k + "xn"])
    P.op(add_eng, lambda e: e.tensor_tensor(out=of[:], in0=xn[:], in1=bt[:], op=ALU.add),
         reads=[k + "xn", "lnb"], writes=[ofk])
    if ob is not None:
        P.op("scalar", lambda e: e.copy(out=ob[:], in_=of[:]), reads=[ofk], writes=[obk])


def ln_scratch(cx, n=2):
    return Ring([(cx.sb([128, 2, 6], F32), cx.sb([128, 2], F32), cx.sb([128, 1], F32), cx.sb([128, 1], F32),
                  cx.sb([128, 1024], F32), f"ln{i}") for i in range(n)])


def load_ln_params(P, cx, g_ap, b_ap):
    gt = cx.sb([128, 1024], F32)
    bt = cx.sb([128, 1024], F32)
    P.dma("sync", lambda e: e.dma_start(out=gt[:], in_=g_ap.partition_broadcast(128)), "c1", writes=["lng"])
    P.dma("sync", lambda e: e.dma_start(out=bt[:], in_=b_ap.partition_broadcast(128)), "c2", writes=["lnb"])
    return gt, bt


def transpose_store_hT(P, hb, hbk, pT, pTk, hTt, hTk, idb, hT_d, t, semkey):
    for c in range(8):
        P.op("tensor", lambda e, c=c: e.transpose(out=pT[:, c, :], in_=hb[:, c * 128:(c + 1) * 128], identity=idb[:]),
             reads=[hbk, "idb"], writes=[pTk + "_%d" % c])
    P.op("vector", lambda e: e.tensor_copy(out=hTt[:], in_=pT[:]), reads=[pTk + "_%d" % c for c in range(8)],
         writes=[hTk])
    return P.dma("sync", lambda e: e.dma_start(
        out=hT_d.rearrange("(c p) s -> p c s", p=128)[:, :, t * 128:(t + 1) * 128], in_=hTt[:]),
        semkey, reads=[hTk])


def phase_ln_in(nc, P, T):
    cx = Ctx(nc, P)
    idf, idb = load_consts(cx, P, T["ident"])
    gt, bt = load_ln_params(P, cx, T["ln_in_g"], T["ln_in_b"])
    xr = cx.ring(3, [128, 1024], F32, "x")
    ofr = cx.ring(2, [128, 1024], F32, "of")
    obr = cx.ring(2, [128, 1024], BF16, "ob")
    pTr = cx.ring(2, [128, 8, 128], BF16, "pT", psum=True)
    hTr = cx.ring(2, [128, 8, 128], BF16, "hT")
    smr = ln_scratch(cx)
    evs = []
    pre = {}

    def prefetch(t):
        xt, xk = xr.next()
        P.dma("sync", lambda e, xt=xt, t=t: e.dma_start(out=xt[:], in_=T["x"][t * 128:(t + 1) * 128, :]), xk, writes=[xk])
        pre[t] = (xt, xk)
    prefetch(0)
    prefetch(1)
    for t in range(NT):
        if t + 2 < NT:
            prefetch(t + 2)
        xt, xk = pre.pop(t)
        of, ofk = ofr.next()
        ob, obk = obr.next()
        ln_core(P, cx, xt, xk, gt, bt, of, ofk, ob, obk, smr.next())
        evs.append(P.dma("sync", lambda e, of=of, t=t: e.dma_start(out=T["h_d"][t * 128:(t + 1) * 128, :], in_=of[:]),
                         ofk, reads=[ofk]))
        pT, pTk = pTr.next()
        hTt, hTk = hTr.next()
        evs.append(transpose_store_hT(P, ob, obk, pT, pTk, hTt, hTk, idb, T["hT_d"], t, hTk))
    P.flush(evs)
    cx.close()


A_STAGE = 9
QPERM = [0, 2, 1, 3]


def phase_A(nc, P, T, l, QTa, QTb, KT, Vaug):
    cx = Ctx(nc, P)
    idf, idb = load_consts(cx, P, T["ident"])
    w_in = T["w_in"][l]
    b_in = T["b_in"][l]
    wv = w_in.rearrange("(c p) n -> p c n", p=128)
    w1 = cx.sb([128, 8, 2048], BF16)
    for s in range(4):
        hs = QPERM[s]
        P.dma("gpsimd", lambda e, s=s, hs=hs: e.dma_start(out=w1[:, :, s * 64:(s + 1) * 64], in_=wv[:, :, hs * 64:(hs + 1) * 64]),
              "w1q", writes=[("w1", s)])
    P.dma("gpsimd", lambda e: e.dma_start(out=w1[:, :, 256:2048], in_=wv[:, :, 256:2048]), "w1r", writes=[("w1", 4)])
    w1k = [("w1", i) for i in range(5)]
    bq = cx.sb([128, 512], F32)
    for s in range(4):
        hs = QPERM[s]
        P.dma("sync", lambda e, s=s, hs=hs: e.dma_start(out=bq[:, s * 64:(s + 1) * 64],
                                                        in_=b_in[hs * 64:(hs + 1) * 64].partition_broadcast(128)),
              "c1", writes=[("bq", s)])
    P.dma("sync", lambda e: e.dma_start(out=bq[:, 256:512], in_=b_in[256:512].partition_broadcast(128)), "c2",
          writes=[("bq", 4)])
    bqk = [("bq", i) for i in range(5)]
    bst = cx.sb([12, 128], F32)
    bc = cx.sb([128, 12], F32)
    P.dma("sync", lambda e: e.dma_start(out=bst[:], in_=b_in[512:2048].rearrange("(c p) -> c p", p=128)), "c3", writes=["bst"])
    pB = cx.ps([128, 512], F32)
    P.op("tensor", lambda e: e.transpose(out=pB[:, 0:12], in_=bst[:], identity=idf[0:12, 0:12]), reads=["bst", "idf"], writes=["pB"], mode=32)
    P.op("vector", lambda e: e.tensor_copy(out=bc[:], in_=pB[:, 0:12]), reads=["pB"], writes=["bc"])
    gq = cx.sb([128, 384], F32)
    for s in range(6):
        src = T["q_norm_g"][l] if s < 4 else T["k_norm_g"][l]
        P.dma("sync", lambda e, s=s, src=src: e.dma_start(out=gq[:, s * 64:(s + 1) * 64], in_=src.partition_broadcast(128)),
              "c4" if s < 4 else "c5", writes=[("gq", s)])
    P.op("vector", lambda e: e.tensor_scalar(out=gq[:, 0:256], in0=gq[:, 0:256], scalar1=0.125, scalar2=None, op0=ALU.mult),
         reads=[("gq", s) for s in range(4)], writes=["gqs"])
    gqk = ["gqs", ("gq", 4), ("gq", 5)]
    P.op("gpsimd", lambda e: e.memset(Vaug[:, :, :, 64:66], 1.0), writes=["Vones"])
    for h in range(4):
        P.op("gpsimd", lambda e, h=h: e.memset(QTa[:, h, :], 0.0), writes=["QTz"])

    hTr = cx.ring(2, [128, 8, 512], BF16, "hTb")
    csr = cx.ring(2, [128, 2, 384], F32, "cs")
    pqr = cx.ring(2, [128, 512], F32, "pq", psum=True)
    pcr = cx.ring(2, [128, 512], F32, "pc", psum=True)
    pTr = cx.ring(2, [128, 8, 128], BF16, "pT3", psum=True)
    zbr = cx.ring(2, [128, 512], F32, "zb")
    sqr = cx.ring(2, [128, 384], F32, "sq")
    ssr = cx.ring(2, [128, 6], F32, "ss")
    xnr = cx.ring(2, [128, 384], F32, "xn")
    t1r = cx.ring(2, [128, 384], F32, "t1")
    t2r = cx.ring(2, [128, 384], F32, "t2")
    qbr = cx.ring(2, [128, 384], BF16, "qb")
    ztr = cx.ring(3, [128, 512], F32, "zt")
    hT_v = T["hT_d"].rearrange("(c p) s -> p c s", p=128)
    zc_v = T["zc_d"]
    evs = []
    for blk in range(NB if A_STAGE >= 1 else 0):
        hT, hTk = hTr.next()
        P.dma("sync", lambda e, hT=hT, blk=blk: e.dma_start(out=hT[:], in_=hT_v[:, :, blk * 512:(blk + 1) * 512]), hTk, writes=[hTk])
        for tt in range(4 if A_STAGE >= 2 else 0):
            t = blk * 4 + tt
            cs, csk = csr.next()
            P.dma("sync", lambda e, cs=cs, t=t: e.dma_start(out=cs[:, 0, :], in_=T["cos6"][t * 128:(t + 1) * 128, :]), csk + "a", writes=[csk + "a"])
            P.dma("sync", lambda e, cs=cs, t=t: e.dma_start(out=cs[:, 1, :], in_=T["sin6"][t * 128:(t + 1) * 128, :]), csk + "b", writes=[csk + "b"])
            pq, pqk = pqr.next()
            for kc in range(8):
                P.op("tensor", lambda e, pq=pq, hT=hT, kc=kc, tt=tt: e.matmul(pq[:], lhsT=hT[:, kc, tt * 128:(tt + 1) * 128], rhs=w1[:, kc, 0:512],
                                                                       start=(kc == 0), stop=(kc == 7)),
                     reads=[hTk] + w1k, writes=[pqk])
            zb, zbk = zbr.next()
            P.op("vector", lambda e, zb=zb, pq=pq: e.tensor_tensor(out=zb[:], in0=pq[:], in1=bq[:], op=ALU.add),
                 reads=[pqk] + bqk, writes=[zbk])
            if A_STAGE < 2.15:
                continue
            sq, sqk = sqr.next()
            P.op("gpsimd", lambda e, sq=sq, zb=zb: e.tensor_tensor(out=sq[:], in0=zb[:, 0:384], in1=zb[:, 0:384], op=ALU.mult),
                 reads=[zbk], writes=[sqk])
            ss, ssk = ssr.next()
            P.op("vector", lambda e, ss=ss, sq=sq: e.tensor_reduce(out=ss[:], in_=sq[:].rearrange("p (h d) -> p h d", d=64), axis=AX.X, op=ALU.add),
                 reads=[sqk], writes=[ssk])
            P.op("vector", lambda e, ss=ss: e.tensor_scalar(out=ss[:], in0=ss[:], scalar1=1.0 / 64, scalar2=1e-6, op0=ALU.mult, op1=ALU.add),
                 reads=[ssk], writes=[ssk])
            P.op("scalar", lambda e, ss=ss: e.activation(out=ss[:], in_=ss[:], func=AF.Sqrt), reads=[ssk], writes=[ssk])
            P.op("vector", lambda e, ss=ss: e.reciprocal(out=ss[:], in_=ss[:]), reads=[ssk], writes=[ssk])
            if A_STAGE < 2.25:
                continue
            xn, xnk = xnr.next()
            P.op("vector", lambda e, xn=xn, zb=zb, ss=ss: e.tensor_tensor(
                out=xn[:].rearrange("p (h d) -> p h d", d=64), in0=zb[:, 0:384].rearrange("p (h d) -> p h d", d=64),
                in1=ss[:, :].unsqueeze(2).to_broadcast([128, 6, 64]), op=ALU.mult), reads=[zbk, ssk], writes=[xnk])
            if A_STAGE < 2.35:
                continue
            P.op("gpsimd", lambda e, xn=xn: e.tensor_tensor(out=xn[:], in0=xn[:], in1=gq[:], op=ALU.mult), reads=[xnk] + gqk, writes=[xnk])
            if A_STAGE < 2.45:
                continue
            t1, t1k = t1r.next()
            P.op("vector", lambda e, t1=t1, xn=xn, cs=cs: e.tensor_tensor(out=t1[:], in0=xn[:], in1=cs[:, 0, :], op=ALU.mult),
                 reads=[xnk, csk + "a"], writes=[t1k])
            t2, t2k = t2r.next()
            xv = xn[:].rearrange("p (a b c) -> p a b c", b=2, c=16)
            sv = cs[:, 1, :].rearrange("p (a b c) -> p a b c", b=2, c=16)
            tv = t2[:].rearrange("p (a b c) -> p a b c", b=2, c=16)
            P.op("gpsimd", lambda e, xv=xv, sv=sv, tv=tv: e.tensor_tensor(out=tv[:, :, 0, :], in0=xv[:, :, 1, :], in1=sv[:, :, 0, :], op=ALU.mult),
                 reads=[xnk, csk + "b"], writes=[t2k + "x"])
            P.op("gpsimd", lambda e, xv=xv, sv=sv, tv=tv: e.tensor_tensor(out=tv[:, :, 1, :], in0=xv[:, :, 0, :], in1=sv[:, :, 1, :], op=ALU.mult),
                 reads=[xnk, csk + "b"], writes=[t2k + "y"])
            qb, qbk = qbr.next()
            P.op("vector", lambda e, qb=qb, t1=t1, t2=t2: e.tensor_tensor(out=qb[:], in0=t1[:], in1=t2[:], op=ALU.add),
                 reads=[t1k, t2k + "x", t2k + "y"], writes=[qbk])
            if A_STAGE < 2.55:
                continue
            pT, pTk = pTr.next()
            for c in range(3):
                P.op("tensor", lambda e, pT=pT, qb=qb, c=c: e.transpose(out=pT[:, c, :], in_=qb[:, c * 128:(c + 1) * 128], identity=idb[:]),
                     reads=[qbk, "idb"], writes=[pTk + "_%d" % c])
            for c in range(2):
                for g in range(2):
                    h = 2 * g + c
                    P.op("vector", lambda e, pT=pT, c=c, g=g, h=h, t=t: e.tensor_copy(out=QTa[64 * g:64 * g + 64, h, t * 128:(t + 1) * 128], in_=pT[64 * g:64 * g + 64, c, :]),
                         reads=[pTk + "_%d" % c, "QTz"], writes=[("QT", h, t)])
            P.op("vector", lambda e, pT=pT, t=t: e.tensor_copy(out=KT[:, t * 128:(t + 1) * 128], in_=pT[:, 2, :]),
                 reads=[pTk + "_2"], writes=[("KT", t)])
            P.op("vector", lambda e, zb=zb, t=t: e.tensor_copy(out=Vaug[:, t, :, 0:64], in_=zb[:, 384:512].rearrange("p (g d) -> p g d", d=64)),
                 reads=[zbk], writes=[("V", t)])
        for cc in range(12 if A_STAGE >= 3 else 0):
            pc, pck = pcr.next()
            for kc in range(8):
                P.op("tensor", lambda e, pc=pc, hT=hT, kc=kc, cc=cc: e.matmul(pc[:], lhsT=w1[:, kc, 512 + cc * 128:512 + (cc + 1) * 128], rhs=hT[:, kc, :],
                                                                       start=(kc == 0), stop=(kc == 7)),
                     reads=[hTk] + w1k, writes=[pck])
            zt, ztk = ztr.next()
            fn = AF.Sigmoid if cc in (8, 9) else AF.Identity
            P.op("scalar", lambda e, zt=zt, pc=pc, cc=cc, fn=fn: e.activation(out=zt[:], in_=pc[:], func=fn, bias=bc[:, cc:cc + 1]),
                 reads=[pck, "bc"], writes=[ztk])
            evs.append(P.dma("sync", lambda e, zt=zt, cc=cc, blk=blk: e.dma_start(out=zc_v[cc * 128:(cc + 1) * 128, blk * 512:(blk + 1) * 512], in_=zt[:]),
                             ztk, reads=[ztk]))
    P.flush(evs)
    cx.close()


def phase_B1(nc, P, T, l, QTa, QTb, KT, Vaug):
    cx = Ctx(nc, P)
    idf, idb = load_consts(cx, P, T["ident"])
    pSr = cx.ring(2, [128, 512], F32, "pS", psum=True)
    pO = [cx.ps([128, 512], F32) for _ in range(4)]
    pTr = cx.ring(1, [128, 2, 512], BF16, "pTa", psum=True)
    PTr = cx.ring(3, [128, 512], BF16, "PT")
    atr = cx.ring(2, [128, 4, 256], BF16, "at")
    aTr = cx.ring(2, [128, 2, 512], BF16, "aT")
    rcr = cx.ring(4, [128, 1], F32, "rc")
    Obr = cx.ring(2, [128, 4, 66], F32, "Ob")
    br_v = T["brT_d"].rearrange("(c p) s -> p c s", p=128)
    evs = []
    par = 0
    for qb in range(NB):
        at, atk = atr.next()
        for h in range(4):
            if True:
                g = h // 2
                par ^= 1
                qk = [("QT", h, qb * 4 + i) for i in range(4)]

                def emitS(kc, h=h, qb=qb, qk=qk):
                    pS, pSk = pSr.next()
                    P.op("tensor", lambda e, pS=pS: e.matmul(pS[:], lhsT=KT[:, kc * 128:(kc + 1) * 128],
                                                         rhs=QTa[:, h, qb * 512:(qb + 1) * 512], start=True, stop=True),
                         reads=qk + [("KT", kc)], writes=[pSk])
                    return pS, pSk
                cur = emitS(0)
                for kc in range(64):
                    nxt = emitS(kc + 1) if kc < 63 else None
                    pS, pSk = cur
                    PT, PTk = PTr.next()
                    P.op("scalar", lambda e, PT=PT, pS=pS: e.activation(out=PT[:], in_=pS[:], func=AF.Exp), reads=[pSk], writes=[PTk])
                    for qt in range(4):
                        P.op("tensor", lambda e, PT=PT, qt=qt, kc=kc, g=g: e.matmul(
                            pO[qt][:, 0:65], lhsT=PT[:, qt * 128:(qt + 1) * 128], rhs=Vaug[:, kc, g, 0:65],
                            start=(kc == 0), stop=(kc == 63)),
                            reads=[PTk, ("V", kc), "Vones"], writes=[("pO", qt)])
                    cur = nxt
                Ob, Obk = Obr.next()
                for qt in range(4):
                    P.op("vector", lambda e, Ob=Ob, qt=qt: e.tensor_copy(out=Ob[:, qt, :], in_=pO[qt][:, 0:66]),
                         reads=[("pO", qt)], writes=[(Obk, qt)])
                for qt in range(4):
                    rc, rck = rcr.next()
                    P.op("vector", lambda e, rc=rc, qt=qt, Ob=Ob: e.reciprocal(out=rc[:], in_=Ob[:, qt, 64:65]),
                         reads=[(Obk, qt)], writes=[rck])
                    P.op("vector", lambda e, rc=rc, qt=qt, Ob=Ob, at=at, h=h: e.tensor_scalar(
                        out=at[:, qt, h * 64:(h + 1) * 64], in0=Ob[:, qt, 0:64], scalar1=rc[:, 0:1], scalar2=None, op0=ALU.mult),
                        reads=[(Obk, qt), rck], writes=[(atk, qt, h)])
        pT, pTk = pTr.next()
        for qt in range(4):
            for hf in range(2):
                P.op("tensor", lambda e, pT=pT, at=at, qt=qt, hf=hf: e.transpose(out=pT[:, hf, qt * 128:(qt + 1) * 128], in_=at[:, qt, hf * 128:(hf + 1) * 128], identity=idb[:]),
                     reads=[(atk, qt, hh) for hh in range(4)] + ["idb"], writes=[(pTk, qt, hf)])
        aT, aTk = aTr.next()
        P.op("vector", lambda e, aT=aT, pT=pT: e.tensor_copy(out=aT[:], in_=pT[:]), reads=[(pTk, qt, hf) for qt in range(4) for hf in range(2)], writes=[aTk])
        evs.append(P.dma("sync", lambda e, aT=aT, qb=qb: e.dma_start(out=br_v[:, 0:2, qb * 512:(qb + 1) * 512], in_=aT[:]), aTk, reads=[aTk]))
    P.flush(evs)
    cx.close()


def phase_B2a(nc, P, T, l):
    cx = Ctx(nc, P)
    idf, idb = load_consts(cx, P, T["ident"])
    pmisc = cx.ps([128, 512], F32)
    cst = cx.sb([38, 256], F32)
    P.dma("sync", lambda e: e.dma_start(out=cst[0:31, :], in_=T["cf_conv_w"][l]), "c3", writes=[("cst", 0)])
    P.dma("sync", lambda e: e.dma_start(out=cst[31:34, :], in_=T["sc_conv_w"][l]), "c4", writes=[("cst", 1)])
    for i, nm in enumerate(("cf_conv_b", "cf_ln_g", "cf_ln_b", "pool_scale")):
        P.dma("sync", lambda e, i=i, nm=nm: e.dma_start(out=cst[34 + i:35 + i, :], in_=T[nm][l:l + 1, :]), "c5", writes=[("cst", 2 + i)])
    chp = cx.sb([128, 2, 38], F32)
    for c in range(2):
        P.op("tensor", lambda e, c=c: e.transpose(out=pmisc[:, 64 + c * 64:64 + c * 64 + 38], in_=cst[:, c * 128:(c + 1) * 128], identity=idf[0:38, 0:38]),
             reads=[("cst", i) for i in range(6)] + ["idf"], writes=["pmisc"], mode=64)
        P.op("vector", lambda e, c=c: e.tensor_copy(out=chp[:, c, :], in_=pmisc[:, 64 + c * 64:64 + c * 64 + 38]), reads=["pmisc"], writes=[("chp", c)])
    chk = [("chp", 0), ("chp", 1)]
    pwf = cx.sb([128, 2, 128], F32)
    pwbd = cx.sb([128, 2, 128], BF16)
    P.op("vector", lambda e: e.memset(pwf[:], 0.0), writes=["pwf"])
    for gi in range(4):
        c, hh = gi // 2, gi % 2
        P.dma("sync", lambda e, gi=gi, c=c, hh=hh: e.dma_start(out=pwf[hh * 64:(hh + 1) * 64, c, hh * 64:(hh + 1) * 64], in_=T["pool_w"][l][gi]),
              "c6", reads=[], writes=["pwf"])
    P.op("vector", lambda e: e.tensor_copy(out=pwbd[:], in_=pwf[:]), reads=["pwf"], writes=["pwbd"])
    invc = cx.sb([128, 3, 2, 512], F32)
    P.dma("sync", lambda e: e.dma_start(out=invc[:], in_=T["invc"].rearrange("a p c s -> p a c s")), "c7", writes=["invc"])
    onesf = cx.sb([128, 128], F32)
    P.op("vector", lambda e: e.memset(onesf[:], 1.0), writes=["onesf"])

    zchr = cx.ring(2, [128, 12, ZW], F32, "zch")
    brr = cx.ring(2, [128, 6, 512], BF16, "brT")
    cu = cx.sb([128, 2, ZW], F32)
    ag = cx.sb([128, 2, ZW], F32)
    acc = cx.sb([128, 2, 512], F32)
    ysq = cx.sb([128, 2, 512], F32)
    mean = cx.sb([128, 512], F32)
    rstd = cx.sb([128, 512], F32)
    pC = cx.sb([128, ZW], F32)
    pD = cx.sb([128, ZW], F32)
    Wt = cx.sb([128, 2, 512], F32)
    ypool = cx.sb([128, 2, 512], BF16)
    pMr = cx.ring(4, [128, 512], F32, "pM", psum=True)
    zc_v = T["zc_d"].rearrange("(c p) s -> p c s", p=128)
    br_v = T["brT_d"].rearrange("(c p) s -> p c s", p=128)
    evs = []
    for blk in range(NB):
        zch, zk = zchr.next()
        brT, bk = brr.next()
        zks = [(zk, q4) for q4 in range(4)]
        lo = max(0, blk * 512 - HALO)
        hi = min(S, blk * 512 + 512 + HALO)
        off = lo - (blk * 512 - HALO)
        for q4 in range(4):
            P.dma("sync", lambda e, zch=zch, lo=lo, hi=hi, off=off, q4=q4: e.dma_start(out=zch[:, 3 * q4:3 * q4 + 3, off:off + hi - lo], in_=zc_v[:, 3 * q4:3 * q4 + 3, lo:hi]),
                  zk + "_%d" % q4, writes=[(zk, q4)])
        if blk == 0:
            P.op("gpsimd", lambda e, zch=zch: e.memset(zch[:, :, 0:HALO], 0.0), writes=[(zk, 4)])
            zks = zks + [(zk, 4)]
        if blk == NB - 1:
            P.op("gpsimd", lambda e, zch=zch: e.memset(zch[:, :, ZW - HALO:ZW], 0.0), writes=[(zk, 4)])
            zks = zks + [(zk, 4)]
        P.op("gpsimd", lambda e, zch=zch: e.tensor_tensor(out=cu[:], in0=zch[:, 2:4, :], in1=zch[:, 4:6, :], op=ALU.mult), reads=zks, writes=["cu"])
        for c in range(2):
            P.op("vector", lambda e, c=c: e.tensor_scalar(out=acc[:, c, :], in0=cu[:, c, 15:527], scalar1=chp[:, c, 31:32], scalar2=None, op0=ALU.mult),
                 reads=["cu"] + chk, writes=[("acc", c)])
            for k in (1, 2):
                P.op("vector", lambda e, c=c, k=k: e.scalar_tensor_tensor(out=acc[:, c, :], in0=cu[:, c, 15 + k:527 + k], scalar=chp[:, c, 31 + k:32 + k],
                                                                      in1=acc[:, c, :], op0=ALU.mult, op1=ALU.add),
                     reads=["cu", ("acc", c)] + chk, writes=[("acc", c)])
            P.op("vector", lambda e, c=c, zch=zch, brT=brT: e.tensor_tensor(out=brT[:, c, :], in0=acc[:, c, :], in1=zch[:, c, HALO:HALO + 512], op=ALU.mult),
                 reads=[("acc", c)] + zks, writes=[(bk, c)])
        P.op("gpsimd", lambda e, zch=zch: e.tensor_tensor(out=ag[:], in0=zch[:, 6:8, :], in1=zch[:, 8:10, :], op=ALU.mult), reads=zks, writes=["ag"])
        for c in range(2):
            P.op("vector", lambda e, c=c: e.tensor_scalar(out=acc[:, c, :], in0=ag[:, c, 1:513], scalar1=chp[:, c, 0:1], scalar2=chp[:, c, 34:35],
                                                      op0=ALU.mult, op1=ALU.add),
                 reads=["ag"] + chk, writes=[("acc", c)])
            for k in range(1, 31):
                P.op("vector", lambda e, c=c, k=k: e.scalar_tensor_tensor(out=acc[:, c, :], in0=ag[:, c, 1 + k:513 + k], scalar=chp[:, c, k:k + 1],
                                                                      in1=acc[:, c, :], op0=ALU.mult, op1=ALU.add),
                     reads=["ag", ("acc", c)] + chk, writes=[("acc", c)])
        P.op("gpsimd", lambda e: e.tensor_tensor(out=ysq[:], in0=acc[:], in1=acc[:], op=ALU.mult), reads=[("acc", 0), ("acc", 1)], writes=["ysq"])
        pS, pSk = pMr.next()
        pQ, pQk = pMr.next()
        for c in range(2):
            P.op("tensor", lambda e, c=c, pS=pS: e.matmul(pS[:], lhsT=onesf[:], rhs=acc[:, c, :], start=(c == 0), stop=(c == 1)),
                 reads=["onesf", ("acc", c)], writes=[pSk])
        for c in range(2):
            P.op("tensor", lambda e, c=c, pQ=pQ: e.matmul(pQ[:], lhsT=onesf[:], rhs=ysq[:, c, :], start=(c == 0), stop=(c == 1)),
                 reads=["onesf", "ysq"], writes=[pQk])
        P.op("scalar", lambda e, pS=pS: e.activation(out=mean[:], in_=pS[:], func=AF.Identity, scale=1.0 / 256), reads=[pSk], writes=["mean"])
        P.op("gpsimd", lambda e: e.tensor_tensor(out=rstd[:], in0=mean[:], in1=mean[:], op=ALU.mult), reads=["mean"], writes=["rstd"])
        P.op("vector", lambda e, pQ=pQ: e.scalar_tensor_tensor(out=rstd[:], in0=pQ[:], scalar=1.0 / 256, in1=rstd[:], op0=ALU.mult, op1=ALU.subtract),
             reads=[pQk, "rstd"], writes=["rstd"])
        P.op("vector", lambda e: e.tensor_scalar(out=rstd[:], in0=rstd[:], scalar1=LN_EPS, scalar2=None, op0=ALU.add), reads=["rstd"], writes=["rstd"])
        P.op("scalar", lambda e: e.activation(out=rstd[:], in_=rstd[:], func=AF.Sqrt), reads=["rstd"], writes=["rstd"])
        P.op("vector", lambda e: e.reciprocal(out=rstd[:], in_=rstd[:]), reads=["rstd"], writes=["rstd"])
        for c in range(2):
            P.op("vector", lambda e, c=c: e.tensor_tensor(out=acc[:, c, :], in0=acc[:, c, :], in1=mean[:], op=ALU.subtract),
                 reads=[("acc", c), "mean", pSk], writes=[("acc", c)])
            P.op("vector", lambda e, c=c: e.tensor_tensor(out=acc[:, c, :], in0=acc[:, c, :], in1=rstd[:], op=ALU.mult),
                 reads=[("acc", c), "rstd"], writes=[("acc", c)])
            P.op("scalar", lambda e, c=c, brT=brT: e.activation(out=brT[:, 2 + c, :], in_=acc[:, c, :], func=AF.Silu, scale=chp[:, c, 35:36], bias=chp[:, c, 36:37]),
                 reads=[("acc", c)] + chk, writes=[(bk, 2 + c)])
        P.op("gpsimd", lambda e, zch=zch: e.tensor_tensor(out=cu[:, :, 1:ZW], in0=zch[:, 10:12, 0:ZW - 1], in1=zch[:, 10:12, 1:ZW], op=ALU.add),
             reads=zks, writes=["cu"])
        P.op("gpsimd", lambda e: e.tensor_tensor(out=ag[:, :, 2:ZW - 1], in0=cu[:, :, 1:ZW - 2], in1=cu[:, :, 3:ZW], op=ALU.add),
             reads=["cu"], writes=["ag"])
        P.op("gpsimd", lambda e: e.tensor_tensor(out=pC[:, 4:ZW - 3], in0=ag[:, 1, 2:ZW - 5], in1=ag[:, 1, 6:ZW - 1], op=ALU.add),
             reads=["ag"], writes=["pC"])
        P.op("gpsimd", lambda e: e.tensor_tensor(out=pD[:, 8:ZW - 7], in0=pC[:, 4:ZW - 11], in1=pC[:, 12:ZW - 3], op=ALU.add),
             reads=["pC"], writes=["pD"])
        ty = 0 if blk == 0 else (2 if blk == NB - 1 else 1)
        srcs = [(cu, 0, 0), (ag, 0, 1), (pC, None, 0), (pD, None, 1)]
        for gi, (src, cidx, hh) in enumerate(srcs):
            c = gi // 2
            sl = slice(hh * 64, (hh + 1) * 64)
            sap = src[sl, cidx, HALO:HALO + 512] if cidx is not None else src[sl, HALO:HALO + 512]
            P.op("vector", lambda e, sap=sap, sl=sl, c=c, ty=ty: e.tensor_tensor(out=Wt[sl, c, :], in0=sap, in1=invc[sl, ty, c, :], op=ALU.mult),
                 reads=["cu", "ag", "pC", "pD", "invc"], writes=[("Wt", gi)])
        P.op("vector", lambda e, zch=zch: e.tensor_tensor(out=ypool[:], in0=Wt[:], in1=zch[:, 10:12, HALO:HALO + 512], op=ALU.subtract),
             reads=[("Wt", gi) for gi in range(4)] + zks, writes=["ypool"])
        for c in range(2):
            pP, pPk = pMr.next()
            P.op("tensor", lambda e, c=c, pP=pP: e.matmul(pP[:], lhsT=pwbd[:, c, :], rhs=ypool[:, c, :], start=True, stop=True),
                 reads=["pwbd", "ypool"], writes=[pPk])
            P.op("scalar", lambda e, c=c, pP=pP, brT=brT: e.activation(out=brT[:, 4 + c, :], in_=pP[:], func=AF.Identity, scale=chp[:, c, 37:38]),
                 reads=[pPk] + chk, writes=[(bk, 4 + c)])
        evs.append(P.dma("sync", lambda e, brT=brT, blk=blk: e.dma_start(out=br_v[:, 2:8, blk * 512:(blk + 1) * 512], in_=brT[:]), bk,
                         reads=[(bk, i) for i in range(6)]))
    P.flush(evs)
    cx.close()


def phase_B2b(nc, P, T, l, Dall, Wall):
    cx = Ctx(nc, P)
    idf, idb = load_consts(cx, P, T["ident"])
    wv = T["w_in"][l].rearrange("(c p) n -> p c n", p=128)
    wg = cx.sb([128, 8, 4096], BF16)
    for i in range(2):
        P.dma("gpsimd", lambda e, i=i: e.dma_start(out=wg[:, :, i * 2048:(i + 1) * 2048], in_=wv[:, :, 2048 + i * 2048:2048 + (i + 1) * 2048]),
              "wg%d" % i, writes=[("wg", i)])
    wgk = [("wg", 0), ("wg", 1)]
    wbr = cx.sb([128, 8, 1024], BF16)
    P.dma("gpsimd", lambda e: e.dma_start(out=wbr[:], in_=T["w_branch"][l].rearrange("g (c p) n -> p (g c) n", p=128)), "wbr", writes=["wbr"])
    wo = cx.sb([128, 8, 1024], BF16)
    P.dma("gpsimd", lambda e: e.dma_start(out=wo[:], in_=T["w_out"][l].rearrange("(c p) n -> p c n", p=128)), "wo", writes=["wo"])
    wr = cx.sb([128, 8, 32], BF16)
    P.dma("gpsimd", lambda e: e.dma_start(out=wr[:], in_=T["w_router"][l].rearrange("(c p) n -> p c n", p=128)), "wr", writes=["wr"])
    brb = cx.sb([128, 32], F32)
    P.dma("sync", lambda e: e.dma_start(out=brb[:], in_=T["b_router"][l].partition_broadcast(128)), "c3", writes=["brb"])
    gst = cx.sb([32, 128], F32)
    bg = cx.sb([128, 32], F32)
    P.dma("sync", lambda e: e.dma_start(out=gst[:], in_=T["b_in"][l][2048:6144].rearrange("(c p) -> c p", p=128)), "c4", writes=["gst"])
    pmisc = cx.ps([128, 512], F32)
    P.op("tensor", lambda e: e.transpose(out=pmisc[:, 0:32], in_=gst[:], identity=idf[0:32, 0:32]), reads=["gst", "idf"], writes=["pmisc"], mode=32)
    P.op("vector", lambda e: e.tensor_copy(out=bg[:], in_=pmisc[:, 0:32]), reads=["pmisc"], writes=["bg"])
    onesb = cx.sb([128, 128], BF16)
    P.op("vector", lambda e: e.memset(onesb[:], 1.0), writes=["onesb"])
    Uf = cx.sb([128, 128], F32)
    Ub = cx.sb([128, 128], BF16)
    P.dma("sync", lambda e: e.dma_start(out=Uf[:], in_=T["utri"][:, :]), "c8", writes=["Uf"])
    P.op("vector", lambda e: e.tensor_copy(out=Ub[:], in_=Uf[:]), reads=["Uf"], writes=["Ub"])
    iotaC = cx.sb([128, 32], F32)
    P.dma("sync", lambda e: e.dma_start(out=iotaC[:], in_=T["iotac"][:, :]), "c9", writes=["iotaC"])
    cnt = cx.sb([128, 32], F32)
    P.op("vector", lambda e: e.memset(cnt[:], 0.0), writes=["cnt"])
    gt, bt = load_ln_params(P, cx, T["ln1_g"][l], T["ln1_b"][l])

    hTr = cx.ring(2, [128, 8, 512], BF16, "hTb")
    brr = cx.ring(2, [128, 8, 512], BF16, "brT")
    gtr = cx.ring(2, [128, 512], BF16, "gate")
    tmr = cx.ring(2, [128, 512], F32, "tmp")
    acr = cx.ring(2, [128, 512], F32, "macc")
    mT = cx.sb([128, 8, 512], BF16)
    pGr = cx.ring(2, [128, 512], F32, "pG", psum=True)
    pJr = cx.ring(2, [128, 512], F32, "pJ", psum=True)
    pMr = cx.ring(2, [128, 512], F32, "pM", psum=True)
    pTr = cx.ring(1, [128, 8, 128], BF16, "pT8", psum=True)
    htr = cx.ring(2, [128, 1024], F32, "ht")
    ofr = cx.ring(2, [128, 1024], F32, "of")
    obr = cx.ring(2, [128, 1024], BF16, "ob")
    h1Tr = cx.ring(2, [128, 8, 128], BF16, "h1T")
    smr = ln_scratch(cx, 1)
    lgr = cx.ring(2, [128, 32], F32, "lg")
    t8r = cx.ring(2, [128, 8], F32, "t8")
    mkr = cx.ring(2, [128, 32], BF16, "mk")
    dfr = cx.ring(2, [128, 32], F32, "df")
    slr = cx.ring(2, [128, 32], F32, "sl")
    s1r = cx.ring(2, [128, 4], F32, "s1")
    dkr = cx.ring(2, [128, 4], F32, "dk")
    hT_v = T["hT_d"].rearrange("(c p) s -> p c s", p=128)
    br_v = T["brT_d"].rearrange("(c p) s -> p c s", p=128)
    Xe = T["Xe_d"]
    evs = []
    for blk in range(NB):
        hT, hTk = hTr.next()
        P.dma("sync", lambda e, hT=hT, blk=blk: e.dma_start(out=hT[:], in_=hT_v[:, :, blk * 512:(blk + 1) * 512]), hTk, writes=[hTk])
        brT, bk = brr.next()
        P.dma("sync", lambda e, brT=brT, blk=blk: e.dma_start(out=brT[:], in_=br_v[:, :, blk * 512:(blk + 1) * 512]), bk, writes=[bk])
        for oc in range(8):
            ma, mak = acr.next()
            for g in range(4):
                pG, pGk = pGr.next()
                for kc in range(8):
                    P.op("tensor", lambda e, pG=pG, kc=kc, g=g, oc=oc, hT=hT: e.matmul(
                        pG[:], lhsT=wg[:, kc, g * 1024 + oc * 128:g * 1024 + (oc + 1) * 128], rhs=hT[:, kc, :], start=(kc == 0), stop=(kc == 7)),
                        reads=[hTk] + wgk, writes=[pGk])
                gate, gtk = gtr.next()
                P.op("scalar", lambda e, gate=gate, pG=pG, g=g, oc=oc: e.activation(out=gate[:], in_=pG[:], func=AF.Sigmoid, bias=bg[:, g * 8 + oc:g * 8 + oc + 1]),
                     reads=[pGk, "bg"], writes=[gtk])
                pJ, pJk = pJr.next()
                for c in range(2):
                    P.op("tensor", lambda e, pJ=pJ, c=c, g=g, oc=oc, brT=brT: e.matmul(
                        pJ[:], lhsT=wbr[:, g * 2 + c, oc * 128:(oc + 1) * 128], rhs=brT[:, g * 2 + c, :], start=(c == 0), stop=(c == 1)),
                        reads=["wbr", bk], writes=[pJk])
                if g == 0:
                    P.op("vector", lambda e, ma=ma, gate=gate, pJ=pJ: e.tensor_tensor(out=ma[:], in0=gate[:], in1=pJ[:], op=ALU.mult),
                         reads=[gtk, pJk], writes=[mak])
                else:
                    tm, tmk = tmr.next()
                    P.op("vector", lambda e, tm=tm, gate=gate, pJ=pJ: e.tensor_tensor(out=tm[:], in0=gate[:], in1=pJ[:], op=ALU.mult),
                         reads=[gtk, pJk], writes=[tmk])
                    if g < 3:
                        P.op("gpsimd", lambda e, ma=ma, tm=tm: e.tensor_tensor(out=ma[:], in0=ma[:], in1=tm[:], op=ALU.add), reads=[mak, tmk], writes=[mak])
                    else:
                        P.op("gpsimd", lambda e, ma=ma, tm=tm, oc=oc: e.tensor_tensor(out=mT[:, oc, :], in0=ma[:], in1=tm[:], op=ALU.add),
                             reads=[mak, tmk], writes=[("mT", oc)])
        for tt in range(4):
            t = blk * 4 + tt
            ht, htk = htr.next()
            P.dma("sync", lambda e, ht=ht, t=t: e.dma_start(out=ht[:], in_=T["h_d"][t * 128:(t + 1) * 128, :]), htk, writes=[htk])
            for hf in range(2):
                pM, pMk = pMr.next()
                for kc in range(8):
                    P.op("tensor", lambda e, pM=pM, kc=kc, tt=tt, hf=hf: e.matmul(
                        pM[:], lhsT=mT[:, kc, tt * 128:(tt + 1) * 128], rhs=wo[:, kc, hf * 512:(hf + 1) * 512], start=(kc == 0), stop=(kc == 7)),
                        reads=[("mT", kc), "wo"], writes=[pMk])
                P.op("vector", lambda e, ht=ht, pM=pM, hf=hf: e.scalar_tensor_tensor(
                    out=ht[:, hf * 512:(hf + 1) * 512], in0=ht[:, hf * 512:(hf + 1) * 512], scalar=DN_ALPHA, in1=pM[:], op0=ALU.mult, op1=ALU.add),
                    reads=[htk, pMk], writes=[htk])
            of, ofk = ofr.next()
            ob, obk = obr.next()
            ln_core(P, cx, ht, htk, gt, bt, of, ofk, ob, obk, smr.next())
            evs.append(P.dma("sync", lambda e, of=of, t=t: e.dma_start(out=T["h1_d"][t * 128:(t + 1) * 128, :], in_=of[:]), ofk, reads=[ofk]))
            pT, pTk = pTr.next()
            for c in range(8):
                P.op("tensor", lambda e, c=c, pT=pT, ob=ob: e.transpose(out=pT[:, c, :], in_=ob[:, c * 128:(c + 1) * 128], identity=idb[:]),
                     reads=[obk, "idb"], writes=[pTk + "_%d" % c])
            h1T, h1Tk = h1Tr.next()
            P.op("vector", lambda e, h1T=h1T, pT=pT: e.tensor_copy(out=h1T[:], in_=pT[:]), reads=[pTk + "_%d" % c for c in range(8)], writes=[h1Tk])
            for kc in range(8):
                P.op("tensor", lambda e, kc=kc, h1T=h1T: e.matmul(pmisc[:, 0:32], lhsT=h1T[:, kc, :], rhs=wr[:, kc, :], start=(kc == 0), stop=(kc == 7)),
                     reads=[h1Tk, "wr"], writes=["pmisc"])
            lg, lgk = lgr.next()
            P.op("vector", lambda e, lg=lg: e.tensor_tensor(out=lg[:], in0=pmisc[:, 0:32], in1=brb[:], op=ALU.add), reads=["pmisc", "brb"], writes=[lgk])
            t8, t8k = t8r.next()
            P.op("vector", lambda e, t8=t8, lg=lg: e.max(out=t8[:], in_=lg[:]), reads=[lgk], writes=[t8k])
            mk, mkk = mkr.next()
            P.op("vector", lambda e, mk=mk, lg=lg, t8=t8: e.tensor_scalar(out=mk[:], in0=lg[:], scalar1=t8[:, 3:4], scalar2=None, op0=ALU.is_ge),
                 reads=[lgk, t8k], writes=[mkk])
            s1, s1k = s1r.next()
            P.op("vector", lambda e, s1=s1, t8=t8: e.tensor_scalar(out=s1[:, 0:1], in0=t8[:, 0:1], scalar1=-1.0, scalar2=None, op0=ALU.mult),
                 reads=[t8k], writes=[s1k + "n"])
            P.op("scalar", lambda e, s1=s1, t8=t8, t=t: e.activation(out=Wall[:, t, :], in_=t8[:, 0:4], func=AF.Exp, bias=s1[:, 0:1], accum_out=s1[:, 1:2]),
                 reads=[t8k, s1k + "n"], writes=[("Wall", t), s1k + "s"])
            P.op("vector", lambda e, s1=s1: e.reciprocal(out=s1[:, 2:3], in_=s1[:, 1:2]), reads=[s1k + "s"], writes=[s1k + "r"])
            P.op("vector", lambda e, s1=s1, t=t: e.tensor_scalar(out=Wall[:, t, :], in0=Wall[:, t, :], scalar1=s1[:, 2:3], scalar2=None, op0=ALU.mult),
                 reads=[("Wall", t), s1k + "r"], writes=[("Wall", t)])
            P.op("tensor", lambda e, mk=mk: e.matmul(pmisc[:, 64:96], lhsT=Ub[:], rhs=mk[:], start=True, stop=True), reads=["Ub", mkk], writes=["pmisc"])
            P.op("tensor", lambda e, mk=mk: e.matmul(pmisc[:, 128:160], lhsT=onesb[:], rhs=mk[:], start=True, stop=True), reads=["onesb", mkk], writes=["pmisc"])
            df, dfk = dfr.next()
            P.op("vector", lambda e, df=df: e.tensor_tensor(out=df[:], in0=pmisc[:, 64:96], in1=cnt[:], op=ALU.add), reads=["pmisc", "cnt"], writes=[dfk])
            P.op("vector", lambda e: e.tensor_tensor(out=cnt[:], in0=pmisc[:, 128:160], in1=cnt[:], op=ALU.add), reads=["pmisc", "cnt"], writes=["cnt"])
            sl, slk = slr.next()
            P.op("vector", lambda e, sl=sl, df=df: e.tensor_scalar(out=sl[:], in0=df[:], scalar1=float(CAP), scalar2=1.0e6, op0=ALU.is_ge, op1=ALU.mult),
                 reads=[dfk], writes=[slk])
            P.op("vector", lambda e, sl=sl, df=df: e.tensor_tensor(out=df[:], in0=df[:], in1=sl[:], op=ALU.add), reads=[dfk, slk], writes=[dfk])
            P.op("vector", lambda e, df=df: e.tensor_tensor(out=df[:], in0=df[:], in1=iotaC[:], op=ALU.add), reads=[dfk, "iotaC"], writes=[dfk])
            dk, dkk = dkr.next()
            for k in range(4):
                P.op("vector", lambda e, sl=sl, lg=lg, t8=t8, k=k: e.tensor_scalar(out=sl[:], in0=lg[:], scalar1=t8[:, k:k + 1], scalar2=None, op0=ALU.is_equal),
                     reads=[lgk, t8k, dfk], writes=[slk])
                P.op("vector", lambda e, sl=sl, df=df: e.tensor_tensor(out=sl[:], in0=sl[:], in1=df[:], op=ALU.mult), reads=[slk, dfk], writes=[slk])
                P.op("vector", lambda e, sl=sl, dk=dk, k=k: e.tensor_reduce(out=dk[:, k:k + 1], in_=sl[:], axis=AX.X, op=ALU.add), reads=[slk], writes=[(dkk, k)])
            P.op("vector", lambda e, dk=dk, t=t: e.tensor_copy(out=Dall[:, t, :], in_=dk[:]), reads=[(dkk, k) for k in range(4)], writes=[("Dall", t)])
            for k in range(4):
                evs.append(P.dma("gpsimd", lambda e, ob=ob, t=t, k=k: e.indirect_dma_start(
                    out=Xe[:, :], out_offset=bass.IndirectOffsetOnAxis(ap=Dall[:, t, k:k + 1], axis=0), in_=ob[:], in_offset=None,
                    bounds_check=bc_reg(e, P), oob_is_err=False), obk + "sc", reads=[obk, ("Dall", t)]))
    P.flush(evs)
    cx.close()


def phase_C(nc, P, T, l):
    cx = Ctx(nc, P)
    idf, idb = load_consts(cx, P, T["ident"])
    gst = cx.sb([32, 2048], F32)
    bgu = cx.sb([128, 16, 32], F32)
    P.dma("sync", lambda e: e.dma_start(out=gst[:], in_=T["b_gate_up"][l]), "c1", writes=["gst"])
    pmisc = cx.ps([128, 16, 32], F32)
    for c in range(16):
        P.op("tensor", lambda e, c=c: e.transpose(out=pmisc[:, c, :], in_=gst[:, c * 128:(c + 1) * 128], identity=idf[0:32, 0:32]),
             reads=["gst", "idf"], writes=["pmisc"], mode=32)
    P.op("vector", lambda e: e.tensor_copy(out=bgu[:], in_=pmisc[:]), reads=["pmisc"], writes=["bgu"])
    wgr = cx.ring(2, [128, 8, 2048], BF16, "wgu")
    wdr = cx.ring(2, [128, 8, 1024], BF16, "wdn")
    bdr = cx.ring(2, [128, 1024], F32, "bd")
    xr = cx.ring(4, [128, 1024], BF16, "xs")
    XTr = cx.ring(2, [128, 8, 512], BF16, "XT")
    aTr = cx.ring(2, [128, 8, 512], BF16, "actT")
    pTr = cx.ring(1, [128, 8, 128], BF16, "pT8", psum=True)
    pGr = cx.ring(2, [128, 512], F32, "pG", psum=True)
    pLr = cx.ring(2, [128, 512], F32, "pL", psum=True)
    pYr = cx.ring(2, [128, 512], F32, "pY", psum=True)
    ggr = cx.ring(3, [128, 512], F32, "gg")
    sgr = cx.ring(3, [128, 512], F32, "sg")
    llr = cx.ring(3, [128, 512], F32, "ll")
    yr = cx.ring(2, [128, 1024], F32, "y")
    Xe, Ye = T["Xe_d"], T["Ye_d"]
    evs = []

    def load_w(e_i):
        wgu, wguk = wgr.next()
        wdn, wdnk = wdr.next()
        bd, bdk = bdr.next()
        src = T["w_gate_up"][l][e_i].rearrange("(c p) n -> p c n", p=128)
        for hf in range(2):
            P.dma("gpsimd", lambda e, wgu=wgu, src=src, hf=hf: e.dma_start(out=wgu[:, 4 * hf:4 * hf + 4, :], in_=src[:, 4 * hf:4 * hf + 4, :]),
                  wguk + "_%d" % hf, writes=[(wguk, hf)])
        P.dma("gpsimd", lambda e, wdn=wdn, e_i=e_i: e.dma_start(out=wdn[:], in_=T["w_down"][l][e_i].rearrange("(c p) n -> p c n", p=128)), wdnk, writes=[wdnk])
        P.dma("sync", lambda e, bd=bd, e_i=e_i: e.dma_start(out=bd[:], in_=T["b_down"][l][e_i].partition_broadcast(128)), bdk, writes=[bdk])
        return (wgu, wguk, wdn, wdnk, bd, bdk)

    NBLK = NE * (CAP // 512)
    BPE = CAP // 512
    W = {}
    W[0] = load_w(0)
    blkst = {}

    def PREP(b):
        base = b * 512
        XT, XTk = XTr.next()
        for st in range(4):
            xs, xsk = xr.next()
            P.dma("sync", lambda e, xs=xs, base=base, st=st: e.dma_start(out=xs[:], in_=Xe[base + st * 128:base + (st + 1) * 128, :]), xsk, writes=[xsk])
            pT, pTk = pTr.next()
            for c in range(8):
                P.op("tensor", lambda e, c=c, pT=pT, xs=xs: e.transpose(out=pT[:, c, :], in_=xs[:, c * 128:(c + 1) * 128], identity=idb[:]),
                     reads=[xsk, "idb"], writes=[pTk + "_%d" % c])
            P.op("vector", lambda e, XT=XT, pT=pT, st=st: e.tensor_copy(out=XT[:, :, st * 128:(st + 1) * 128], in_=pT[:]),
                 reads=[pTk + "_%d" % c for c in range(8)], writes=[(XTk, st)])
        aT, aTk = aTr.next()
        blkst[b] = dict(XT=XT, XTk=XTk, aT=aT, aTk=aTk, pend=None)

    def GU(b, fcs):
        ex = b // BPE
        if b % BPE == 0 and fcs[0] == 2 and ex + 1 < NE:
            W[ex + 1] = load_w(ex + 1)
        wgu, wguk, wdn, wdnk, bd, bdk = W[ex]
        wk = [(wguk, 0), (wguk, 1)]
        st_ = blkst[b]
        XT, XTk, aT, aTk = st_["XT"], st_["XTk"], st_["aT"], st_["aTk"]
        XTks = [(XTk, st) for st in range(4)]
        for fc in fcs:
            pG, pGk = pGr.next()
            pL, pLk = pLr.next()
            for kc in range(8):
                P.op("tensor", lambda e, pG=pG, kc=kc, fc=fc, XT=XT, wgu=wgu: e.matmul(pG[:], lhsT=wgu[:, kc, fc * 128:(fc + 1) * 128], rhs=XT[:, kc, :],
                                                                               start=(kc == 0), stop=(kc == 7)),
                     reads=XTks + wk, writes=[pGk])
            for kc in range(8):
                P.op("tensor", lambda e, pL=pL, kc=kc, fc=fc, XT=XT, wgu=wgu: e.matmul(pL[:], lhsT=wgu[:, kc, 1024 + fc * 128:1024 + (fc + 1) * 128], rhs=XT[:, kc, :],
                                                                               start=(kc == 0), stop=(kc == 7)),
                     reads=XTks + wk, writes=[pLk])
            gg, ggk = ggr.next()
            sg, sgk = sgr.next()
            ll, llk = llr.next()
            P.op("scalar", lambda e, ll=ll, pL=pL, fc=fc, ex=ex: e.activation(out=ll[:], in_=pL[:], func=AF.Identity, bias=bgu[:, 8 + fc, ex:ex + 1]),
                 reads=[pLk, "bgu"], writes=[llk])
            P.op("vector", lambda e, gg=gg, pG=pG, fc=fc, ex=ex: e.tensor_scalar(out=gg[:], in0=pG[:], scalar1=bgu[:, fc, ex:ex + 1], scalar2=7.0, op0=ALU.add, op1=ALU.min),
                 reads=[pGk, "bgu"], writes=[ggk])
            P.op("scalar", lambda e, sg=sg, gg=gg: e.activation(out=sg[:], in_=gg[:], func=AF.Sigmoid, scale=1.702), reads=[ggk], writes=[sgk])
            P.op("gpsimd", lambda e, ll=ll: e.tensor_scalar(out=ll[:], in0=ll[:], scalar1=7.0, scalar2=-7.0, op0=ALU.min, op1=ALU.max), reads=[llk], writes=[llk])
            if st_["pend"] is not None:
                st_["pend"]()

            def fin(gg=gg, ggk=ggk, sg=sg, sgk=sgk, ll=ll, llk=llk, fc=fc, aT=aT, aTk=aTk):
                P.op("gpsimd", lambda e: e.tensor_tensor(out=gg[:], in0=gg[:], in1=sg[:], op=ALU.mult), reads=[ggk, sgk], writes=[ggk])
                P.op("vector", lambda e: e.scalar_tensor_tensor(out=aT[:, fc, :], in0=ll[:], scalar=1.0, in1=gg[:], op0=ALU.add, op1=ALU.mult),
                     reads=[ggk, llk], writes=[(aTk, fc)])
            st_["pend"] = fin
        if fcs[-1] == 7:
            st_["pend"]()
            st_["pend"] = None

    def DOWN(b):
        ex = b // BPE
        base = b * 512
        wgu, wguk, wdn, wdnk, bd, bdk = W[ex]
        st_ = blkst.pop(b)
        aT, aTk = st_["aT"], st_["aTk"]
        aTks = [(aTk, fc) for fc in range(8)]
        for st in range(4):
            y, yk = yr.next()
            for hf in range(2):
                pY, pYk = pYr.next()
                for fc in range(8):
                    P.op("tensor", lambda e, pY=pY, fc=fc, st=st, hf=hf, aT=aT, wdn=wdn: e.matmul(
                        pY[:], lhsT=aT[:, fc, st * 128:(st + 1) * 128], rhs=wdn[:, fc, hf * 512:(hf + 1) * 512], start=(fc == 0), stop=(fc == 7)),
                        reads=aTks + [wdnk], writes=[pYk])
                if hf == 0:
                    P.op("vector", lambda e, y=y, pY=pY, hf=hf, bd=bd: e.tensor_tensor(out=y[:, hf * 512:(hf + 1) * 512], in0=pY[:], in1=bd[:, hf * 512:(hf + 1) * 512], op=ALU.add),
                         reads=[pYk, bdk], writes=[(yk, hf)])
                else:
                    P.op("scalar", lambda e, y=y, pY=pY, hf=hf: e.activation(out=y[:, hf * 512:(hf + 1) * 512], in_=pY[:], func=AF.Identity),
                         reads=[pYk], writes=[(yk, "t")])
                    P.op("gpsimd", lambda e, y=y, hf=hf, bd=bd: e.tensor_tensor(out=y[:, hf * 512:(hf + 1) * 512], in0=y[:, hf * 512:(hf + 1) * 512], in1=bd[:, hf * 512:(hf + 1) * 512], op=ALU.add),
                         reads=[(yk, "t"), bdk], writes=[(yk, hf)])
            evs.append(P.dma("sync", lambda e, y=y, base=base, st=st: e.dma_start(out=Ye[base + st * 128:base + (st + 1) * 128, :], in_=y[:]), yk,
                             reads=[(yk, 0), (yk, 1)]))

    PREP(0)
    for b in range(NBLK + 1):
        if b < NBLK:
            GU(b, [0, 1])
        if b >= 1:
            DOWN(b - 1)
        if b < NBLK:
            GU(b, [2, 3])
            if b + 1 < NBLK:
                PREP(b + 1)
            GU(b, [4, 5, 6, 7])
    P.flush(evs)
    cx.close()


def phase_D(nc, P, T, l, Dall, Wall, last):
    cx = Ctx(nc, P)
    idf, idb = load_consts(cx, P, T["ident"])
    gt, bt = load_ln_params(P, cx, T["ln2_g"][l], T["ln2_b"][l])
    ykr = [cx.ring(3, [128, 1024], F32, "yk%d_" % k) for k in range(4)]
    htr = cx.ring(3, [128, 1024], F32, "ht")
    rr = cx.ring(2, [128, 1024], F32, "r")
    ofr = cx.ring(2, [128, 1024], F32, "of")
    obr = cx.ring(2, [128, 1024], BF16, "ob")
    pTr = cx.ring(2, [128, 8, 128], BF16, "pT", psum=True)
    hTr = cx.ring(2, [128, 8, 128], BF16, "hT")
    smr = ln_scratch(cx)
    Ye = T["Ye_d"]
    evs = []
    pre = {}

    def prefetch(t):
        ys = []
        for k in range(4):
            yk_, ykk = ykr[k].next()
            P.op("gpsimd", lambda e, yk_=yk_: e.memset(yk_[:], 0.0), writes=[ykk])
            P.dma("gpsimd", lambda e, yk_=yk_, t=t, k=k: e.indirect_dma_start(
                out=yk_[:], out_offset=None, in_=Ye[:, :], in_offset=bass.IndirectOffsetOnAxis(ap=Dall[:, t, k:k + 1], axis=0),
                bounds_check=bc_reg(e, P), oob_is_err=False), ykk, writes=[ykk])
            ys.append((yk_, ykk))
        ht, htk = htr.next()
        P.dma("sync", lambda e, ht=ht, t=t: e.dma_start(out=ht[:], in_=T["h1_d"][t * 128:(t + 1) * 128, :]), htk, writes=[htk])
        pre[t] = (ys, ht, htk)

    prefetch(0)
    prefetch(1)
    for t in range(NT):
        if t + 2 < NT:
            prefetch(t + 2)
        ys, ht, htk = pre.pop(t)
        r, rk = rr.next()
        P.op("vector", lambda e, r=r, ht=ht: e.tensor_scalar(out=r[:], in0=ht[:], scalar1=DN_ALPHA, scalar2=None, op0=ALU.mult), reads=[htk], writes=[rk])
        for k in range(4):
            yk_, ykk = ys[k]
            P.op("vector", lambda e, r=r, yk_=yk_, t=t, k=k: e.scalar_tensor_tensor(out=r[:], in0=yk_[:], scalar=Wall[:, t, k:k + 1], in1=r[:], op0=ALU.mult, op1=ALU.add),
                 reads=[ykk, rk], writes=[rk])
        of, ofk = ofr.next()
        ob, obk = obr.next()
        ln_core(P, cx, r, rk, gt, bt, of, ofk, None if last else ob, obk, smr.next(), add_eng="vector")
        dst = T["out"] if last else T["h_d"]
        evs.append(P.dma("sync", lambda e, of=of, t=t, dst=dst: e.dma_start(out=dst[t * 128:(t + 1) * 128, :], in_=of[:]), ofk, reads=[ofk]))
        if not last:
            pT, pTk = pTr.next()
            hTt, hTk = hTr.next()
            evs.append(transpose_store_hT(P, ob, obk, pT, pTk, hTt, hTk, idb, T["hT_d"], t, hTk))
    P.flush(evs)
    cx.close()


PARAM_SPECS = [
    ("ln_in_g", (1024,)), ("ln_in_b", (1024,)), ("w_in", (2, 1024, 6144)), ("b_in", (2, 6144)),
    ("q_norm_g", (2, 64)), ("k_norm_g", (2, 64)), ("sc_conv_w", (2, 3, 256)), ("cf_conv_w", (2, 31, 256)),
    ("cf_conv_b", (2, 256)), ("cf_ln_g", (2, 256)), ("cf_ln_b", (2, 256)), ("pool_w", (2, 4, 64, 64)),
    ("pool_scale", (2, 256)), ("w_branch", (2, 4, 256, 1024)), ("w_out", (2, 1024, 1024)),
    ("ln1_g", (2, 1024)), ("ln1_b", (2, 1024)), ("w_router", (2, 1024, 32)), ("b_router", (2, 32)),
    ("w_gate_up", (2, 32, 1024, 2048)), ("b_gate_up", (2, 32, 2048)), ("w_down", (2, 32, 1024, 1024)),
    ("b_down", (2, 32, 1024)), ("ln2_g", (2, 1024)), ("ln2_b", (2, 1024)),
]
CONST_SPECS = [("ident", (128, 128)), ("cos6", (S, 384)), ("sin6", (S, 384)), ("invc", (3, 128, 2, 512)),
               ("utri", (128, 128)), ("iotac", (128, 32))]
SCRATCH = [("h_d", (S, D), F32), ("hT_d", (D, S), BF16), ("zc_d", (1536, S), F32), ("brT_d", (1024, S), BF16),
           ("h1_d", (S, D), F32), ("Xe_d", (NSLOT, D), BF16), ("Ye_d", (NSLOT, D), F32)]


def build_program(upto=99, debug=(), small=False, astage=9):
    global A_STAGE
    A_STAGE = astage
    nc = bass.Bass("TRN2", target_bir_lowering=False)
    T = {}
    T["x"] = nc.dram_tensor("x", [S, D], F32, kind="ExternalInput").ap()
    for nm, shp in PARAM_SPECS + CONST_SPECS:
        if small and nm in ("w_gate_up", "w_down"):
            shp = (2, 1) + tuple(shp[2:])
        T[nm] = nc.dram_tensor(nm, list(shp), F32, kind="ExternalInput").ap()
    T["out"] = nc.dram_tensor("out", [S, D], F32, kind="ExternalOutput").ap()
    for nm, shp, dt in SCRATCH:
        T[nm] = nc.dram_tensor(nm, list(shp), dt, kind="ExternalOutput" if nm in debug else "Internal").ap()
    P = Prog(nc)
    with ExitStack() as top:
        Dall = top.enter_context(nc.sbuf_tensor("Dall", [128, NT, 4], I32))
        Wall = top.enter_context(nc.sbuf_tensor("Wall", [128, NT, 4], F32))
        ph = 0
        phase_ln_in(nc, P, T)
        for l in range(DEPTH):
            if ph >= upto:
                break
            with ExitStack() as att:
                QTa = att.enter_context(nc.sbuf_tensor("QT%d" % l, [128, 4, S], BF16))
                QTb = None
                KT = att.enter_context(nc.sbuf_tensor("KT%d" % l, [128, S], BF16))
                Vaug = att.enter_context(nc.sbuf_tensor("Vaug%d" % l, [128, NT, 2, 66], BF16))
                phase_A(nc, P, T, l, QTa, QTb, KT, Vaug)
                ph += 1
                if ph >= upto:
                    break
                phase_B1(nc, P, T, l, QTa, QTb, KT, Vaug)
                ph += 1
            if ph >= upto:
                break
            phase_B2a(nc, P, T, l)
            ph += 1
            if ph >= upto:
                break
            phase_B2b(nc, P, T, l, Dall, Wall)
            ph += 1
            if ph >= upto:
                break
            phase_C(nc, P, T, l)
            ph += 1
            if ph >= upto:
                break
            phase_D(nc, P, T, l, Dall, Wall, last=(l == DEPTH - 1))
            ph += 1
    return nc


def host_consts():
    c = {}
    c["ident"] = np.eye(128, dtype=np.float32)
    rows = S // 64
    row = np.repeat(np.arange(rows, dtype=np.float32), 64)
    col = np.tile(np.arange(64, dtype=np.float32), rows)
    inv = (np.float32(10000.0) ** (-np.arange(0, 32, 2, dtype=np.float32) / np.float32(32))).astype(np.float32)
    ar = (row[:, None] * inv).astype(np.float32)
    ac = (col[:, None] * inv).astype(np.float32)
    cr, sr, cc, sc = np.cos(ar), np.sin(ar), np.cos(ac), np.sin(ac)
    cos1 = np.concatenate([cr, cr, cc, cc], axis=1).astype(np.float32)
    sin1 = np.concatenate([-sr, sr, -sc, sc], axis=1).astype(np.float32)
    c["cos6"] = np.ascontiguousarray(np.tile(cos1, (1, 6)))
    c["sin6"] = np.ascontiguousarray(np.tile(sin1, (1, 6)))
    invc = np.zeros((3, 128, 2, 512), np.float32)
    wins = (2, 4, 8, 16)
    for ty, blk in enumerate((0, 1, NB - 1)):
        t = np.arange(blk * 512, blk * 512 + 512)
        for gi, w in enumerate(wins):
            lo = np.maximum(t - w // 2, 0)
            hi = np.minimum(t + (w - 1 - w // 2), S - 1)
            ic = (1.0 / (hi - lo + 1)).astype(np.float32)
            cidx, hh = gi // 2, gi % 2
            invc[ty, hh * 64:(hh + 1) * 64, cidx, :] = ic[None, :]
    c["invc"] = invc
    c["utri"] = np.triu(np.ones((128, 128), np.float32), k=1)
    c["iotac"] = np.tile((np.arange(32, dtype=np.float32) * CAP)[None, :], (128, 1)).astype(np.float32)
    return c


_CACHE = {}


def kernel(**inputs):
    if "nc" not in _CACHE:
        _CACHE["nc"] = build_program()
    nc = _CACHE["nc"]
    consts = host_consts()
    shared = {nm: np.ascontiguousarray(np.asarray(inputs[nm], dtype=np.float32)) for nm, _ in PARAM_SPECS}
    shared.update(consts)
    x = np.asarray(inputs["x"], dtype=np.float32)
    in_maps = []
    for b in range(8):
        m = dict(shared)
        m["x"] = np.ascontiguousarray(x[b])
        in_maps.append(m)
    res = run_bass_kernel_spmd(nc, in_maps, core_ids=list(range(8)))
    return np.stack([np.asarray(r["out"]) for r in res.results], axis=0).astype(np.float32)
```

```python
import math
from contextlib import ExitStack
import numpy as np
import concourse.bass as bass
import concourse.mybir as mybir
from concourse.bass_utils import run_bass_kernel_spmd

F32 = mybir.dt.float32
BF16 = mybir.dt.bfloat16
I32 = mybir.dt.int32
AF = mybir.ActivationFunctionType
ALU = mybir.AluOpType
AX = mybir.AxisListType

ENGS = ("sync", "scalar", "vector", "gpsimd", "tensor")
S = 8192
D = 1024
NT = 64
NB = 16
NE = 32
CAP = 1536
NSLOT = NE * CAP
DEPTH = 2
DN_ALPHA = (2.0 * DEPTH) ** 0.25
LN_EPS = 1e-5
HALO = 16
ZW = 512 + 2 * HALO


class _Probe:
    def __init__(self):
        self.name = None

    def __getattr__(self, nm):
        def f(*a, **k):
            self.name = nm
            return self
        return f


class Prog:
    def __init__(self, nc):
        self.nc = nc
        self.esem = {e: nc.alloc_semaphore("es_" + e) for e in ENGS}
        self.ebase = {e: 0 for e in ENGS}
        self.dsem = {}
        self.nflush = 0
        self._reset()

    def _reset(self):
        self.ops = {e: [] for e in ENGS}
        self.waited = {e: {} for e in ENGS}
        self.last_w = {}
        self.readers = {}
        self.signal = {e: set() for e in ENGS}
        self.pe_mode = None

    def _need(self, eng, ev, waits):
        if ev is None:
            return
        if ev[0] == "e" and ev[1] == eng and eng == "tensor":
            return
        key = (ev[0], ev[1])
        if self.waited[eng].get(key, -1) >= ev[2]:
            return
        waits[key] = max(waits.get(key, -1), ev[2])

    def _deps(self, eng, reads, writes, extra=()):
        waits = {}
        for k in reads:
            self._need(eng, self.last_w.get(k), waits)
        for k in writes:
            self._need(eng, self.last_w.get(k), waits)
            for ev in self.readers.get(k, ()):
                self._need(eng, ev, waits)
        for ev in extra:
            self._need(eng, ev, waits)
        out = []
        for key, v in waits.items():
            self.waited[eng][key] = v
            out.append((key[0], key[1], v))
            if key[0] == "e":
                self.signal[key[1]].add(v)
        return out

    def _commit(self, ev, reads, writes):
        for k in reads:
            self.readers.setdefault(k, []).append(ev)
        for k in writes:
            self.last_w[k] = ev
            self.readers[k] = []

    def op(self, eng, fn, reads=(), writes=(), extra=(), mode=128):
        waits = self._deps(eng, reads, writes, extra)
        idx = len(self.ops[eng])
        if eng == "tensor":
            pr = _Probe()
            fn(pr)
            mode = (pr.name, mode)
            if self.pe_mode is not None and self.pe_mode != mode and idx > 0:
                key = ("e", "tensor")
                if self.waited[eng].get(key, -1) < idx - 1:
                    self.waited[eng][key] = idx - 1
                    waits.append(("e", "tensor", idx - 1))
                    self.signal["tensor"].add(idx - 1)
            self.pe_mode = mode
        self.ops[eng].append(dict(fn=fn, waits=waits, dma=None))
        ev = ("e", eng, idx)
        self._commit(ev, reads, writes)
        return ev

    def dma(self, eng, fn, semkey, reads=(), writes=(), extra=()):
        waits = self._deps(eng, reads, writes, extra)
        sn = "ds_" + semkey
        if sn not in self.dsem:
            self.dsem[sn] = [self.nc.alloc_semaphore(sn), 0]
        st = self.dsem[sn]
        st[1] += 16
        self.ops[eng].append(dict(fn=fn, waits=waits, dma=(st[0], st[1])))
        ev = ("d", sn, st[1])
        self._commit(ev, reads, writes)
        return ev

    def wait(self, eng, evs):
        waits = self._deps(eng, (), (), evs)
        self.ops[eng].append(dict(fn=None, waits=waits, dma=None))

    def flush(self, final_events=()):
        nc = self.nc
        if final_events:
            self.wait("sync", final_events)
        val = {}
        for e in ENGS:
            v = self.ebase[e]
            m = {}
            for idx in sorted(self.signal[e]):
                v += 1
                m[idx] = v
            val[e] = m
            self.ebase[e] = v
        ops, esem, dsem = self.ops, self.esem, self.dsem

        def emit(e, name):
            for idx, o in enumerate(ops[name]):
                for kind, who, v in o["waits"]:
                    if kind == "e":
                        e.wait_ge(esem[who], val[who][v])
                    else:
                        e.wait_ge(dsem[who][0], v)
                if o["fn"] is None:
                    continue
                ins = o["fn"](e)
                if o["dma"] is not None:
                    ins.then_inc(o["dma"][0], 16)
                elif idx in val[name]:
                    ins.then_inc(esem[name], 1)

        with nc.Block() as block:
            @block.sync
            def _(e):
                emit(e, "sync")

            @block.scalar
            def _(e):
                emit(e, "scalar")

            @block.vector
            def _(e):
                emit(e, "vector")

            @block.gpsimd
            def _(e):
                emit(e, "gpsimd")

            @block.tensor
            def _(e):
                emit(e, "tensor")
        self.nflush += 1
        self._reset()


_REG = {}


def bc_reg(e, P):
    key = P.nflush
    if key not in _REG:
        _REG.clear()
        _REG[key] = e.to_reg(NSLOT - 1)
    return _REG[key]


class Ring:
    def __init__(self, items):
        self.items = items
        self.i = -1

    def next(self):
        self.i = (self.i + 1) % len(self.items)
        return self.items[self.i]


class Ctx:
    _uid = [0]

    def __init__(self, nc, P):
        self.nc, self.P = nc, P
        self.stack = ExitStack()
        self.n = 0
        Ctx._uid[0] += 1
        self.uid = Ctx._uid[0]

    def sb(self, shape, dt, name=None):
        self.n += 1
        return self.stack.enter_context(self.nc.sbuf_tensor(name or f"t{self.n}_{self.uid}", list(shape), dt))

    def ps(self, shape, dt, name=None):
        self.n += 1
        return self.stack.enter_context(self.nc.psum_tensor(name or f"p{self.n}_{self.uid}", list(shape), dt))

    def ring(self, n, shape, dt, key, psum=False):
        return Ring([((self.ps if psum else self.sb)(shape, dt), f"{key}{i}") for i in range(n)])

    def close(self):
        self.stack.close()


def load_consts(cx, P, ident_d):
    nc = cx.nc
    idf = cx.sb([128, 128], F32)
    idb = cx.sb([128, 128], BF16)
    P.dma("sync", lambda e: e.dma_start(out=idf[:], in_=ident_d[:, :]), "c0", writes=["idf"])
    P.op("vector", lambda e: e.tensor_copy(out=idb[:], in_=idf[:]), reads=["idf"], writes=["idb"])
    return idf, idb


def ln_core(P, cx, r, rk, gt, bt, of, ofk, ob, obk, sm, add_eng="gpsimd"):
    st, mv, rs, nb, xn, k = sm
    for c in range(2):
        P.op("vector", lambda e, c=c: e.bn_stats(out=st[:, c, :], in_=r[:, c * 512:(c + 1) * 512]),
             reads=[rk], writes=[k + "st%d" % c])
    P.op("vector", lambda e: e.bn_aggr(out=mv[:], in_=st[:].rearrange("p a b -> p (a b)")),
         reads=[k + "st0", k + "st1"], writes=[k + "mv"])
    P.op("vector", lambda e: e.tensor_scalar(out=rs[:], in0=mv[:, 1:2], scalar1=LN_EPS, scalar2=None, op0=ALU.add),
         reads=[k + "mv"], writes=[k + "rs"])
    P.op("scalar", lambda e: e.activation(out=rs[:], in_=rs[:], func=AF.Sqrt), reads=[k + "rs"], writes=[k + "rs"])
    P.op("vector", lambda e: e.reciprocal(out=rs[:], in_=rs[:]), reads=[k + "rs"], writes=[k + "rs"])
    P.op("vector", lambda e: e.tensor_scalar(out=nb[:], in0=mv[:, 0:1], scalar1=rs[:, 0:1], scalar2=-1.0,
                                             op0=ALU.mult, op1=ALU.mult),
         reads=[k + "mv", k + "rs"], writes=[k + "nb"])
    P.op("scalar", lambda e: e.activation(out=xn[:], in_=r[:], func=AF.Identity, scale=rs[:, 0:1], bias=nb[:, 0:1]),
         reads=[rk, k + "rs", k + "nb"], writes=[k + "xn"])
    P.op("vector", lambda e: e.tensor_tensor(out=xn[:], in0=xn[:], in1=gt[:], op=ALU.mult),
         reads=[k + "xn", "lng"], writes=[k + "xn"])
    P.op(add_eng, lambda e: e.tensor_tensor(out=of[:], in0=xn[:], in1=bt[:], op=ALU.add),
         reads=[k + "xn", "lnb"], writes=[ofk])
    if ob is not None:
        P.op("scalar", lambda e: e.copy(out=ob[:], in_=of[:]), reads=[ofk], writes=[obk])


def ln_scratch(cx, n=2):
    return Ring([(cx.sb([128, 2, 6], F32), cx.sb([128, 2], F32), cx.sb([128, 1], F32), cx.sb([128, 1], F32),
                  cx.sb([128, 1024], F32), f"ln{i}") for i in range(n)])


def load_ln_params(P, cx, g_ap, b_ap):
    gt = cx.sb([128, 1024], F32)
    bt = cx.sb([128, 1024], F32)
    P.dma("sync", lambda e: e.dma_start(out=gt[:], in_=g_ap.partition_broadcast(128)), "c1", writes=["lng"])
    P.dma("sync", lambda e: e.dma_start(out=bt[:], in_=b_ap.partition_broadcast(128)), "c2", writes=["lnb"])
    return gt, bt


def transpose_store_hT(P, hb, hbk, pT, pTk, hTt, hTk, idb, hT_d, t, semkey):
    for c in range(8):
        P.op("tensor", lambda e, c=c: e.transpose(out=pT[:, c, :], in_=hb[:, c * 128:(c + 1) * 128], identity=idb[:]),
             reads=[hbk, "idb"], writes=[pTk + "_%d" % c])
    P.op("vector", lambda e: e.tensor_copy(out=hTt[:], in_=pT[:]), reads=[pTk + "_%d" % c for c in range(8)],
         writes=[hTk])
    return P.dma("sync", lambda e: e.dma_start(
        out=hT_d.rearrange("(c p) s -> p c s", p=128)[:, :, t * 128:(t + 1) * 128], in_=hTt[:]),
        semkey, reads=[hTk])


def phase_ln_in(nc, P, T):
    cx = Ctx(nc, P)
    idf, idb = load_consts(cx, P, T["ident"])
    gt, bt = load_ln_params(P, cx, T["ln_in_g"], T["ln_in_b"])
    xr = cx.ring(3, [128, 1024], F32, "x")
    ofr = cx.ring(2, [128, 1024], F32, "of")
    obr = cx.ring(2, [128, 1024], BF16, "ob")
    pTr = cx.ring(2, [128, 8, 128], BF16, "pT", psum=True)
    hTr = cx.ring(2, [128, 8, 128], BF16, "hT")
    smr = ln_scratch(cx)
    evs = []
    pre = {}

    def prefetch(t):
        xt, xk = xr.next()
        P.dma("sync", lambda e, xt=xt, t=t: e.dma_start(out=xt[:], in_=T["x"][t * 128:(t + 1) * 128, :]), xk, writes=[xk])
        pre[t] = (xt, xk)
    prefetch(0)
    prefetch(1)
    for t in range(NT):
        if t + 2 < NT:
            prefetch(t + 2)
        xt, xk = pre.pop(t)
        of, ofk = ofr.next()
        ob, obk = obr.next()
        ln_core(P, cx, xt, xk, gt, bt, of, ofk, ob, obk, smr.next())
        evs.append(P.dma("sync", lambda e, of=of, t=t: e.dma_start(out=T["h_d"][t * 128:(t + 1) * 128, :], in_=of[:]),
                         ofk, reads=[ofk]))
        pT, pTk = pTr.next()
        hTt, hTk = hTr.next()
        evs.append(transpose_store_hT(P, ob, obk, pT, pTk, hTt, hTk, idb, T["hT_d"], t, hTk))
    P.flush(evs)
    cx.close()


A_STAGE = 9
QPERM = [0, 2, 1, 3]


def phase_A(nc, P, T, l, QTa, QTb, KT, Vaug):
    cx = Ctx(nc, P)
    idf, idb = load_consts(cx, P, T["ident"])
    w_in = T["w_in"][l]
    b_in = T["b_in"][l]
    wv = w_in.rearrange("(c p) n -> p c n", p=128)
    w1 = cx.sb([128, 8, 2048], BF16)
    for s in range(4):
        hs = QPERM[s]
        P.dma("gpsimd", lambda e, s=s, hs=hs: e.dma_start(out=w1[:, :, s * 64:(s + 1) * 64], in_=wv[:, :, hs * 64:(hs + 1) * 64]),
              "w1q", writes=[("w1", s)])
    P.dma("gpsimd", lambda e: e.dma_start(out=w1[:, :, 256:2048], in_=wv[:, :, 256:2048]), "w1r", writes=[("w1", 4)])
    w1k = [("w1", i) for i in range(5)]
    bq = cx.sb([128, 512], F32)
    for s in range(4):
        hs = QPERM[s]
        P.dma("sync", lambda e, s=s, hs=hs: e.dma_start(out=bq[:, s * 64:(s + 1) * 64],
                                                        in_=b_in[hs * 64:(hs + 1) * 64].partition_broadcast(128)),
              "c1", writes=[("bq", s)])
    P.dma("sync", lambda e: e.dma_start(out=bq[:, 256:512], in_=b_in[256:512].partition_broadcast(128)), "c2",
          writes=[("bq", 4)])
    bqk = [("bq", i) for i in range(5)]
    bst = cx.sb([12, 128], F32)
    bc = cx.sb([128, 12], F32)
    P.dma("sync", lambda e: e.dma_start(out=bst[:], in_=b_in[512:2048].rearrange("(c p) -> c p", p=128)), "c3", writes=["bst"])
    pB = cx.ps([128, 512], F32)
    P.op("tensor", lambda e: e.transpose(out=pB[:, 0:12], in_=bst[:], identity=idf[0:12, 0:12]), reads=["bst", "idf"], writes=["pB"], mode=32)
    P.op("vector", lambda e: e.tensor_copy(out=bc[:], in_=pB[:, 0:12]), reads=["pB"], writes=["bc"])
    gq = cx.sb([128, 384], F32)
    for s in range(6):
        src = T["q_norm_g"][l] if s < 4 else T["k_norm_g"][l]
        P.dma("sync", lambda e, s=s, src=src: e.dma_start(out=gq[:, s * 64:(s + 1) * 64], in_=src.partition_broadcast(128)),
              "c4" if s < 4 else "c5", writes=[("gq", s)])
    P.op("vector", lambda e: e.tensor_scalar(out=gq[:, 0:256], in0=gq[:, 0:256], scalar1=0.125, scalar2=None, op0=ALU.mult),
         reads=[("gq", s) for s in range(4)], writes=["gqs"])
    gqk = ["gqs", ("gq", 4), ("gq", 5)]
    P.op("gpsimd", lambda e: e.memset(Vaug[:, :, :, 64:66], 1.0), writes=["Vones"])
    for h in range(4):
        P.op("gpsimd", lambda e, h=h: e.memset(QTa[:, h, :], 0.0), writes=["QTz"])

    hTr = cx.ring(2, [128, 8, 512], BF16, "hTb")
    csr = cx.ring(2, [128, 2, 384], F32, "cs")
    pqr = cx.ring(2, [128, 512], F32, "pq", psum=True)
    pcr = cx.ring(2, [128, 512], F32, "pc", psum=True)
    pTr = cx.ring(2, [128, 8, 128], BF16, "pT3", psum=True)
    zbr = cx.ring(2, [128, 512], F32, "zb")
    sqr = cx.ring(2, [128, 384], F32, "sq")
    ssr = cx.ring(2, [128, 6], F32, "ss")
    xnr = cx.ring(2, [128, 384], F32, "xn")
    t1r = cx.ring(2, [128, 384], F32, "t1")
    t2r = cx.ring(2, [128, 384], F32, "t2")
    qbr = cx.ring(2, [128, 384], BF16, "qb")
    ztr = cx.ring(3, [128, 512], F32, "zt")
    hT_v = T["hT_d"].rearrange("(c p) s -> p c s", p=128)
    zc_v = T["zc_d"]
    evs = []
    for blk in range(NB if A_STAGE >= 1 else 0):
        hT, hTk = hTr.next()
        P.dma("sync", lambda e, hT=hT, blk=blk: e.dma_start(out=hT[:], in_=hT_v[:, :, blk * 512:(blk + 1) * 512]), hTk, writes=[hTk])
        for tt in range(4 if A_STAGE >= 2 else 0):
            t = blk * 4 + tt
            cs, csk = csr.next()
            P.dma("sync", lambda e, cs=cs, t=t: e.dma_start(out=cs[:, 0, :], in_=T["cos6"][t * 128:(t + 1) * 128, :]), csk + "a", writes=[csk + "a"])
            P.dma("sync", lambda e, cs=cs, t=t: e.dma_start(out=cs[:, 1, :], in_=T["sin6"][t * 128:(t + 1) * 128, :]), csk + "b", writes=[csk + "b"])
            pq, pqk = pqr.next()
            for kc in range(8):
                P.op("tensor", lambda e, pq=pq, hT=hT, kc=kc, tt=tt: e.matmul(pq[:], lhsT=hT[:, kc, tt * 128:(tt + 1) * 128], rhs=w1[:, kc, 0:512],
                                                                       start=(kc == 0), stop=(kc == 7)),
                     reads=[hTk] + w1k, writes=[pqk])
            zb, zbk = zbr.next()
            P.op("vector", lambda e, zb=zb, pq=pq: e.tensor_tensor(out=zb[:], in0=pq[:], in1=bq[:], op=ALU.add),
                 reads=[pqk] + bqk, writes=[zbk])
            if A_STAGE < 2.15:
                continue
            sq, sqk = sqr.next()
            P.op("gpsimd", lambda e, sq=sq, zb=zb: e.tensor_tensor(out=sq[:], in0=zb[:, 0:384], in1=zb[:, 0:384], op=ALU.mult),
                 reads=[zbk], writes=[sqk])
            ss, ssk = ssr.next()
            P.op("vector", lambda e, ss=ss, sq=sq: e.tensor_reduce(out=ss[:], in_=sq[:].rearrange("p (h d) -> p h d", d=64), axis=AX.X, op=ALU.add),
                 reads=[sqk], writes=[ssk])
            P.op("vector", lambda e, ss=ss: e.tensor_scalar(out=ss[:], in0=ss[:], scalar1=1.0 / 64, scalar2=1e-6, op0=ALU.mult, op1=ALU.add),
                 reads=[ssk], writes=[ssk])
            P.op("scalar", lambda e, ss=ss: e.activation(out=ss[:], in_=ss[:], func=AF.Sqrt), reads=[ssk], writes=[ssk])
            P.op("vector", lambda e, ss=ss: e.reciprocal(out=ss[:], in_=ss[:]), reads=[ssk], writes=[ssk])
            if A_STAGE < 2.25:
                continue
            xn, xnk = xnr.next()
            P.op("vector", lambda e, xn=xn, zb=zb, ss=ss: e.tensor_tensor(
                out=xn[:].rearrange("p (h d) -> p h d", d=64), in0=zb[:, 0:384].rearrange("p (h d) -> p h d", d=64),
                in1=ss[:, :].unsqueeze(2).to_broadcast([128, 6, 64]), op=ALU.mult), reads=[zbk, ssk], writes=[xnk])
            if A_STAGE < 2.35:
                continue
            P.op("gpsimd", lambda e, xn=xn: e.tensor_tensor(out=xn[:], in0=xn[:], in1=gq[:], op=ALU.mult), reads=[xnk] + gqk, writes=[xnk])
            if A_STAGE < 2.45:
                continue
            t1, t1k = t1r.next()
            P.op("vector", lambda e, t1=t1, xn=xn, cs=cs: e.tensor_tensor(out=t1[:], in0=xn[:], in1=cs[:, 0, :], op=ALU.mult),
                 reads=[xnk, csk + "a"], writes=[t1k])
            t2, t2k = t2r.next()
            xv = xn[:].rearrange("p (a b c) -> p a b c", b=2, c=16)
            sv = cs[:, 1, :].rearrange("p (a b c) -> p a b c", b=2, c=16)
            tv = t2[:].rearrange("p (a b c) -> p a b c", b=2, c=16)
            P.op("gpsimd", lambda e, xv=xv, sv=sv, tv=tv: e.tensor_tensor(out=tv[:, :, 0, :], in0=xv[:, :, 1, :], in1=sv[:, :, 0, :], op=ALU.mult),
                 reads=[xnk, csk + "b"], writes=[t2k + "x"])
            P.op("gpsimd", lambda e, xv=xv, sv=sv, tv=tv: e.tensor_tensor(out=tv[:, :, 1, :], in0=xv[:, :, 0, :], in1=sv[:, :, 1, :], op=ALU.mult),
                 reads=[xnk, csk + "b"], writes=[t2k + "y"])
            qb, qbk = qbr.next()
            P.op("vector", lambda e, qb=qb, t1=t1, t2=t2: e.tensor_tensor(out=qb[:], in0=t1[:], in1=t2[:], op=ALU.add),
                 reads=[t1k, t2k + "x", t2k + "y"], writes=[qbk])
            if A_STAGE < 2.55:
                continue
            pT, pTk = pTr.next()
            for c in range(3):
                P.op("tensor", lambda e, pT=pT, qb=qb, c=c: e.transpose(out=pT[:, c, :], in_=qb[:, c * 128:(c + 1) * 128], identity=idb[:]),
                     reads=[qbk, "idb"], writes=[pTk + "_%d" % c])
            for c in range(2):
                for g in range(2):
                    h = 2 * g + c
                    P.op("vector", lambda e, pT=pT, c=c, g=g, h=h, t=t: e.tensor_copy(out=QTa[64 * g:64 * g + 64, h, t * 128:(t + 1) * 128], in_=pT[64 * g:64 * g + 64, c, :]),
                         reads=[pTk + "_%d" % c, "QTz"], writes=[("QT", h, t)])
            P.op("vector", lambda e, pT=pT, t=t: e.tensor_copy(out=KT[:, t * 128:(t + 1) * 128], in_=pT[:, 2, :]),
                 reads=[pTk + "_2"], writes=[("KT", t)])
            P.op("vector", lambda e, zb=zb, t=t: e.tensor_copy(out=Vaug[:, t, :, 0:64], in_=zb[:, 384:512].rearrange("p (g d) -> p g d", d=64)),
                 reads=[zbk], writes=[("V", t)])
        for cc in range(12 if A_STAGE >= 3 else 0):
            pc, pck = pcr.next()
            for kc in range(8):
                P.op("tensor", lambda e, pc=pc, hT=hT, kc=kc, cc=cc: e.matmul(pc[:], lhsT=w1[:, kc, 512 + cc * 128:512 + (cc + 1) * 128], rhs=hT[:, kc, :],
                                                                       start=(kc == 0), stop=(kc == 7)),
                     reads=[hTk] + w1k, writes=[pck])
            zt, ztk = ztr.next()
            fn = AF.Sigmoid if cc in (8, 9) else AF.Identity
            P.op("scalar", lambda e, zt=zt, pc=pc, cc=cc, fn=fn: e.activation(out=zt[:], in_=pc[:], func=fn, bias=bc[:, cc:cc + 1]),
                 reads=[pck, "bc"], writes=[ztk])
            evs.append(P.dma("sync", lambda e, zt=zt, cc=cc, blk=blk: e.dma_start(out=zc_v[cc * 128:(cc + 1) * 128, blk * 512:(blk + 1) * 512], in_=zt[:]),
                             ztk, reads=[ztk]))
    P.flush(evs)
    cx.close()


def phase_B1(nc, P, T, l, QTa, QTb, KT, Vaug):
    cx = Ctx(nc, P)
    idf, idb = load_consts(cx, P, T["ident"])
    pSr = cx.ring(2, [128, 512], F32, "pS", psum=True)
    pO = [cx.ps([128, 512], F32) for _ in range(4)]
    pTr = cx.ring(1, [128, 2, 512], BF16, "pTa", psum=True)
    PTr = cx.ring(3, [128, 512], BF16, "PT")
    atr = cx.ring(2, [128, 4, 256], BF16, "at")
    aTr = cx.ring(2, [128, 2, 512], BF16, "aT")
    rcr = cx.ring(4, [128, 1], F32, "rc")
    Obr = cx.ring(2, [128, 4, 66], F32, "Ob")
    br_v = T["brT_d"].rearrange("(c p) s -> p c s", p=128)
    evs = []
    par = 0
    for qb in range(NB):
        at, atk = atr.next()
        for h in range(4):
            if True:
                g = h // 2
                par ^= 1
                qk = [("QT", h, qb * 4 + i) for i in range(4)]

                def emitS(kc, h=h, qb=qb, qk=qk):
                    pS, pSk = pSr.next()
                    P.op("tensor", lambda e, pS=pS: e.matmul(pS[:], lhsT=KT[:, kc * 128:(kc + 1) * 128],
                                                         rhs=QTa[:, h, qb * 512:(qb + 1) * 512], start=True, stop=True),
                         reads=qk + [("KT", kc)], writes=[pSk])
                    return pS, pSk
                cur = emitS(0)
                for kc in range(64):
                    nxt = emitS(kc + 1) if kc < 63 else None
                    pS, pSk = cur
                    PT, PTk = PTr.next()
                    P.op("scalar", lambda e, PT=PT, pS=pS: e.activation(out=PT[:], in_=pS[:], func=AF.Exp), reads=[pSk], writes=[PTk])
                    for qt in range(4):
                        P.op("tensor", lambda e, PT=PT, qt=qt, kc=kc, g=g: e.matmul(
                            pO[qt][:, 0:65], lhsT=PT[:, qt * 128:(qt + 1) * 128], rhs=Vaug[:, kc, g, 0:65],
                            start=(kc == 0), stop=(kc == 63)),
                            reads=[PTk, ("V", kc), "Vones"], writes=[("pO", qt)])
                    cur = nxt
                Ob, Obk = Obr.next()
                for qt in range(4):
                    P.op("vector", lambda e, Ob=Ob, qt=qt: e.tensor_copy(out=Ob[:, qt, :], in_=pO[qt][:, 0:66]),
                         reads=[("pO", qt)], writes=[(Obk, qt)])
                for qt in range(4):
                    rc, rck = rcr.next()
                    P.op("vector", lambda e, rc=rc, qt=qt, Ob=Ob: e.reciprocal(out=rc[:], in_=Ob[:, qt, 64:65]),
                         reads=[(Obk, qt)], writes=[rck])
                    P.op("vector", lambda e, rc=rc, qt=qt, Ob=Ob, at=at, h=h: e.tensor_scalar(
                        out=at[:, qt, h * 64:(h + 1) * 64], in0=Ob[:, qt, 0:64], scalar1=rc[:, 0:1], scalar2=None, op0=ALU.mult),
                        reads=[(Obk, qt), rck], writes=[(atk, qt, h)])
        pT, pTk = pTr.next()
        for qt in range(4):
            for hf in range(2):
                P.op("tensor", lambda e, pT=pT, at=at, qt=qt, hf=hf: e.transpose(out=pT[:, hf, qt * 128:(qt + 1) * 128], in_=at[:, qt, hf * 128:(hf + 1) * 128], identity=idb[:]),
                     reads=[(atk, qt, hh) for hh in range(4)] + ["idb"], writes=[(pTk, qt, hf)])
        aT, aTk = aTr.next()
        P.op("vector", lambda e, aT=aT, pT=pT: e.tensor_copy(out=aT[:], in_=pT[:]), reads=[(pTk, qt, hf) for qt in range(4) for hf in range(2)], writes=[aTk])
        evs.append(P.dma("sync", lambda e, aT=aT, qb=qb: e.dma_start(out=br_v[:, 0:2, qb * 512:(qb + 1) * 512], in_=aT[:]), aTk, reads=[aTk]))
    P.flush(evs)
    cx.close()


def phase_B2a(nc, P, T, l):
    cx = Ctx(nc, P)
    idf, idb = load_consts(cx, P, T["ident"])
    pmisc = cx.ps([128, 512], F32)
    cst = cx.sb([38, 256], F32)
    P.dma("sync", lambda e: e.dma_start(out=cst[0:31, :], in_=T["cf_conv_w"][l]), "c3", writes=[("cst", 0)])
    P.dma("sync", lambda e: e.dma_start(out=cst[31:34, :], in_=T["sc_conv_w"][l]), "c4", writes=[("cst", 1)])
    for i, nm in enumerate(("cf_conv_b", "cf_ln_g", "cf_ln_b", "pool_scale")):
        P.dma("sync", lambda e, i=i, nm=nm: e.dma_start(out=cst[34 + i:35 + i, :], in_=T[nm][l:l + 1, :]), "c5", writes=[("cst", 2 + i)])
    chp = cx.sb([128, 2, 38], F32)
    for c in range(2):
        P.op("tensor", lambda e, c=c: e.transpose(out=pmisc[:, 64 + c * 64:64 + c * 64 + 38], in_=cst[:, c * 128:(c + 1) * 128], identity=idf[0:38, 0:38]),
             reads=[("cst", i) for i in range(6)] + ["idf"], writes=["pmisc"], mode=64)
        P.op("vector", lambda e, c=c: e.tensor_copy(out=chp[:, c, :], in_=pmisc[:, 64 + c * 64:64 + c * 64 + 38]), reads=["pmisc"], writes=[("chp", c)])
    chk = [("chp", 0), ("chp", 1)]
    pwf = cx.sb([128, 2, 128], F32)
    pwbd = cx.sb([128, 2, 128], BF16)
    P.op("vector", lambda e: e.memset(pwf[:], 0.0), writes=["pwf"])
    for gi in range(4):
        c, hh = gi // 2, gi % 2
        P.dma("sync", lambda e, gi=gi, c=c, hh=hh: e.dma_start(out=pwf[hh * 64:(hh + 1) * 64, c, hh * 64:(hh + 1) * 64], in_=T["pool_w"][l][gi]),
              "c6", reads=[], writes=["pwf"])
    P.op("vector", lambda e: e.tensor_copy(out=pwbd[:], in_=pwf[:]), reads=["pwf"], writes=["pwbd"])
    invc = cx.sb([128, 3, 2, 512], F32)
    P.dma("sync", lambda e: e.dma_start(out=invc[:], in_=T["invc"].rearrange("a p c s -> p a c s")), "c7", writes=["invc"])
    onesf = cx.sb([128, 128], F32)
    P.op("vector", lambda e: e.memset(onesf[:], 1.0), writes=["onesf"])

    zchr = cx.ring(2, [128, 12, ZW], F32, "zch")
    brr = cx.ring(2, [128, 6, 512], BF16, "brT")
    cu = cx.sb([128, 2, ZW], F32)
    ag = cx.sb([128, 2, ZW], F32)
    acc = cx.sb([128, 2, 512], F32)
    ysq = cx.sb([128, 2, 512], F32)
    mean = cx.sb([128, 512], F32)
    rstd = cx.sb([128, 512], F32)
    pC = cx.sb([128, ZW], F32)
    pD = cx.sb([128, ZW], F32)
    Wt = cx.sb([128, 2, 512], F32)
    ypool = cx.sb([128, 2, 512], BF16)
    pMr = cx.ring(4, [128, 512], F32, "pM", psum=True)
    zc_v = T["zc_d"].rearrange("(c p) s -> p c s", p=128)
    br_v = T["brT_d"].rearrange("(c p) s -> p c s", p=128)
    evs = []
    for blk in range(NB):
        zch, zk = zchr.next()
        brT, bk = brr.next()
        zks = [(zk, q4) for q4 in range(4)]
        lo = max(0, blk * 512 - HALO)
        hi = min(S, blk * 512 + 512 + HALO)
        off = lo - (blk * 512 - HALO)
        for q4 in range(4):
            P.dma("sync", lambda e, zch=zch, lo=lo, hi=hi, off=off, q4=q4: e.dma_start(out=zch[:, 3 * q4:3 * q4 + 3, off:off + hi - lo], in_=zc_v[:, 3 * q4:3 * q4 + 3, lo:hi]),
                  zk + "_%d" % q4, writes=[(zk, q4)])
        if blk == 0:
            P.op("gpsimd", lambda e, zch=zch: e.memset(zch[:, :, 0:HALO], 0.0), writes=[(zk, 4)])
            zks = zks + [(zk, 4)]
        if blk == NB - 1:
            P.op("gpsimd", lambda e, zch=zch: e.memset(zch[:, :, ZW - HALO:ZW], 0.0), writes=[(zk, 4)])
            zks = zks + [(zk, 4)]
        P.op("gpsimd", lambda e, zch=zch: e.tensor_tensor(out=cu[:], in0=zch[:, 2:4, :], in1=zch[:, 4:6, :], op=ALU.mult), reads=zks, writes=["cu"])
        for c in range(2):
            P.op("vector", lambda e, c=c: e.tensor_scalar(out=acc[:, c, :], in0=cu[:, c, 15:527], scalar1=chp[:, c, 31:32], scalar2=None, op0=ALU.mult),
                 reads=["cu"] + chk, writes=[("acc", c)])
            for k in (1, 2):
                P.op("vector", lambda e, c=c, k=k: e.scalar_tensor_tensor(out=acc[:, c, :], in0=cu[:, c, 15 + k:527 + k], scalar=chp[:, c, 31 + k:32 + k],
                                                                      in1=acc[:, c, :], op0=ALU.mult, op1=ALU.add),
                     reads=["cu", ("acc", c)] + chk, writes=[("acc", c)])
            P.op("vector", lambda e, c=c, zch=zch, brT=brT: e.tensor_tensor(out=brT[:, c, :], in0=acc[:, c, :], in1=zch[:, c, HALO:HALO + 512], op=ALU.mult),
                 reads=[("acc", c)] + zks, writes=[(bk, c)])
        P.op("gpsimd", lambda e, zch=zch: e.tensor_tensor(out=ag[:], in0=zch[:, 6:8, :], in1=zch[:, 8:10, :], op=ALU.mult), reads=zks, writes=["ag"])
        for c in range(2):
            P.op("vector", lambda e, c=c: e.tensor_scalar(out=acc[:, c, :], in0=ag[:, c, 1:513], scalar1=chp[:, c, 0:1], scalar2=chp[:, c, 34:35],
                                                      op0=ALU.mult, op1=ALU.add),
                 reads=["ag"] + chk, writes=[("acc", c)])
            for k in range(1, 31):
                P.op("vector", lambda e, c=c, k=k: e.scalar_tensor_tensor(out=acc[:, c, :], in0=ag[:, c, 1 + k:513 + k], scalar=chp[:, c, k:k + 1],
                                                                      in1=acc[:, c, :], op0=ALU.mult, op1=ALU.add),
                     reads=["ag", ("acc", c)] + chk, writes=[("acc", c)])
        P.op("gpsimd", lambda e: e.tensor_tensor(out=ysq[:], in0=acc[:], in1=acc[:], op=ALU.mult), reads=[("acc", 0), ("acc", 1)], writes=["ysq"])
        pS, pSk = pMr.next()
        pQ, pQk = pMr.next()
        for c in range(2):
            P.op("tensor", lambda e, c=c, pS=pS: e.matmul(pS[:], lhsT=onesf[:], rhs=acc[:, c, :], start=(c == 0), stop=(c == 1)),
                 reads=["onesf", ("acc", c)], writes=[pSk])
        for c in range(2):
            P.op("tensor", lambda e, c=c, pQ=pQ: e.matmul(pQ[:], lhsT=onesf[:], rhs=ysq[:, c, :], start=(c == 0), stop=(c == 1)),
                 reads=["onesf", "ysq"], writes=[pQk])
        P.op("scalar", lambda e, pS=pS: e.activation(out=mean[:], in_=pS[:], func=AF.Identity, scale=1.0 / 256), reads=[pSk], writes=["mean"])
        P.op("gpsimd", lambda e: e.tensor_tensor(out=rstd[:], in0=mean[:], in1=mean[:], op=ALU.mult), reads=["mean"], writes=["rstd"])
        P.op("vector", lambda e, pQ=pQ: e.scalar_tensor_tensor(out=rstd[:], in0=pQ[:], scalar=1.0 / 256, in1=rstd[:], op0=ALU.mult, op1=ALU.subtract),
             reads=[pQk, "rstd"], writes=["rstd"])
        P.op("vector", lambda e: e.tensor_scalar(out=rstd[:], in0=rstd[:], scalar1=LN_EPS, scalar2=None, op0=ALU.add), reads=["rstd"], writes=["rstd"])
        P.op("scalar", lambda e: e.activation(out=rstd[:], in_=rstd[:], func=AF.Sqrt), reads=["rstd"], writes=["rstd"])
        P.op("vector", lambda e: e.reciprocal(out=rstd[:], in_=rstd[:]), reads=["rstd"], writes=["rstd"])
        for c in range(2):
            P.op("vector", lambda e, c=c: e.tensor_tensor(out=acc[:, c, :], in0=acc[:, c, :], in1=mean[:], op=ALU.subtract),
                 reads=[("acc", c), "mean", pSk], writes=[("acc", c)])
            P.op("vector", lambda e, c=c: e.tensor_tensor(out=acc[:, c, :], in0=acc[:, c, :], in1=rstd[:], op=ALU.mult),
                 reads=[("acc", c), "rstd"], writes=[("acc", c)])
            P.op("scalar", lambda e, c=c, brT=brT: e.activation(out=brT[:, 2 + c, :], in_=acc[:, c, :], func=AF.Silu, scale=chp[:, c, 35:36], bias=chp[:, c, 36:37]),
                 reads=[("acc", c)] + chk, writes=[(bk, 2 + c)])
        P.op("gpsimd", lambda e, zch=zch: e.tensor_tensor(out=cu[:, :, 1:ZW], in0=zch[:, 10:12, 0:ZW - 1], in1=zch[:, 10:12, 1:ZW], op=ALU.add),
             reads=zks, writes=["cu"])
        P.op("gpsimd", lambda e: e.tensor_tensor(out=ag[:, :, 2:ZW - 1], in0=cu[:, :, 1:ZW - 2], in1=cu[:, :, 3:ZW], op=ALU.add),
             reads=["cu"], writes=["ag"])
        P.op("gpsimd", lambda e: e.tensor_tensor(out=pC[:, 4:ZW - 3], in0=ag[:, 1, 2:ZW - 5], in1=ag[:, 1, 6:ZW - 1], op=ALU.add),
             reads=["ag"], writes=["pC"])
        P.op("gpsimd", lambda e: e.tensor_tensor(out=pD[:, 8:ZW - 7], in0=pC[:, 4:ZW - 11], in1=pC[:, 12:ZW - 3], op=ALU.add),
             reads=["pC"], writes=["pD"])
        ty = 0 if blk == 0 else (2 if blk == NB - 1 else 1)
        srcs = [(cu, 0, 0), (ag, 0, 1), (pC, None, 0), (pD, None, 1)]
        for gi, (src, cidx, hh) in enumerate(srcs):
            c = gi // 2
            sl = slice(hh * 64, (hh + 1) * 64)
            sap = src[sl, cidx, HALO:HALO + 512] if cidx is not None else src[sl, HALO:HALO + 512]
            P.op("vector", lambda e, sap=sap, sl=sl, c=c, ty=ty: e.tensor_tensor(out=Wt[sl, c, :], in0=sap, in1=invc[sl, ty, c, :], op=ALU.mult),
                 reads=["cu", "ag", "pC", "pD", "invc"], writes=[("Wt", gi)])
        P.op("vector", lambda e, zch=zch: e.tensor_tensor(out=ypool[:], in0=Wt[:], in1=zch[:, 10:12, HALO:HALO + 512], op=ALU.subtract),
             reads=[("Wt", gi) for gi in range(4)] + zks, writes=["ypool"])
        for c in range(2):
            pP, pPk = pMr.next()
            P.op("tensor", lambda e, c=c, pP=pP: e.matmul(pP[:], lhsT=pwbd[:, c, :], rhs=ypool[:, c, :], start=True, stop=True),
                 reads=["pwbd", "ypool"], writes=[pPk])
            P.op("scalar", lambda e, c=c, pP=pP, brT=brT: e.activation(out=brT[:, 4 + c, :], in_=pP[:], func=AF.Identity, scale=chp[:, c, 37:38]),
                 reads=[pPk] + chk, writes=[(bk, 4 + c)])
        evs.append(P.dma("sync", lambda e, brT=brT, blk=blk: e.dma_start(out=br_v[:, 2:8, blk * 512:(blk + 1) * 512], in_=brT[:]), bk,
                         reads=[(bk, i) for i in range(6)]))
    P.flush(evs)
    cx.close()


def phase_B2b(nc, P, T, l, Dall, Wall):
    cx = Ctx(nc, P)
    idf, idb = load_consts(cx, P, T["ident"])
    wv = T["w_in"][l].rearrange("(c p) n -> p c n", p=128)
    wg = cx.sb([128, 8, 4096], BF16)
    for i in range(2):
        P.dma("gpsimd", lambda e, i=i: e.dma_start(out=wg[:, :, i * 2048:(i + 1) * 2048], in_=wv[:, :, 2048 + i * 2048:2048 + (i + 1) * 2048]),
              "wg%d" % i, writes=[("wg", i)])
    wgk = [("wg", 0), ("wg", 1)]
    wbr = cx.sb([128, 8, 1024], BF16)
    P.dma("gpsimd", lambda e: e.dma_start(out=wbr[:], in_=T["w_branch"][l].rearrange("g (c p) n -> p (g c) n", p=128)), "wbr", writes=["wbr"])
    wo = cx.sb([128, 8, 1024], BF16)
    P.dma("gpsimd", lambda e: e.dma_start(out=wo[:], in_=T["w_out"][l].rearrange("(c p) n -> p c n", p=128)), "wo", writes=["wo"])
    wr = cx.sb([128, 8, 32], BF16)
    P.dma("gpsimd", lambda e: e.dma_start(out=wr[:], in_=T["w_router"][l].rearrange("(c p) n -> p c n", p=128)), "wr", writes=["wr"])
    brb = cx.sb([128, 32], F32)
    P.dma("sync", lambda e: e.dma_start(out=brb[:], in_=T["b_router"][l].partition_broadcast(128)), "c3", writes=["brb"])
    gst = cx.sb([32, 128], F32)
    bg = cx.sb([128, 32], F32)
    P.dma("sync", lambda e: e.dma_start(out=gst[:], in_=T["b_in"][l][2048:6144].rearrange("(c p) -> c p", p=128)), "c4", writes=["gst"])
    pmisc = cx.ps([128, 512], F32)
    P.op("tensor", lambda e: e.transpose(out=pmisc[:, 0:32], in_=gst[:], identity=idf[0:32, 0:32]), reads=["gst", "idf"], writes=["pmisc"], mode=32)
    P.op("vector", lambda e: e.tensor_copy(out=bg[:], in_=pmisc[:, 0:32]), reads=["pmisc"], writes=["bg"])
    onesb = cx.sb([128, 128], BF16)
    P.op("vector", lambda e: e.memset(onesb[:], 1.0), writes=["onesb"])
    Uf = cx.sb([128, 128], F32)
    Ub = cx.sb([128, 128], BF16)
    P.dma("sync", lambda e: e.dma_start(out=Uf[:], in_=T["utri"][:, :]), "c8", writes=["Uf"])
    P.op("vector", lambda e: e.tensor_copy(out=Ub[:], in_=Uf[:]), reads=["Uf"], writes=["Ub"])
    iotaC = cx.sb([128, 32], F32)
    P.dma("sync", lambda e: e.dma_start(out=iotaC[:], in_=T["iotac"][:, :]), "c9", writes=["iotaC"])
    cnt = cx.sb([128, 32], F32)
    P.op("vector", lambda e: e.memset(cnt[:], 0.0), writes=["cnt"])
    gt, bt = load_ln_params(P, cx, T["ln1_g"][l], T["ln1_b"][l])

    hTr = cx.ring(2, [128, 8, 512], BF16, "hTb")
    brr = cx.ring(2, [128, 8, 512], BF16, "brT")
    gtr = cx.ring(2, [128, 512], BF16, "gate")
    tmr = cx.ring(2, [128, 512], F32, "tmp")
    acr = cx.ring(2, [128, 512], F32, "macc")
    mT = cx.sb([128, 8, 512], BF16)
    pGr = cx.ring(2, [128, 512], F32, "pG", psum=True)
    pJr = cx.ring(2, [128, 512], F32, "pJ", psum=True)
    pMr = cx.ring(2, [128, 512], F32, "pM", psum=True)
    pTr = cx.ring(1, [128, 8, 128], BF16, "pT8", psum=True)
    htr = cx.ring(2, [128, 1024], F32, "ht")
    ofr = cx.ring(2, [128, 1024], F32, "of")
    obr = cx.ring(2, [128, 1024], BF16, "ob")
    h1Tr = cx.ring(2, [128, 8, 128], BF16, "h1T")
    smr = ln_scratch(cx, 1)
    lgr = cx.ring(2, [128, 32], F32, "lg")
    t8r = cx.ring(2, [128, 8], F32, "t8")
    mkr = cx.ring(2, [128, 32], BF16, "mk")
    dfr = cx.ring(2, [128, 32], F32, "df")
    slr = cx.ring(2, [128, 32], F32, "sl")
    s1r = cx.ring(2, [128, 4], F32, "s1")
    dkr = cx.ring(2, [128, 4], F32, "dk")
    hT_v = T["hT_d"].rearrange("(c p) s -> p c s", p=128)
    br_v = T["brT_d"].rearrange("(c p) s -> p c s", p=128)
    Xe = T["Xe_d"]
    evs = []
    for blk in range(NB):
        hT, hTk = hTr.next()
        P.dma("sync", lambda e, hT=hT, blk=blk: e.dma_start(out=hT[:], in_=hT_v[:, :, blk * 512:(blk + 1) * 512]), hTk, writes=[hTk])
        brT, bk = brr.next()
        P.dma("sync", lambda e, brT=brT, blk=blk: e.dma_start(out=brT[:], in_=br_v[:, :, blk * 512:(blk + 1) * 512]), bk, writes=[bk])
        for oc in range(8):
            ma, mak = acr.next()
            for g in range(4):
                pG, pGk = pGr.next()
                for kc in range(8):
                    P.op("tensor", lambda e, pG=pG, kc=kc, g=g, oc=oc, hT=hT: e.matmul(
                        pG[:], lhsT=wg[:, kc, g * 1024 + oc * 128:g * 1024 + (oc + 1) * 128], rhs=hT[:, kc, :], start=(kc == 0), stop=(kc == 7)),
                        reads=[hTk] + wgk, writes=[pGk])
                gate, gtk = gtr.next()
                P.op("scalar", lambda e, gate=gate, pG=pG, g=g, oc=oc: e.activation(out=gate[:], in_=pG[:], func=AF.Sigmoid, bias=bg[:, g * 8 + oc:g * 8 + oc + 1]),
                     reads=[pGk, "bg"], writes=[gtk])
                pJ, pJk = pJr.next()
                for c in range(2):
                    P.op("tensor", lambda e, pJ=pJ, c=c, g=g, oc=oc, brT=brT: e.matmul(
                        pJ[:], lhsT=wbr[:, g * 2 + c, oc * 128:(oc + 1) * 128], rhs=brT[:, g * 2 + c, :], start=(c == 0), stop=(c == 1)),
                        reads=["wbr", bk], writes=[pJk])
                if g == 0:
                    P.op("vector", lambda e, ma=ma, gate=gate, pJ=pJ: e.tensor_tensor(out=ma[:], in0=gate[:], in1=pJ[:], op=ALU.mult),
                         reads=[gtk, pJk], writes=[mak])
                else:
                    tm, tmk = tmr.next()
                    P.op("vector", lambda e, tm=tm, gate=gate, pJ=pJ: e.tensor_tensor(out=tm[:], in0=gate[:], in1=pJ[:], op=ALU.mult),
                         reads=[gtk, pJk], writes=[tmk])
                    if g < 3:
                        P.op("gpsimd", lambda e, ma=ma, tm=tm: e.tensor_tensor(out=ma[:], in0=ma[:], in1=tm[:], op=ALU.add), reads=[mak, tmk], writes=[mak])
                    else:
                        P.op("gpsimd", lambda e, ma=ma, tm=tm, oc=oc: e.tensor_tensor(out=mT[:, oc, :], in0=ma[:], in1=tm[:], op=ALU.add),
                             reads=[mak, tmk], writes=[("mT", oc)])
        for tt in range(4):
            t = blk * 4 + tt
            ht, htk = htr.next()
            P.dma("sync", lambda e, ht=ht, t=t: e.dma_start(out=ht[:], in_=T["h_d"][t * 128:(t + 1) * 128, :]), htk, writes=[htk])
            for hf in range(2):
                pM, pMk = pMr.next()
                for kc in range(8):
                    P.op("tensor", lambda e, pM=pM, kc=kc, tt=tt, hf=hf: e.matmul(
                        pM[:], lhsT=mT[:, kc, tt * 128:(tt + 1) * 128], rhs=wo[:, kc, hf * 512:(hf + 1) * 512], start=(kc == 0), stop=(kc == 7)),
                        reads=[("mT", kc), "wo"], writes=[pMk])
                P.op("vector", lambda e, ht=ht, pM=pM, hf=hf: e.scalar_tensor_tensor(
                    out=ht[:, hf * 512:(hf + 1) * 512], in0=ht[:, hf * 512:(hf + 1) * 512], scalar=DN_ALPHA, in1=pM[:], op0=ALU.mult, op1=ALU.add),
                    reads=[htk, pMk], writes=[htk])
            of, ofk = ofr.next()
            ob, obk = obr.next()
            ln_core(P, cx, ht, htk, gt, bt, of, ofk, ob, obk, smr.next())
            evs.append(P.dma("sync", lambda e, of=of, t=t: e.dma_start(out=T["h1_d"][t * 128:(t + 1) * 128, :], in_=of[:]), ofk, reads=[ofk]))
            pT, pTk = pTr.next()
            for c in range(8):
                P.op("tensor", lambda e, c=c, pT=pT, ob=ob: e.transpose(out=pT[:, c, :], in_=ob[:, c * 128:(c + 1) * 128], identity=idb[:]),
                     reads=[obk, "idb"], writes=[pTk + "_%d" % c])
            h1T, h1Tk = h1Tr.next()
            P.op("vector", lambda e, h1T=h1T, pT=pT: e.tensor_copy(out=h1T[:], in_=pT[:]), reads=[pTk + "_%d" % c for c in range(8)], writes=[h1Tk])
            for kc in range(8):
                P.op("tensor", lambda e, kc=kc, h1T=h1T: e.matmul(pmisc[:, 0:32], lhsT=h1T[:, kc, :], rhs=wr[:, kc, :], start=(kc == 0), stop=(kc == 7)),
                     reads=[h1Tk, "wr"], writes=["pmisc"])
            lg, lgk = lgr.next()
            P.op("vector", lambda e, lg=lg: e.tensor_tensor(out=lg[:], in0=pmisc[:, 0:32], in1=brb[:], op=ALU.add), reads=["pmisc", "brb"], writes=[lgk])
            t8, t8k = t8r.next()
            P.op("vector", lambda e, t8=t8, lg=lg: e.max(out=t8[:], in_=lg[:]), reads=[lgk], writes=[t8k])
            mk, mkk = mkr.next()
            P.op("vector", lambda e, mk=mk, lg=lg, t8=t8: e.tensor_scalar(out=mk[:], in0=lg[:], scalar1=t8[:, 3:4], scalar2=None, op0=ALU.is_ge),
                 reads=[lgk, t8k], writes=[mkk])
            s1, s1k = s1r.next()
            P.op("vector", lambda e, s1=s1, t8=t8: e.tensor_scalar(out=s1[:, 0:1], in0=t8[:, 0:1], scalar1=-1.0, scalar2=None, op0=ALU.mult),
                 reads=[t8k], writes=[s1k + "n"])
            P.op("scalar", lambda e, s1=s1, t8=t8, t=t: e.activation(out=Wall[:, t, :], in_=t8[:, 0:4], func=AF.Exp, bias=s1[:, 0:1], accum_out=s1[:, 1:2]),
                 reads=[t8k, s1k + "n"], writes=[("Wall", t), s1k + "s"])
            P.op("vector", lambda e, s1=s1: e.reciprocal(out=s1[:, 2:3], in_=s1[:, 1:2]), reads=[s1k + "s"], writes=[s1k + "r"])
            P.op("vector", lambda e, s1=s1, t=t: e.tensor_scalar(out=Wall[:, t, :], in0=Wall[:, t, :], scalar1=s1[:, 2:3], scalar2=None, op0=ALU.mult),
                 reads=[("Wall", t), s1k + "r"], writes=[("Wall", t)])
            P.op("tensor", lambda e, mk=mk: e.matmul(pmisc[:, 64:96], lhsT=Ub[:], rhs=mk[:], start=True, stop=True), reads=["Ub", mkk], writes=["pmisc"])
            P.op("tensor", lambda e, mk=mk: e.matmul(pmisc[:, 128:160], lhsT=onesb[:], rhs=mk[:], start=True, stop=True), reads=["onesb", mkk], writes=["pmisc"])
            df, dfk = dfr.next()
            P.op("vector", lambda e, df=df: e.tensor_tensor(out=df[:], in0=pmisc[:, 64:96], in1=cnt[:], op=ALU.add), reads=["pmisc", "cnt"], writes=[dfk])
            P.op("vector", lambda e: e.tensor_tensor(out=cnt[:], in0=pmisc[:, 128:160], in1=cnt[:], op=ALU.add), reads=["pmisc", "cnt"], writes=["cnt"])
            sl, slk = slr.next()
            P.op("vector", lambda e, sl=sl, df=df: e.tensor_scalar(out=sl[:], in0=df[:], scalar1=float(CAP), scalar2=1.0e6, op0=ALU.is_ge, op1=ALU.mult),
                 reads=[dfk], writes=[slk])
            P.op("vector", lambda e, sl=sl, df=df: e.tensor_tensor(out=df[:], in0=df[:], in1=sl[:], op=ALU.add), reads=[dfk, slk], writes=[dfk])
            P.op("vector", lambda e, df=df: e.tensor_tensor(out=df[:], in0=df[:], in1=iotaC[:], op=ALU.add), reads=[dfk, "iotaC"], writes=[dfk])
            dk, dkk = dkr.next()
            for k in range(4):
                P.op("vector", lambda e, sl=sl, lg=lg, t8=t8, k=k: e.tensor_scalar(out=sl[:], in0=lg[:], scalar1=t8[:, k:k + 1], scalar2=None, op0=ALU.is_equal),
                     reads=[lgk, t8k, dfk], writes=[slk])
                P.op("vector", lambda e, sl=sl, df=df: e.tensor_tensor(out=sl[:], in0=sl[:], in1=df[:], op=ALU.mult), reads=[slk, dfk], writes=[slk])
                P.op("vector", lambda e, sl=sl, dk=dk, k=k: e.tensor_reduce(out=dk[:, k:k + 1], in_=sl[:], axis=AX.X, op=ALU.add), reads=[slk], writes=[(dkk, k)])
            P.op("vector", lambda e, dk=dk, t=t: e.tensor_copy(out=Dall[:, t, :], in_=dk[:]), reads=[(dkk, k) for k in range(4)], writes=[("Dall", t)])
            for k in range(4):
                evs.append(P.dma("gpsimd", lambda e, ob=ob, t=t, k=k: e.indirect_dma_start(
                    out=Xe[:, :], out_offset=bass.IndirectOffsetOnAxis(ap=Dall[:, t, k:k + 1], axis=0), in_=ob[:], in_offset=None,
                    bounds_check=bc_reg(e, P), oob_is_err=False), obk + "sc", reads=[obk, ("Dall", t)]))
    P.flush(evs)
    cx.close()


def phase_C(nc, P, T, l):
    cx = Ctx(nc, P)
    idf, idb = load_consts(cx, P, T["ident"])
    gst = cx.sb([32, 2048], F32)
    bgu = cx.sb([128, 16, 32], F32)
    P.dma("sync", lambda e: e.dma_start(out=gst[:], in_=T["b_gate_up"][l]), "c1", writes=["gst"])
    pmisc = cx.ps([128, 16, 32], F32)
    for c in range(16):
        P.op("tensor", lambda e, c=c: e.transpose(out=pmisc[:, c, :], in_=gst[:, c * 128:(c + 1) * 128], identity=idf[0:32, 0:32]),
             reads=["gst", "idf"], writes=["pmisc"], mode=32)
    P.op("vector", lambda e: e.tensor_copy(out=bgu[:], in_=pmisc[:]), reads=["pmisc"], writes=["bgu"])
    wgr = cx.ring(2, [128, 8, 2048], BF16, "wgu")
    wdr = cx.ring(2, [128, 8, 1024], BF16, "wdn")
    bdr = cx.ring(2, [128, 1024], F32, "bd")
    xr = cx.ring(4, [128, 1024], BF16, "xs")
    XTr = cx.ring(2, [128, 8, 512], BF16, "XT")
    aTr = cx.ring(2, [128, 8, 512], BF16, "actT")
    pTr = cx.ring(1, [128, 8, 128], BF16, "pT8", psum=True)
    pGr = cx.ring(2, [128, 512], F32, "pG", psum=True)
    pLr = cx.ring(2, [128, 512], F32, "pL", psum=True)
    pYr = cx.ring(2, [128, 512], F32, "pY", psum=True)
    ggr = cx.ring(3, [128, 512], F32, "gg")
    sgr = cx.ring(3, [128, 512], F32, "sg")
    llr = cx.ring(3, [128, 512], F32, "ll")
    yr = cx.ring(2, [128, 1024], F32, "y")
    Xe, Ye = T["Xe_d"], T["Ye_d"]
    evs = []

    def load_w(e_i):
        wgu, wguk = wgr.next()
        wdn, wdnk = wdr.next()
        bd, bdk = bdr.next()
        src = T["w_gate_up"][l][e_i].rearrange("(c p) n -> p c n", p=128)
        for hf in range(2):
            P.dma("gpsimd", lambda e, wgu=wgu, src=src, hf=hf: e.dma_start(out=wgu[:, 4 * hf:4 * hf + 4, :], in_=src[:, 4 * hf:4 * hf + 4, :]),
                  wguk + "_%d" % hf, writes=[(wguk, hf)])
        P.dma("gpsimd", lambda e, wdn=wdn, e_i=e_i: e.dma_start(out=wdn[:], in_=T["w_down"][l][e_i].rearrange("(c p) n -> p c n", p=128)), wdnk, writes=[wdnk])
        P.dma("sync", lambda e, bd=bd, e_i=e_i: e.dma_start(out=bd[:], in_=T["b_down"][l][e_i].partition_broadcast(128)), bdk, writes=[bdk])
        return (wgu, wguk, wdn, wdnk, bd, bdk)

    NBLK = NE * (CAP // 512)
    BPE = CAP // 512
    W = {}
    W[0] = load_w(0)
    blkst = {}

    xload = {}

    def LOADX(b):
        base = b * 512
        lst = []
        for st in range(4):
            xs, xsk = xr.next()
            P.dma("sync", lambda e, xs=xs, base=base, st=st: e.dma_start(out=xs[:], in_=Xe[base + st * 128:base + (st + 1) * 128, :]), xsk, writes=[xsk])
            lst.append((xs, xsk))
        xload[b] = lst

    def PREP(b):
        XT, XTk = XTr.next()
        for st, (xs, xsk) in enumerate(xload.pop(b)):
            pT, pTk = pTr.next()
            for c in range(8):
                P.op("tensor", lambda e, c=c, pT=pT, xs=xs: e.transpose(out=pT[:, c, :], in_=xs[:, c * 128:(c + 1) * 128], identity=idb[:]),
                     reads=[xsk, "idb"], writes=[pTk + "_%d" % c])
            P.op("vector", lambda e, XT=XT, pT=pT, st=st: e.tensor_copy(out=XT[:, :, st * 128:(st + 1) * 128], in_=pT[:]),
                 reads=[pTk + "_%d" % c for c in range(8)], writes=[(XTk, st)])
        aT, aTk = aTr.next()
        blkst[b] = dict(XT=XT, XTk=XTk, aT=aT, aTk=aTk, pend=None)

    def GU(b, fcs):
        ex = b // BPE
        if b % BPE == 0 and fcs[0] == 2 and ex + 1 < NE:
            W[ex + 1] = load_w(ex + 1)
        wgu, wguk, wdn, wdnk, bd, bdk = W[ex]
        wk = [(wguk, 0), (wguk, 1)]
        st_ = blkst[b]
        XT, XTk, aT, aTk = st_["XT"], st_["XTk"], st_["aT"], st_["aTk"]
        XTks = [(XTk, st) for st in range(4)]
        for fc in fcs:
            pG, pGk = pGr.next()
            pL, pLk = pLr.next()
            for kc in range(8):
                P.op("tensor", lambda e, pG=pG, kc=kc, fc=fc, XT=XT, wgu=wgu: e.matmul(pG[:], lhsT=wgu[:, kc, fc * 128:(fc + 1) * 128], rhs=XT[:, kc, :],
                                                                               start=(kc == 0), stop=(kc == 7)),
                     reads=XTks + wk, writes=[pGk])
            for kc in range(8):
                P.op("tensor", lambda e, pL=pL, kc=kc, fc=fc, XT=XT, wgu=wgu: e.matmul(pL[:], lhsT=wgu[:, kc, 1024 + fc * 128:1024 + (fc + 1) * 128], rhs=XT[:, kc, :],
                                                                               start=(kc == 0), stop=(kc == 7)),
                     reads=XTks + wk, writes=[pLk])
            gg, ggk = ggr.next()
            sg, sgk = sgr.next()
            ll, llk = llr.next()
            P.op("scalar", lambda e, ll=ll, pL=pL, fc=fc, ex=ex: e.activation(out=ll[:], in_=pL[:], func=AF.Identity, bias=bgu[:, 8 + fc, ex:ex + 1]),
                 reads=[pLk, "bgu"], writes=[llk])
            P.op("vector", lambda e, gg=gg, pG=pG, fc=fc, ex=ex: e.tensor_scalar(out=gg[:], in0=pG[:], scalar1=bgu[:, fc, ex:ex + 1], scalar2=7.0, op0=ALU.add, op1=ALU.min),
                 reads=[pGk, "bgu"], writes=[ggk])
            P.op("scalar", lambda e, sg=sg, gg=gg: e.activation(out=sg[:], in_=gg[:], func=AF.Sigmoid, scale=1.702), reads=[ggk], writes=[sgk])
            P.op("gpsimd", lambda e, ll=ll: e.tensor_scalar(out=ll[:], in0=ll[:], scalar1=7.0, scalar2=-7.0, op0=ALU.min, op1=ALU.max), reads=[llk], writes=[llk])
            if st_["pend"] is not None:
                st_["pend"]()

            def fin(gg=gg, ggk=ggk, sg=sg, sgk=sgk, ll=ll, llk=llk, fc=fc, aT=aT, aTk=aTk):
                P.op("gpsimd", lambda e: e.tensor_tensor(out=gg[:], in0=gg[:], in1=sg[:], op=ALU.mult), reads=[ggk, sgk], writes=[ggk])
                P.op("vector", lambda e: e.scalar_tensor_tensor(out=aT[:, fc, :], in0=ll[:], scalar=1.0, in1=gg[:], op0=ALU.add, op1=ALU.mult),
                     reads=[ggk, llk], writes=[(aTk, fc)])
            st_["pend"] = fin
        if fcs[-1] == 7:
            st_["pend"]()
            st_["pend"] = None

    def DOWN(b):
        ex = b // BPE
        base = b * 512
        wgu, wguk, wdn, wdnk, bd, bdk = W[ex]
        st_ = blkst.pop(b)
        aT, aTk = st_["aT"], st_["aTk"]
        aTks = [(aTk, fc) for fc in range(8)]
        for st in range(4):
            y, yk = yr.next()
            for hf in range(2):
                pY, pYk = pYr.next()
                for fc in range(8):
                    P.op("tensor", lambda e, pY=pY, fc=fc, st=st, hf=hf, aT=aT, wdn=wdn: e.matmul(
                        pY[:], lhsT=aT[:, fc, st * 128:(st + 1) * 128], rhs=wdn[:, fc, hf * 512:(hf + 1) * 512], start=(fc == 0), stop=(fc == 7)),
                        reads=aTks + [wdnk], writes=[pYk])
                if hf == 0:
                    P.op("vector", lambda e, y=y, pY=pY, hf=hf, bd=bd: e.tensor_tensor(out=y[:, hf * 512:(hf + 1) * 512], in0=pY[:], in1=bd[:, hf * 512:(hf + 1) * 512], op=ALU.add),
                         reads=[pYk, bdk], writes=[(yk, hf)])
                else:
                    P.op("scalar", lambda e, y=y, pY=pY, hf=hf: e.activation(out=y[:, hf * 512:(hf + 1) * 512], in_=pY[:], func=AF.Identity),
                         reads=[pYk], writes=[(yk, "t")])
                    P.op("gpsimd", lambda e, y=y, hf=hf, bd=bd: e.tensor_tensor(out=y[:, hf * 512:(hf + 1) * 512], in0=y[:, hf * 512:(hf + 1) * 512], in1=bd[:, hf * 512:(hf + 1) * 512], op=ALU.add),
                         reads=[(yk, "t"), bdk], writes=[(yk, hf)])
            evs.append(P.dma("sync", lambda e, y=y, base=base, st=st: e.dma_start(out=Ye[base + st * 128:base + (st + 1) * 128, :], in_=y[:]), yk,
                             reads=[(yk, 0), (yk, 1)]))

    LOADX(0)
    PREP(0)
    for b in range(NBLK + 1):
        if b + 1 < NBLK:
            LOADX(b + 1)
        if b < NBLK:
            GU(b, [0, 1])
        if b >= 1:
            DOWN(b - 1)
        if b < NBLK:
            GU(b, [2, 3])
            if b + 1 < NBLK:
                PREP(b + 1)
            GU(b, [4, 5, 6, 7])
    P.flush(evs)
    cx.close()


def phase_D(nc, P, T, l, Dall, Wall, last):
    cx = Ctx(nc, P)
    idf, idb = load_consts(cx, P, T["ident"])
    gt, bt = load_ln_params(P, cx, T["ln2_g"][l], T["ln2_b"][l])
    ykr = [cx.ring(3, [128, 1024], F32, "yk%d_" % k) for k in range(4)]
    htr = cx.ring(3, [128, 1024], F32, "ht")
    rr = cx.ring(2, [128, 1024], F32, "r")
    ofr = cx.ring(2, [128, 1024], F32, "of")
    obr = cx.ring(2, [128, 1024], BF16, "ob")
    pTr = cx.ring(2, [128, 8, 128], BF16, "pT", psum=True)
    hTr = cx.ring(2, [128, 8, 128], BF16, "hT")
    smr = ln_scratch(cx)
    Ye = T["Ye_d"]
    evs = []
    pre = {}

    def prefetch(t):
        ys = []
        for k in range(4):
            yk_, ykk = ykr[k].next()
            P.op("gpsimd", lambda e, yk_=yk_: e.memset(yk_[:], 0.0), writes=[ykk])
            P.dma("gpsimd", lambda e, yk_=yk_, t=t, k=k: e.indirect_dma_start(
                out=yk_[:], out_offset=None, in_=Ye[:, :], in_offset=bass.IndirectOffsetOnAxis(ap=Dall[:, t, k:k + 1], axis=0),
                bounds_check=bc_reg(e, P), oob_is_err=False), ykk, writes=[ykk])
            ys.append((yk_, ykk))
        ht, htk = htr.next()
        P.dma("sync", lambda e, ht=ht, t=t: e.dma_start(out=ht[:], in_=T["h1_d"][t * 128:(t + 1) * 128, :]), htk, writes=[htk])
        pre[t] = (ys, ht, htk)

    prefetch(0)
    prefetch(1)
    for t in range(NT):
        if t + 2 < NT:
            prefetch(t + 2)
        ys, ht, htk = pre.pop(t)
        r, rk = rr.next()
        P.op("vector", lambda e, r=r, ht=ht: e.tensor_scalar(out=r[:], in0=ht[:], scalar1=DN_ALPHA, scalar2=None, op0=ALU.mult), reads=[htk], writes=[rk])
        for k in range(4):
            yk_, ykk = ys[k]
            P.op("vector", lambda e, r=r, yk_=yk_, t=t, k=k: e.scalar_tensor_tensor(out=r[:], in0=yk_[:], scalar=Wall[:, t, k:k + 1], in1=r[:], op0=ALU.mult, op1=ALU.add),
                 reads=[ykk, rk], writes=[rk])
        of, ofk = ofr.next()
        ob, obk = obr.next()
        ln_core(P, cx, r, rk, gt, bt, of, ofk, None if last else ob, obk, smr.next(), add_eng="vector")
        dst = T["out"] if last else T["h_d"]
        evs.append(P.dma("sync", lambda e, of=of, t=t, dst=dst: e.dma_start(out=dst[t * 128:(t + 1) * 128, :], in_=of[:]), ofk, reads=[ofk]))
        if not last:
            pT, pTk = pTr.next()
            hTt, hTk = hTr.next()
            evs.append(transpose_store_hT(P, ob, obk, pT, pTk, hTt, hTk, idb, T["hT_d"], t, hTk))
    P.flush(evs)
    cx.close()


PARAM_SPECS = [
    ("ln_in_g", (1024,)), ("ln_in_b", (1024,)), ("w_in", (2, 1024, 6144)), ("b_in", (2, 6144)),
    ("q_norm_g", (2, 64)), ("k_norm_g", (2, 64)), ("sc_conv_w", (2, 3, 256)), ("cf_conv_w", (2, 31, 256)),
    ("cf_conv_b", (2, 256)), ("cf_ln_g", (2, 256)), ("cf_ln_b", (2, 256)), ("pool_w", (2, 4, 64, 64)),
    ("pool_scale", (2, 256)), ("w_branch", (2, 4, 256, 1024)), ("w_out", (2, 1024, 1024)),
    ("ln1_g", (2, 1024)), ("ln1_b", (2, 1024)), ("w_router", (2, 1024, 32)), ("b_router", (2, 32)),
    ("w_gate_up", (2, 32, 1024, 2048)), ("b_gate_up", (2, 32, 2048)), ("w_down", (2, 32, 1024, 1024)),
    ("b_down", (2, 32, 1024)), ("ln2_g", (2, 1024)), ("ln2_b", (2, 1024)),
]
CONST_SPECS = [("ident", (128, 128)), ("cos6", (S, 384)), ("sin6", (S, 384)), ("invc", (3, 128, 2, 512)),
               ("utri", (128, 128)), ("iotac", (128, 32))]
SCRATCH = [("h_d", (S, D), F32), ("hT_d", (D, S), BF16), ("zc_d", (1536, S), F32), ("brT_d", (1024, S), BF16),
           ("h1_d", (S, D), F32), ("Xe_d", (NSLOT, D), BF16), ("Ye_d", (NSLOT, D), F32)]


def build_program(upto=99, debug=(), small=False, astage=9):
    global A_STAGE
    A_STAGE = astage
    nc = bass.Bass("TRN2", target_bir_lowering=False)
    T = {}
    T["x"] = nc.dram_tensor("x", [S, D], F32, kind="ExternalInput").ap()
    for nm, shp in PARAM_SPECS + CONST_SPECS:
        if small and nm in ("w_gate_up", "w_down"):
            shp = (2, 1) + tuple(shp[2:])
        T[nm] = nc.dram_tensor(nm, list(shp), F32, kind="ExternalInput").ap()
    T["out"] = nc.dram_tensor("out", [S, D], F32, kind="ExternalOutput").ap()
    for nm, shp, dt in SCRATCH:
        T[nm] = nc.dram_tensor(nm, list(shp), dt, kind="ExternalOutput" if nm in debug else "Internal").ap()
    P = Prog(nc)
    with ExitStack() as top:
        Dall = top.enter_context(nc.sbuf_tensor("Dall", [128, NT, 4], I32))
        Wall = top.enter_context(nc.sbuf_tensor("Wall", [128, NT, 4], F32))
        ph = 0
        phase_ln_in(nc, P, T)
        for l in range(DEPTH):
            if ph >= upto:
                break
            with ExitStack() as att:
                QTa = att.enter_context(nc.sbuf_tensor("QT%d" % l, [128, 4, S], BF16))
                QTb = None
                KT = att.enter_context(nc.sbuf_tensor("KT%d" % l, [128, S], BF16))
                Vaug = att.enter_context(nc.sbuf_tensor("Vaug%d" % l, [128, NT, 2, 66], BF16))
                phase_A(nc, P, T, l, QTa, QTb, KT, Vaug)
                ph += 1
                if ph >= upto:
                    break
                phase_B1(nc, P, T, l, QTa, QTb, KT, Vaug)
                ph += 1
            if ph >= upto:
                break
            phase_B2a(nc, P, T, l)
            ph += 1
            if ph >= upto:
                break
            phase_B2b(nc, P, T, l, Dall, Wall)
            ph += 1
            if ph >= upto:
                break
            phase_C(nc, P, T, l)
            ph += 1
            if ph >= upto:
                break
            phase_D(nc, P, T, l, Dall, Wall, last=(l == DEPTH - 1))
            ph += 1
    return nc


def host_consts():
    c = {}
    c["ident"] = np.eye(128, dtype=np.float32)
    rows = S // 64
    row = np.repeat(np.arange(rows, dtype=np.float32), 64)
    col = np.tile(np.arange(64, dtype=np.float32), rows)
    inv = (np.float32(10000.0) ** (-np.arange(0, 32, 2, dtype=np.float32) / np.float32(32))).astype(np.float32)
    ar = (row[:, None] * inv).astype(np.float32)
    ac = (col[:, None] * inv).astype(np.float32)
    cr, sr, cc, sc = np.cos(ar), np.sin(ar), np.cos(ac), np.sin(ac)
    cos1 = np.concatenate([cr, cr, cc, cc], axis=1).astype(np.float32)
    sin1 = np.concatenate([-sr, sr, -sc, sc], axis=1).astype(np.float32)
    c["cos6"] = np.ascontiguousarray(np.tile(cos1, (1, 6)))
    c["sin6"] = np.ascontiguousarray(np.tile(sin1, (1, 6)))
    invc = np.zeros((3, 128, 2, 512), np.float32)
    wins = (2, 4, 8, 16)
    for ty, blk in enumerate((0, 1, NB - 1)):
        t = np.arange(blk * 512, blk * 512 + 512)
        for gi, w in enumerate(wins):
            lo = np.maximum(t - w // 2, 0)
            hi = np.minimum(t + (w - 1 - w // 2), S - 1)
            ic = (1.0 / (hi - lo + 1)).astype(np.float32)
            cidx, hh = gi // 2, gi % 2
            invc[ty, hh * 64:(hh + 1) * 64, cidx, :] = ic[None, :]
    c["invc"] = invc
    c["utri"] = np.triu(np.ones((128, 128), np.float32), k=1)
    c["iotac"] = np.tile((np.arange(32, dtype=np.float32) * CAP)[None, :], (128, 1)).astype(np.float32)
    return c


_CACHE = {}


def kernel(**inputs):
    if "nc" not in _CACHE:
        _CACHE["nc"] = build_program()
    nc = _CACHE["nc"]
    consts = host_consts()
    shared = {nm: np.ascontiguousarray(np.asarray(inputs[nm], dtype=np.float32)) for nm, _ in PARAM_SPECS}
    shared.update(consts)
    x = np.asarray(inputs["x"], dtype=np.float32)
    in_maps = []
    for b in range(8):
        m = dict(shared)
        m["x"] = np.ascontiguousarray(x[b])
        in_maps.append(m)
    res = run_bass_kernel_spmd(nc, in_maps, core_ids=list(range(8)))
    return np.stack([np.asarray(r["out"]) for r in res.results], axis=0).astype(np.float32)
```
